# Optimizing a Trainium2 kernel written in Bass

```python
import jax, jax.numpy as jnp
from jax import lax
import numpy as np


D_MODEL = 1024
BATCH = 16
SEQ = 2048
DEPTH = 4

GRID_W = 64
CTX_LEN = 256
HEAD_DIM = 64
HQ_A = D_MODEL // (2 * HEAD_DIM)
HKV_A = HQ_A // 4
HQ_B = D_MODEL // (2 * HEAD_DIM)
HKV_B = HQ_B // 4
Q_BLOCK = 128
WINDOW = 128
KEY_SPAN = Q_BLOCK + 2 * WINDOW
ROPE_THETA = 10000.0
ROPE_AXIS_DIM = HEAD_DIM // 2
ATT_WIDTHS = (HQ_A * HEAD_DIM, HKV_A * HEAD_DIM, HKV_A * HEAD_DIM,
              HQ_B * HEAD_DIM, HKV_B * HEAD_DIM, HKV_B * HEAD_DIM)
ATT_SPLITS = tuple(int(s) for s in np.cumsum(ATT_WIDTHS)[:-1])
ATT_IN_WIDTH = sum(ATT_WIDTHS)
ATT_OUT_WIDTH = (HQ_A + HQ_B) * HEAD_DIM
CONV_DIM = D_MODEL
CONV_WIDTH = 3
N_EXPERTS = 16
EXPERT_FF = D_MODEL
CAPACITY_FACTOR = 2
N_ADA = 6
N_ATT_LAYERS = (DEPTH + 1) // 2
N_CONV_LAYERS = DEPTH // 2
EPS = 1e-6
NEG_INF = -1e30

kernel_name = 'hybrid_dit_attn_window_shortconv_ecmoe'


def _rmsnorm(x, g):
    xf = x.astype(jnp.float32)
    xf = xf * lax.rsqrt(jnp.mean(xf * xf, axis=-1, keepdims=True) + EPS)
    return xf.astype(x.dtype) * g


def _modulate(h, shift, scale):
    return h * (1.0 + scale) + shift


def _axial_rope_tables(n_tokens):
    rows = n_tokens // GRID_W
    row = jnp.repeat(jnp.arange(rows, dtype=jnp.float32), GRID_W)
    col = jnp.tile(jnp.arange(GRID_W, dtype=jnp.float32), rows)
    inv_freq = ROPE_THETA ** (-jnp.arange(0, ROPE_AXIS_DIM, 2, dtype=jnp.float32) / ROPE_AXIS_DIM)
    ang_r = row[:, None] * inv_freq[None, :]
    ang_c = col[:, None] * inv_freq[None, :]
    return (jnp.cos(ang_r), jnp.sin(ang_r), jnp.cos(ang_c), jnp.sin(ang_c))


def _rotate(x, cos, sin):
    a, b = jnp.split(x, 2, axis=-1)
    cos = cos[None, :, None, :]
    sin = sin[None, :, None, :]
    return jnp.concatenate([a * cos - b * sin, a * sin + b * cos], axis=-1)


def _apply_axial_rope(x, rope):
    cos_r, sin_r, cos_c, sin_c = rope
    xr, xc = jnp.split(x.astype(jnp.float32), 2, axis=-1)
    return jnp.concatenate([_rotate(xr, cos_r, sin_r), _rotate(xc, cos_c, sin_c)], axis=-1).astype(x.dtype)


def _project_heads(h, w_in):
    p = h @ w_in
    qa, ka, va, qb, kb, vb = jnp.split(p, ATT_SPLITS, axis=-1)
    B, n = h.shape[0], h.shape[1]
    shp = lambda t: t.reshape(B, n, -1, HEAD_DIM)
    return shp(qa), shp(ka), shp(va), shp(qb), shp(kb), shp(vb)


def _sink_column(sink, hkv, g, B, nq):
    return jnp.broadcast_to(sink.astype(jnp.float32).reshape(1, hkv, g, 1, 1), (B, hkv, g, nq, 1))


def _global_attention_latent(q, k, v, k_ctx, v_ctx):
    B, n, hq, hd = q.shape
    hkv = k.shape[2]
    g = hq // hkv
    nb = n // Q_BLOCK
    qblk = (q * hd ** -0.5).reshape(B, nb, Q_BLOCK, hkv, g, hd).transpose(1, 0, 2, 3, 4, 5)

    def one_block(qb):
        s = jnp.concatenate([jnp.einsum('bqhgd,bkhd->bhgqk', qb, k),
                             jnp.einsum('bqhgd,bkhd->bhgqk', qb, k_ctx)], axis=-1)
        p = jax.nn.softmax(s.astype(jnp.float32), axis=-1).astype(v.dtype)
        return (jnp.einsum('bhgqk,bkhd->bqhgd', p[..., :n], v)
                + jnp.einsum('bhgqk,bkhd->bqhgd', p[..., n:], v_ctx))

    o = lax.map(one_block, qblk)
    return o.transpose(1, 0, 2, 3, 4, 5).reshape(B, n, hq * hd)


def _window_attention_latent(q, k, v, k_ctx, v_ctx, sink):
    B, n, hq, hd = q.shape
    hkv = k.shape[2]
    g = hq // hkv
    nb = n // Q_BLOCK
    qblk = (q * hd ** -0.5).reshape(B, nb, Q_BLOCK, hkv, g, hd).transpose(1, 0, 2, 3, 4, 5)
    k_pad = jnp.pad(k, ((0, 0), (WINDOW, WINDOW), (0, 0), (0, 0)))
    v_pad = jnp.pad(v, ((0, 0), (WINDOW, WINDOW), (0, 0), (0, 0)))
    a_idx = jnp.arange(Q_BLOCK)[:, None]
    b_idx = jnp.arange(KEY_SPAN)[None, :]
    band = (b_idx >= a_idx) & (b_idx <= a_idx + 2 * WINDOW)
    sink_col = _sink_column(sink, hkv, g, B, Q_BLOCK)
    L = k_ctx.shape[1]

    def one_block(args):
        blk, qb = args
        start = blk * Q_BLOCK
        kw = lax.dynamic_slice_in_dim(k_pad, start, KEY_SPAN, axis=1)
        vw = lax.dynamic_slice_in_dim(v_pad, start, KEY_SPAN, axis=1)
        j = start - WINDOW + jnp.arange(KEY_SPAN)
        mask = band & ((j >= 0) & (j < n))[None, :]
        s_w = jnp.einsum('bqhgd,bkhd->bhgqk', qb, kw).astype(jnp.float32)
        s_w = jnp.where(mask, s_w, NEG_INF)
        s_c = jnp.einsum('bqhgd,bkhd->bhgqk', qb, k_ctx).astype(jnp.float32)
        s = jnp.concatenate([s_w, s_c, sink_col], axis=-1)
        p = jax.nn.softmax(s, axis=-1).astype(v.dtype)
        return (jnp.einsum('bhgqk,bkhd->bqhgd', p[..., :KEY_SPAN], vw)
                + jnp.einsum('bhgqk,bkhd->bqhgd', p[..., KEY_SPAN:KEY_SPAN + L], v_ctx))

    o = lax.map(one_block, (jnp.arange(nb), qblk))
    return o.transpose(1, 0, 2, 3, 4, 5).reshape(B, n, hq * hd)


def _context_attention(q, k, v, sink=None):
    B, L, hq, hd = q.shape
    hkv = k.shape[2]
    g = hq // hkv
    qg = (q * hd ** -0.5).reshape(B, L, hkv, g, hd)
    s = jnp.einsum('bqhgd,bkhd->bhgqk', qg, k).astype(jnp.float32)
    if sink is not None:
        s = jnp.concatenate([s, _sink_column(sink, hkv, g, B, L)], axis=-1)
    p = jax.nn.softmax(s, axis=-1).astype(v.dtype)[..., :L]
    return jnp.einsum('bhgqk,bkhd->bqhgd', p, v).reshape(B, L, hq * hd)


def _hybrid_attention(h, hc, w_in, w_out, qg_a, kg_a, qg_b, kg_b, sink, rope, need_ctx):
    qa, ka, va, qb, kb, vb = _project_heads(h, w_in)
    qac, kac, vac, qbc, kbc, vbc = _project_heads(hc, w_in)
    qa = _apply_axial_rope(_rmsnorm(qa, qg_a), rope)
    ka = _apply_axial_rope(_rmsnorm(ka, kg_a), rope)
    qb = _apply_axial_rope(_rmsnorm(qb, qg_b), rope)
    kb = _apply_axial_rope(_rmsnorm(kb, kg_b), rope)
    kac = _rmsnorm(kac, kg_a)
    kbc = _rmsnorm(kbc, kg_b)
    o_a = _global_attention_latent(qa, ka, va, kac, vac)
    o_b = _window_attention_latent(qb, kb, vb, kbc, vbc, sink)
    y = jnp.concatenate([o_a, o_b], axis=-1) @ w_out
    yc = None
    if need_ctx:
        oc_a = _context_attention(_rmsnorm(qac, qg_a), kac, vac)
        oc_b = _context_attention(_rmsnorm(qbc, qg_b), kbc, vbc, sink)
        yc = jnp.concatenate([oc_a, oc_b], axis=-1) @ w_out
    return y, yc


def _short_conv(h, w_in, conv_k, conv_b, w_out):
    bg, cg, v = jnp.split(h @ w_in, 3, axis=-1)
    u = cg * v
    y = lax.conv_general_dilated(u, conv_k[:, None, :].astype(u.dtype), window_strides=(1,),
                                 padding=((CONV_WIDTH // 2, CONV_WIDTH // 2),),
                                 dimension_numbers=('NWC', 'WIO', 'NWC'),
                                 feature_group_count=u.shape[-1]) + conv_b
    return (bg * y) @ w_out


def _expert_choice_moe(h, router_w, w_gate, w_up, w_down):
    B, n, _ = h.shape
    cap = CAPACITY_FACTOR * n // N_EXPERTS
    aff = jax.nn.softmax(jnp.einsum('bnd,de->bne', h, router_w).astype(jnp.float32), axis=-1)
    gate, idx = lax.top_k(aff.transpose(0, 2, 1), cap)
    bidx = jnp.arange(B)[:, None, None]
    xe = h[bidx, idx]
    hid = jax.nn.silu(jnp.einsum('becd,edf->becf', xe, w_gate)) * jnp.einsum('becd,edf->becf', xe, w_up)
    ye = jnp.einsum('becf,efd->becd', hid, w_down) * gate[..., None].astype(h.dtype)
    return jnp.zeros_like(h).at[bidx, idx].add(ye)


def setup_inputs(seed: int = 0) -> dict:
    key = jax.random.key(seed)
    ks = jax.random.split(key, 24)
    f32 = jnp.float32
    nrm = lambda k, shape, s: jax.random.normal(k, shape, f32) * s
    D, F = D_MODEL, EXPERT_FF
    return {
        'x': nrm(ks[0], (BATCH, SEQ, D), 1.0),
        'c': nrm(ks[1], (BATCH, D), 1.0),
        'ctx': nrm(ks[2], (BATCH, CTX_LEN, D), 1.0),
        'c_ctx': nrm(ks[3], (D,), 1.0),
        'ada_w': nrm(ks[4], (DEPTH, D, N_ADA * D), 0.5 * D ** -0.5),
        'ada_b': nrm(ks[5], (DEPTH, N_ADA * D), 0.02),
        'norm1_g': 1.0 + nrm(ks[6], (DEPTH, D), 0.1),
        'norm2_g': 1.0 + nrm(ks[7], (DEPTH, D), 0.1),
        'attn_w_in': nrm(ks[8], (N_ATT_LAYERS, D, ATT_IN_WIDTH), D ** -0.5),
        'attn_w_out': nrm(ks[9], (N_ATT_LAYERS, ATT_OUT_WIDTH, D), ATT_OUT_WIDTH ** -0.5),
        'qnorm_a': 1.0 + nrm(ks[10], (N_ATT_LAYERS, HEAD_DIM), 0.1),
        'knorm_a': 1.0 + nrm(ks[11], (N_ATT_LAYERS, HEAD_DIM), 0.1),
        'qnorm_b': 1.0 + nrm(ks[12], (N_ATT_LAYERS, HEAD_DIM), 0.1),
        'knorm_b': 1.0 + nrm(ks[13], (N_ATT_LAYERS, HEAD_DIM), 0.1),
        'sink_b': nrm(ks[14], (N_ATT_LAYERS, HQ_B), 0.5),
        'conv_w_in': nrm(ks[15], (N_CONV_LAYERS, D, 3 * CONV_DIM), D ** -0.5),
        'conv_k': nrm(ks[16], (N_CONV_LAYERS, CONV_WIDTH, CONV_DIM), CONV_WIDTH ** -0.5),
        'conv_b': nrm(ks[17], (N_CONV_LAYERS, CONV_DIM), 0.02),
        'conv_w_out': nrm(ks[18], (N_CONV_LAYERS, CONV_DIM, D), CONV_DIM ** -0.5),
        'router_w': nrm(ks[19], (DEPTH, D, N_EXPERTS), D ** -0.5),
        'moe_w_gate': nrm(ks[20], (DEPTH, N_EXPERTS, D, F), D ** -0.5),
        'moe_w_up': nrm(ks[21], (DEPTH, N_EXPERTS, D, F), D ** -0.5),
        'moe_w_down': nrm(ks[22], (DEPTH, N_EXPERTS, F, D), F ** -0.5),
    }


def reference(x, c, ctx, c_ctx, ada_w, ada_b, norm1_g, norm2_g, attn_w_in, attn_w_out,
              qnorm_a, knorm_a, qnorm_b, knorm_b, sink_b, conv_w_in, conv_k, conv_b, conv_w_out,
              router_w, moe_w_gate, moe_w_up, moe_w_down):
    rope = _axial_rope_tables(x.shape[1])
    silu_c = jax.nn.silu(c)
    silu_cc = jax.nn.silu(c_ctx)
    for i in range(DEPTH):
        need_ctx = i < DEPTH - 1
        j = i // 2
        mod = silu_c @ ada_w[i] + ada_b[i]
        mod_c = silu_cc @ ada_w[i] + ada_b[i]
        sh1, sc1, g1, sh2, sc2, g2 = [m[:, None, :] for m in jnp.split(mod, N_ADA, axis=-1)]
        sh1c, sc1c, g1c, sh2c, sc2c, g2c = jnp.split(mod_c, N_ADA, axis=-1)
        h = _modulate(_rmsnorm(x, norm1_g[i]), sh1, sc1)
        hc = _modulate(_rmsnorm(ctx, norm1_g[i]), sh1c, sc1c)
        if i % 2 == 0:
            y, yc = _hybrid_attention(h, hc, attn_w_in[j], attn_w_out[j], qnorm_a[j], knorm_a[j],
                                      qnorm_b[j], knorm_b[j], sink_b[j], rope, need_ctx)
        else:
            y = _short_conv(h, conv_w_in[j], conv_k[j], conv_b[j], conv_w_out[j])
            yc = _short_conv(hc, conv_w_in[j], conv_k[j], conv_b[j], conv_w_out[j]) if need_ctx else None
        x = x + g1 * y
        x = x + g2 * _expert_choice_moe(_modulate(_rmsnorm(x, norm2_g[i]), sh2, sc2),
                                        router_w[i], moe_w_gate[i], moe_w_up[i], moe_w_down[i])
        if need_ctx:
            ctx = ctx + g1c * yc
            ctx = ctx + g2c * _expert_choice_moe(_modulate(_rmsnorm(ctx, norm2_g[i]), sh2c, sc2c),
                                                 router_w[i], moe_w_gate[i], moe_w_up[i], moe_w_down[i])
    return x
```

```python
import numpy as np
from contextlib import ExitStack
import concourse.bass as bass
import concourse.mybir as mybir
from concourse.bass_utils import run_bass_kernel_spmd

F32 = mybir.dt.float32
BF16 = mybir.dt.bfloat16
I32 = mybir.dt.int32
U32 = mybir.dt.uint32
ALU = mybir.AluOpType
AF = mybir.ActivationFunctionType
AX = mybir.AxisListType

D = 1024
NL = 2048
NCTX = 256
NT = NL + NCTX
NCH = NT // 128
NCHL = NL // 128
DEPTH = 4
NE = 16
CAP_L = 256
CAP_C = 32
EPS = 1e-6
NB = 2

DEBUG_STOP = None
SBDBG = False


class Eng:
    def __init__(self, name, sems):
        self.name = name
        self.sems = sems
        self.cur = 0
        self.count = 0
        self.q = []
        self.waited = {}

    def mark(self):
        if self.count >= 30000:
            self.cur += 1
            self.count = 0
        self.count += 1
        return (self.sems[self.cur], self.count)


class DmaSem:
    def __init__(self, sem):
        self.sem = sem
        self.count = 0


class Buf:
    __slots__ = ("name", "w", "r", "small")

    def __init__(self, name="", small=False):
        self.name = name
        self.w = None
        self.r = {}
        self.small = small


class Planner:
    def __init__(self, nc, es):
        self.nc = nc
        mk = lambda n: es.enter_context(nc.semaphore(n))
        self.engs = {
            "pe": Eng("pe", [mk(f"pe{i}") for i in range(6)]),
            "act": Eng("act", [mk(f"act{i}") for i in range(3)]),
            "dve": Eng("dve", [mk(f"dve{i}") for i in range(3)]),
            "pool": Eng("pool", [mk(f"pool{i}") for i in range(2)]),
            "sp": Eng("sp", [mk("sp0")]),
        }
        self.owner = {}
        for e in self.engs.values():
            for s in e.sems:
                self.owner[s.num] = e.name
        self.es = es
        self.n_dsem = 0
        self.free_ds = []
        self.phase_ds = []

    def dsem(self, name=None):
        if name is None and self.free_ds:
            d = self.free_ds.pop()
            self.phase_ds.append(d)
            return d
        self.n_dsem += 1
        d = DmaSem(self.es.enter_context(self.nc.semaphore(name or f"d{self.n_dsem}")))
        if name is None:
            self.phase_ds.append(d)
        return d

    def wait(self, eng, ev, force=False):
        if ev is None:
            return
        sem, val = ev
        e = self.engs[eng]
        if self.owner.get(sem.num) == eng and not (force and eng in ("act", "dve", "pool")):
            return
        if e.waited.get(sem.num, 0) >= val:
            return
        e.waited[sem.num] = val
        e.q.append(("w", sem, val))

    def op(self, eng, fn, mark=False):
        e = self.engs[eng]
        if mark:
            ev = e.mark()
            e.q.append(("o", fn, ev[0], 1))
            return ev
        e.q.append(("o", fn, None, 0))
        return None

    def use(self, eng, reads=(), writes=()):
        for b in reads:
            self.wait(eng, b.w, True)
        for b in writes:
            self.wait(eng, b.w, True)
            for num, (sem, val) in list(b.r.items()):
                self.wait(eng, (sem, val), b.small)

    @staticmethod
    def done(ev, reads=(), writes=()):
        for b in reads:
            old = b.r.get(ev[0].num)
            if old is None or old[1] < ev[1]:
                b.r[ev[0].num] = ev
        for b in writes:
            b.w = ev
            b.r = {}

    def do(self, eng, fns, reads=(), writes=()):
        if not isinstance(fns, (list, tuple)):
            fns = [fns]
        self.use(eng, reads, writes)
        for fn in fns[:-1]:
            self.op(eng, fn)
        ev = self.op(eng, fns[-1], mark=True)
        self.done(ev, reads, writes)
        return ev

    def dma(self, eng, fns, ds, reads=(), writes=()):
        if not isinstance(fns, (list, tuple)):
            fns = [fns]
        self.use(eng, reads, writes)
        e = self.engs[eng]
        for fn in fns:
            ds.count += 16
            e.q.append(("o", fn, ds.sem, 16))
        ev = (ds.sem, ds.count)
        self.done(ev, reads, writes)
        return ev

    def barrier(self):
        for en in ("sp", "pool", "act", "dve", "pe"):
            for cn in ("pe", "act", "dve"):
                c = self.engs[cn]
                if cn != en and c.count > 0:
                    self.wait(en, (c.sems[c.cur], c.count))

    def flush(self):
        nc = self.nc
        self.barrier()
        for d in self.phase_ds:
            if d.count > 0:
                self.wait("sp", (d.sem, d.count))
                self.wait("pool", (d.sem, d.count))
        self.free_ds.extend(self.phase_ds)
        self.phase_ds = []
        qs = {k: e.q for k, e in self.engs.items()}
        for e in self.engs.values():
            e.q = []

        def replay(q, eng):
            for ent in q:
                if ent[0] == "w":
                    eng.wait_ge(ent[1], ent[2])
                else:
                    ins = ent[1](eng)
                    if ent[2] is not None:
                        ins.then_inc(ent[2], ent[3])

        with nc.Block() as blk:
            if qs["pe"]:
                @blk.tensor
                def _(e):
                    replay(qs["pe"], e)
            if qs["act"]:
                @blk.scalar
                def _(e):
                    replay(qs["act"], e)
            if qs["dve"]:
                @blk.vector
                def _(e):
                    replay(qs["dve"], e)
            if qs["pool"]:
                @blk.gpsimd
                def _(e):
                    replay(qs["pool"], e)
            if qs["sp"]:
                @blk.sync
                def _(e):
                    replay(qs["sp"], e)


def build_program(n_layers=DEPTH, stop=None):
    nc = bass.Bass("TRN2", target_bir_lowering=False)
    es = ExitStack()
    P = Planner(nc, es)

    def din(name, shape, dt=F32):
        return nc.dram_tensor(name, shape, dt, kind="ExternalInput").ap()

    x_in = din("x", [NB, NL, D])
    ctx_in = din("ctx", [NB, NCTX, D])
    cvec = din("cvec", [3, D])
    ada_w = din("ada_w", [DEPTH, D, 6 * D])
    ada_b = din("ada_b", [DEPTH, 6 * D])
    norm1_g = din("norm1_g", [DEPTH, D])
    norm2_g = din("norm2_g", [DEPTH, D])
    attn_w_in = din("attn_w_in", [2, D, 1536])
    attn_w_out = din("attn_w_out", [2, D, D])
    qk_gain = din("qk_gain", [2, 4, 64])
    sink_b = din("sink_b", [2, 8])
    conv_w_in = din("conv_w_in", [2, D, 3 * D])
    conv_kb = din("conv_kb", [2, 4, D])
    conv_w_out = din("conv_w_out", [2, D, D])
    router_w = din("router_w", [DEPTH, D, NE])
    w_gate = din("moe_w_gate", [DEPTH, NE, D, D])
    w_up = din("moe_w_up", [DEPTH, NE, D, D])
    w_down = din("moe_w_down", [DEPTH, NE, D, D])
    rope_in = din("rope", [NL, 64])
    ident_in = din("ident", [128, 128])
    masks_in = din("masks", [2, 128, 128])
    out = nc.dram_tensor("out", [NB, NL, D], F32, kind="ExternalOutput").ap()

    dbg = bool(stop) or n_layers < DEPTH
    kindI = "ExternalOutput" if dbg else "Internal"
    X = [nc.dram_tensor(f"Xs{b}", [NT, D], F32, kind=kindI).ap() for b in range(NB)]
    H = [nc.dram_tensor(f"Hs{b}", [NT, D], BF16, kind=kindI).ap() for b in range(NB)]
    MOD = nc.dram_tensor("MODs", [DEPTH, 3, 6 * D], F32, kind=kindI).ap()
    DBG = nc.dram_tensor("DBG", [128, 4096], F32, kind="ExternalOutput").ap() if dbg else None
    DBGQ = nc.dram_tensor("DBGQ", [128, 8, NT], F32, kind="ExternalOutput").ap() if dbg else None
    DBGK = nc.dram_tensor("DBGK", [128, 4, 2, NT], F32, kind="ExternalOutput").ap() if dbg else None
    DBGV = nc.dram_tensor("DBGV", [128, NCH, 4, 65], F32, kind="ExternalOutput").ap() if dbg else None
    DBGO = nc.dram_tensor("DBGO", [128, NCH, D], F32, kind="ExternalOutput").ap() if dbg else None

    Xc = [[Buf(f"X{b}_{c}") for c in range(NCH)] for b in range(NB)]
    Hc = [[Buf(f"H{b}_{c}") for c in range(NCH)] for b in range(NB)]
    MODb = Buf("MOD")

    _uid = [0]

    def sb(name, shape, dt, st=es):
        _uid[0] += 1
        if SBDBG:
            print("SB", name, shape, dt, "remaining", nc.sbuf_bytes_remaining)
        return st.enter_context(nc.sbuf_tensor(f"{name}_{_uid[0]}", shape, dt))

    PS = [es.enter_context(nc.psum_tensor(f"ps{i}", [128, 512], F32)) for i in range(8)]
    PSb = [Buf(f"ps{i}") for i in range(8)]

    ident_f = sb("ident_f", [128, 128], F32)
    ident_b = sb("ident_b", [128, 128], BF16)
    mask_t = sb("mask_t", [128, 2, 128], BF16)
    mask_f = sb("mask_f", [128, 2, 128], F32)
    rope_t = sb("rope_t", [128, NCHL, 64], F32)
    xt = [sb(f"xt{i}", [128, D], F32) for i in range(2)]
    xtb = [Buf(f"xt{i}") for i in range(2)]
    xt_ds = [P.dsem(f"xtd{i}") for i in range(2)]
    modt = [sb(f"modt{i}", [128, D], F32) for i in range(6)]
    modb = [Buf(f"modt{i}") for i in range(6)]
    mod_ds = [P.dsem(f"modd{i}") for i in range(6)]
    affT = sb("affT", [48, NT], F32)
    affTb = Buf("affT")
    eps_t = sb("eps_t", [128, 1], F32)
    const_b = Buf("consts")
    setup_ds = P.dsem("setup")
    setup2_ds = P.dsem("setup2")

    def phase0():
        with ExitStack() as st:
            cs = sb("cs", [3, D], F32, st)
            scs = sb("scs", [3, D], F32, st)
            scT = sb("scT", [128, 8, 3], F32, st)
            adab = sb("adab", [3, 6 * D], F32, st)
            modrow = sb("modrow", [3, 6 * D], F32, st)
            wsl = [sb(f"adaw{i}", [128, 8, 512], F32, st) for i in range(2)]
            wslb = [Buf() for _ in range(2)]
            wds = [P.dsem() for i in range(2)]
            csb, adabb, modrowb, scTb = Buf(), Buf(), Buf(), Buf()
            ds2 = P.dsem()
            store_ds = P.dsem("p0store")

            fns = []
            for b in range(NB):
                for r0 in range(0, NL, 256):
                    fns.append(lambda e, b=b, r0=r0: e.dma_start(out=X[b][r0:r0 + 256, :], in_=x_in[b, r0:r0 + 256, :]))
                fns.append(lambda e, b=b: e.dma_start(out=X[b][NL:NT, :], in_=ctx_in[b, :, :]))
            P.dma("sp", fns, setup_ds, writes=[c for b in range(NB) for c in Xc[b]])
            fns = [
                lambda e: e.dma_start(out=ident_f[:], in_=ident_in),
                lambda e: e.dma_start(out=mask_f[:], in_=masks_in.rearrange("m k q -> k m q")),
                lambda e: e.dma_start(out=rope_t[:], in_=rope_in.rearrange("(c p) f -> p c f", p=128)),
                lambda e: e.dma_start(out=cs[:], in_=cvec),
            ]
            P.dma("sp", fns, setup2_ds, writes=[const_b, csb])
            P.do("dve", [lambda e: e.tensor_copy(ident_b[:], ident_f[:]),
                         lambda e: e.tensor_copy(mask_t[:], mask_f[:]),
                         lambda e: e.memset(eps_t[:], EPS),
                         lambda e: e.memset(affT[:], 0.0)], reads=[], writes=[const_b, affTb])
            scsb = Buf()
            P.do("act", lambda e: e.activation(out=scs[:], in_=cs[:], func=AF.Silu), reads=[csb], writes=[scsb])
            fns = []
            for k in range(8):
                fns.append(lambda e, k=k: e.transpose(PS[0][:, k * 3:(k + 1) * 3], scs[0:3, k * 128:(k + 1) * 128], ident_f[0:3, 0:3]))
            P.do("pe", fns, reads=[scsb, const_b], writes=[PSb[0]])
            P.do("dve", lambda e: e.tensor_copy(scT[:].rearrange("p k j -> p (k j)"), PS[0][:, 0:24]), reads=[PSb[0]], writes=[scTb])

            nblk = 12
            it = 0
            for l in range(n_layers):
                P.dma("sp", lambda e, l=l: e.dma_start(out=adab[:], in_=ada_b[l].partition_broadcast(3)), ds2, writes=[adabb])
                for nb in range(nblk):
                    s = it % 2
                    it += 1
                    P.dma("sp", lambda e, l=l, nb=nb, s=s: e.dma_start(
                        out=wsl[s][:], in_=ada_w[l].rearrange("(k p) n -> p k n", p=128)[:, :, nb * 512:(nb + 1) * 512]),
                        wds[s], writes=[wslb[s]])
                    pb = 1 + (it % 2)
                    fns = []
                    for k in range(8):
                        fns.append(lambda e, k=k, s=s, pb=pb: e.matmul(PS[pb][0:3, :], lhsT=scT[:, k, :], rhs=wsl[s][:, k, :],
                                                                      start=(k == 0), stop=(k == 7)))
                    P.do("pe", fns, reads=[wslb[s], scTb], writes=[PSb[pb]])
                    P.do("dve", lambda e, nb=nb, pb=pb: e.tensor_tensor(modrow[:, nb * 512:(nb + 1) * 512], PS[pb][0:3, :],
                                                                      adab[:, nb * 512:(nb + 1) * 512], op=ALU.add),
                         reads=[PSb[pb], adabb], writes=[modrowb])
                P.dma("sp", lambda e, l=l: e.dma_start(out=MOD[l], in_=modrow[:]), store_ds, reads=[modrowb], writes=[MODb])
        P.flush()

    dbg_ds = P.dsem("dbg") if dbg else None

    def dump(src_ap, bufs, col0, ncols, rows=128):
        if not dbg:
            return
        P.dma("pool", lambda e: e.dma_start(out=DBG[0:rows, col0:col0 + ncols], in_=src_ap), dbg_ds, reads=bufs)

    def load_mod(slot, l, j, idx):
        return P.dma("sp", lambda e: e.dma_start(out=modt[slot][:], in_=MOD[l, j, idx * D:(idx + 1) * D].partition_broadcast(128)),
                     mod_ds[slot], reads=[MODb], writes=[modb[slot]])

    def load_scale(slot, l, j, idx, gsrc, gtile, gtb, gds):
        load_mod(slot, l, j, idx)
        P.dma("sp", lambda e: e.dma_start(out=gtile[:], in_=gsrc[l].partition_broadcast(128)), gds, writes=[gtb])
        P.do("dve", lambda e: e.scalar_tensor_tensor(out=modt[slot][:], in0=modt[slot][:], scalar=1.0, in1=gtile[:],
                                                     op0=ALU.add, op1=ALU.mult), reads=[gtb], writes=[modb[slot]])

    class NormCtx:
        def __init__(self, st, tag):
            self.junk = sb(f"junk{tag}", [128, D], BF16, st)
            self.ss = sb(f"ss{tag}", [128, 8], F32, st)
            self.tmp = sb(f"ntmp{tag}", [128, D], F32, st)
            self.sb_ = [Buf(small=True), Buf(small=True)]
            self.tb = Buf()
            self.i = 0
            P.do("dve", lambda e: e.memset(self.ss[:], 0.0), writes=self.sb_)

    def rms_mod(nctx, src, srcb, A, Ab, Bt, Bb, dst, dstb):
        p = nctx.i % 2
        nctx.i += 1
        ss = nctx.ss[:, 4 * p:4 * p + 4]
        ssb = nctx.sb_[p]
        P.do("act", lambda e: e.activation(out=nctx.junk[:], in_=src[:], func=AF.Square, accum_out=ss[:, 0:1]),
             reads=[srcb, const_b], writes=[ssb])
        P.do("act", lambda e: e.activation(out=ss[:, 1:2], in_=ss[:, 0:1], func=AF.Ln, bias=eps_t[:, 0:1], scale=1.0 / D),
             reads=[const_b], writes=[ssb])
        P.do("act", lambda e: e.activation(out=ss[:, 2:3], in_=ss[:, 1:2], func=AF.Exp, scale=-0.5), writes=[ssb])
        P.do("dve", lambda e: e.scalar_tensor_tensor(out=nctx.tmp[:], in0=src[:], scalar=ss[:, 2:3], in1=A[:],
                                                     op0=ALU.mult, op1=ALU.mult),
             reads=[srcb, Ab, ssb], writes=[nctx.tb])
        P.do("dve", lambda e: e.tensor_tensor(dst[:], nctx.tmp[:], Bt[:], op=ALU.add),
             reads=[Bb, nctx.tb], writes=[dstb])
        P.do("dve", lambda e: e.memset(ss[:, 0:1], 0.0), writes=[ssb])

    def transpose8(src, srcb, rows, bank, dstap, dstb, evac="act"):
        pv = PS[bank][:].bitcast(BF16)
        fns = []
        for k in range(8):
            fns.append(lambda e, k=k: e.transpose(pv[:, k * rows:(k + 1) * rows], src[0:rows, k * 128:(k + 1) * 128],
                                                  ident_b[0:rows, 0:rows]))
        P.do("pe", fns, reads=[srcb, const_b], writes=[PSb[bank]])
        inap = pv[:, 0:8 * rows].rearrange("p (k r) -> p k r", r=rows)
        if evac == "act":
            P.do("act", lambda e: e.activation(out=dstap, in_=inap, func=AF.Copy), reads=[PSb[bank]], writes=[dstb])
        else:
            P.do("dve", lambda e: e.tensor_copy(dstap, inap), reads=[PSb[bank]], writes=[dstb])

    class Epi:
        pass

    def make_epi(st, l, b, chunks):
        ep = Epi()
        ep.xn = [sb(f"xn{i}", [128, D], F32, st) for i in range(2)]
        ep.xnb = [Buf() for _ in range(2)]
        ep.xn_ds = [P.dsem() for _ in range(2)]
        ep.h2 = [sb(f"h2_{i}", [128, D], BF16, st) for i in range(2)]
        ep.h2b = [Buf() for _ in range(2)]
        ep.h2_ds = [P.dsem() for _ in range(2)]
        ep.h2T = sb("h2T", [128, 8, 128], BF16, st)
        ep.h2Tb = Buf()
        ep.nctx = NormCtx(st, "e")
        ep.wr = sb("wr", [128, 8, NE], BF16, st)
        ep.wrb = Buf()
        ep.sm = sb("sm", [128, 8], F32, st)
        ep.smb = Buf(small=True)
        ep.ex = sb("ex", [128, NE], F32, st)
        ep.aff = sb("aff48", [128, 48], F32, st)
        ep.affb = Buf(small=True)
        ep.ds = P.dsem()
        ep.cnt = 0
        P.dma("pool", lambda e: e.dma_start(out=ep.wr[:], in_=router_w[l].rearrange("(k p) n -> p k n", p=128)),
              ep.ds, writes=[ep.wrb])
        P.do("dve", lambda e: e.memset(ep.aff[:], 0.0), writes=[ep.affb])
        return ep

    def epilogue_gen(ep, l, b, c, ybanks, T_bank, L_bank):
        isctx = c >= NCHL
        G = modt[5] if isctx else modt[2]
        Gb = modb[5] if isctx else modb[2]
        A2 = modt[3] if isctx else modt[0]
        A2b = modb[3] if isctx else modb[0]
        B2 = modt[4] if isctx else modt[1]
        B2b = modb[4] if isctx else modb[1]
        s = ep.cnt % 2
        ep.cnt += 1
        xs = xt[s]
        P.dma("sp", lambda e: e.dma_start(out=xs[:], in_=X[b][c * 128:(c + 1) * 128, :]), xt_ds[s],
              reads=[Xc[b][c]], writes=[xtb[s]])
        xn, xnb = ep.xn[s], ep.xnb[s]
        P.do("dve", [lambda e: e.tensor_tensor(xn[:, 0:512], PS[ybanks[0]][:], G[:, 0:512], op=ALU.mult),
                     lambda e: e.tensor_tensor(xn[:, 512:1024], PS[ybanks[1]][:], G[:, 512:1024], op=ALU.mult)],
             reads=[PSb[ybanks[0]], PSb[ybanks[1]], Gb], writes=[xnb])
        P.do("dve", lambda e: e.tensor_tensor(xn[:], xn[:], xs[:], op=ALU.add), reads=[xtb[s], xnb], writes=[xnb])
        P.dma("sp", lambda e: e.dma_start(out=X[b][c * 128:(c + 1) * 128, :], in_=xn[:]), ep.xn_ds[s],
              reads=[xnb], writes=[Xc[b][c]])
        h2, h2b = ep.h2[s], ep.h2b[s]
        rms_mod(ep.nctx, xn, xnb, A2, A2b, B2, B2b, h2, h2b)
        P.dma("sp", lambda e: e.dma_start(out=H[b][c * 128:(c + 1) * 128, :], in_=h2[:]), ep.h2_ds[s],
              reads=[h2b], writes=[Hc[b][c]])
        yield
        transpose8(h2, h2b, 128, T_bank, ep.h2T[:], ep.h2Tb)
        yield
        fns = []
        for k in range(8):
            fns.append(lambda e, k=k: e.matmul(PS[L_bank][:, 0:NE], lhsT=ep.h2T[:, k, :], rhs=ep.wr[:, k, :],
                                               start=(k == 0), stop=(k == 7)))
        P.do("pe", fns, reads=[ep.h2Tb, ep.wrb], writes=[PSb[L_bank]])
        sm = ep.sm
        col0 = 32 * b
        P.do("dve", [lambda e: e.memset(sm[:, 2:3], 0.0),
                     lambda e: e.reduce_max(out=sm[:, 0:1], in_=PS[L_bank][:, 0:NE], axis=AX.X)],
             reads=[PSb[L_bank]], writes=[ep.smb])
        P.do("dve", lambda e: e.tensor_scalar(sm[:, 1:2], sm[:, 0:1], -1.0, None, op0=ALU.mult), writes=[ep.smb])
        P.do("act", lambda e: e.activation(out=ep.ex[:], in_=PS[L_bank][:, 0:NE], func=AF.Exp, bias=sm[:, 1:2],
                                           accum_out=sm[:, 2:3]), reads=[PSb[L_bank], ep.smb], writes=[ep.smb])
        P.do("dve", lambda e: e.reciprocal(sm[:, 3:4], sm[:, 2:3]), writes=[ep.smb])
        P.do("dve", lambda e: e.tensor_scalar(ep.aff[:, col0:col0 + NE], ep.ex[:], sm[:, 3:4], None, op0=ALU.mult),
             reads=[ep.smb], writes=[ep.affb])
        ncol = col0 + NE
        yield
        P.do("pe", lambda e: e.transpose(PS[L_bank][0:ncol, 128:256], ep.aff[:, 0:ncol], ident_f[:]),
             reads=[ep.affb, const_b], writes=[PSb[L_bank]])
        P.do("dve", lambda e: e.tensor_copy(affT[col0:col0 + NE, c * 128:(c + 1) * 128], PS[L_bank][col0:col0 + NE, 128:256]),
             reads=[PSb[L_bank]], writes=[affTb])

    def epilogue(ep, l, b, c, ybanks, T_bank, L_bank):
        for _ in epilogue_gen(ep, l, b, c, ybanks, T_bank, L_bank):
            pass

    class Sched:
        def __init__(self):
            self.gens = []

        def add(self, g):
            self.gens.append(g)

        def step(self):
            alive = []
            for g in self.gens:
                try:
                    next(g)
                    alive.append(g)
                except StopIteration:
                    pass
            self.gens = alive

        def drain(self):
            while self.gens:
                self.step()

    def norm1_pass(st, l, b, chunks, hT, hTb, T_bank):
        nctx = NormCtx(st, "n")
        htok = [sb(f"htok{i}", [128, D], BF16, st) for i in range(2)]
        htokb = [Buf() for _ in range(2)]
        xts = list(xt) + [sb(f"xtn{i}", [128, D], F32, st) for i in range(2)]
        xtbs = list(xtb) + [Buf(), Buf()]
        xds = list(xt_ds) + [P.dsem(), P.dsem()]
        NSL = 4

        def load(i):
            c = chunks[i]
            s = i % NSL
            P.dma("sp", lambda e: e.dma_start(out=xts[s][:], in_=X[b][c * 128:(c + 1) * 128, :]), xds[s],
                  reads=[Xc[b][c]], writes=[xtbs[s]])

        for i in range(min(NSL - 1, len(chunks))):
            load(i)
        for i, c in enumerate(chunks):
            if i + NSL - 1 < len(chunks):
                load(i + NSL - 1)
            s = i % NSL
            h = i % 2
            isctx = c >= NCHL
            A, Ab = (modt[3], modb[3]) if isctx else (modt[0], modb[0])
            Bt, Bb = (modt[4], modb[4]) if isctx else (modt[1], modb[1])
            rms_mod(nctx, xts[s], xtbs[s], A, Ab, Bt, Bb, htok[h], htokb[h])
            if l == 0 and b == 0 and c == 0:
                dump(nctx.ss[:, 0:4], nctx.sb_, 0, 4)
                dump(htok[h][:], [htokb[h]], 1024, 1024)
                dump(A[:], [Ab], 2048, 1024)
                dump(Bt[:], [Bb], 3072, 1024)
            transpose8(htok[h], htokb[h], 128, T_bank, hT[:, :, c * 128:(c + 1) * 128], hTb[c])

    def load_mixer_mods(st, l, b, has_ctx, which, shared=None):
        if shared is None:
            shared = (sb(f"gtile{which}", [128, D], F32, st), Buf())
        gt, gtb = shared
        gds = P.dsem()
        base = 0 if which == 1 else 3
        gsrc = norm1_g if which == 1 else norm2_g
        load_scale(0, l, b, base + 1, gsrc, gt, gtb, gds)
        load_mod(1, l, b, base + 0)
        if which == 1:
            load_mod(2, l, b, 2)
        if has_ctx:
            load_scale(3, l, 2, base + 1, gsrc, gt, gtb, gds)
            load_mod(4, l, 2, base + 0)
            if which == 1:
                load_mod(5, l, 2, 2)
        return shared

    def load_win(st, l):
        j = l // 2
        Win = sb("Win", [128, 8, 1536], BF16, st)
        Winb = Buf()
        wds = P.dsem(f"winds{l}")
        src = attn_w_in[j].rearrange("(k p) n -> p k n", p=128)
        colmap = [(0, 512, 0), (768, 1280, 512), (512, 640, 1024), (1280, 1408, 1152), (640, 768, 1280), (1408, 1536, 1408)]
        fns = [lambda e, a=a, bb=bb, o=o: e.dma_start(out=Win[:, :, o:o + (bb - a)], in_=src[:, :, a:bb]) for a, bb, o in colmap]
        P.dma("pool", fns, wds, writes=[Winb])
        return Win, Winb

    def mixer_attn(l, b, has_ctx, Win, Winb):
        j = l // 2
        chunks = list(range(NCH if has_ctx else NCHL))
        with ExitStack() as st0:
            hq = sb("hq", [128, 8, NT], BF16, st0)
            hqb = [Buf() for _ in range(NCH)]
            kT = sb("kT", [128, 4, 2, NT], BF16, st0)
            kTb = [Buf() for _ in range(NCH)]
            vaug = sb("vaug", [128, NCH, 4, 65], BF16, st0)
            vb = [Buf() for _ in range(NCH)]
            ones_b = Buf()
            Wout = sb("Wout", [128, 8, D], BF16, st0)
            Woutb = Buf()
            with ExitStack() as st:
                gsh = load_mixer_mods(st, l, b, has_ctx, 1)
                g4 = sb("g4", [128, 4, 64], F32, st)
                GN = sb("GN", [128, 1280], F32, st)
                GNb = Buf()
                gds_ = P.dsem()
                P.dma("sp", lambda e: e.dma_start(out=g4[:].rearrange("p a d -> p (a d)"),
                                                  in_=qk_gain[j].rearrange("a d -> (a d)").partition_broadcast(128)),
                      gds_, writes=[GNb])
                hoffs = [(0, 0, 8), (1, 512, 8), (2, 1024, 2), (3, 1152, 2)]
                fns = []
                for gi, off, nh in hoffs:
                    fns.append(lambda e, gi=gi, off=off, nh=nh: e.tensor_copy(
                        GN[:, off:off + nh * 64].rearrange("p (h d) -> p h d", d=64),
                        g4[:, gi:gi + 1, :].to_broadcast([128, nh, 64])))
                P.do("dve", fns, reads=[GNb], writes=[GNb])
                P.do("dve", lambda e: e.tensor_scalar(GN[:, 0:1024], GN[:, 0:1024], 0.125, None, op0=ALU.mult), reads=[GNb], writes=[GNb])
                ktok = sb("ktok", [128, 4, 2, 128], BF16, st)
                ktokb = Buf()
                P.do("dve", [lambda e: e.memset(vaug[:, :, :, 64:65], 1.0), lambda e: e.memset(ktok[:], 0.0)],
                     writes=[ones_b, ktokb])
                with ExitStack() as stn:
                    norm1_pass(stn, l, b, chunks, hq, hqb, 0)
                P.barrier()
                load_mixer_mods(st, l, b, has_ctx, 2, gsh)
                wods = P.dsem()
                P.dma("pool", lambda e: e.dma_start(out=Wout[:], in_=attn_w_out[j].rearrange("(k p) n -> p k n", p=128)), wods, writes=[Woutb])

                sq2 = [sb(f"sq{i}", [128, 1280], F32, st) for i in range(2)]
                qn_1 = sb("qn", [128, 1280], F32, st)
                qn2 = [qn_1, qn_1]
                ssq2 = [sb(f"ssq{i}", [128, 64], F32, st) for i in range(2)]
                rt_1 = [sb(f"rt{i}", [128, 640], F32, st) for i in range(2)]
                rt2 = [rt_1, rt_1]
                qkn2 = [sb(f"qkn{i}", [128, 1280], BF16, st) for i in range(2)]
                wkb_1 = Buf()
                wkb2 = [wkb_1, wkb_1]
                sqb2 = [Buf(), Buf()]
                qknb2 = [Buf() for _ in range(2)]
                stb2 = [Buf(small=True) for _ in range(2)]
                rtb_1 = Buf()
                rtb2 = [rtb_1, rtb_1]
                pbanks2 = [(1, 2, 3), (0, 6, 7)]

                def projB(i, c):
                    cs_ = slice(c * 128, (c + 1) * 128)
                    pb = pbanks2[i % 2]
                    for nb_ in range(3):
                        fns = [lambda e, k=k, nb_=nb_: e.matmul(
                            PS[pb[nb_]][:], lhsT=hq[:, k, cs_], rhs=Win[:, k, nb_ * 512:(nb_ + 1) * 512],
                            start=(k == 0), stop=(k == 7)) for k in range(8)]
                        P.do("pe", fns, reads=[hqb[c], Winb], writes=[PSb[pb[nb_]]])

                def restB(i, c):
                    cs_ = slice(c * 128, (c + 1) * 128)
                    p = i % 2
                    pb = pbanks2[p]
                    sq, qn, ssq, rt, qkn = sq2[p], qn2[p], ssq2[p], rt2[p], qkn2[p]
                    wkb, qknb, stb, rtb_ = wkb2[p], qknb2[p], stb2[p], rtb2[p]
                    pbb = [PSb[x] for x in pb]
                    P.do("act", [lambda e: e.activation(out=sq[:, 0:512], in_=PS[pb[0]][:], func=AF.Square),
                                 lambda e: e.activation(out=sq[:, 512:1024], in_=PS[pb[1]][:], func=AF.Square),
                                 lambda e: e.activation(out=sq[:, 1024:1280], in_=PS[pb[2]][:, 0:256], func=AF.Square)],
                         reads=pbb, writes=[sqb2[p]])
                    P.do("act", lambda e: e.activation(out=vaug[:, c, :, 0:64], in_=PS[pb[2]][:, 256:512].rearrange("p (g d) -> p g d", d=64),
                                                       func=AF.Copy), reads=[pbb[2], ones_b], writes=[vb[c]])
                    P.do("dve", lambda e: e.tensor_reduce(out=ssq[:, 0:20], in_=sq[:].rearrange("p (h d) -> p h d", d=64),
                                                          axis=AX.X, op=ALU.add), reads=[sqb2[p]], writes=[stb])
                    P.do("act", lambda e: e.activation(out=ssq[:, 20:40], in_=ssq[:, 0:20], func=AF.Ln, bias=eps_t[:, 0:1], scale=1.0 / 64),
                         reads=[const_b], writes=[stb])
                    P.do("act", lambda e: e.activation(out=ssq[:, 40:60], in_=ssq[:, 20:40], func=AF.Exp, scale=-0.5), writes=[stb])

                def restB2(i, c):
                    cs_ = slice(c * 128, (c + 1) * 128)
                    p = i % 2
                    pb = pbanks2[p]
                    sq, qn, ssq, rt, qkn = sq2[p], qn2[p], ssq2[p], rt2[p], qkn2[p]
                    wkb, qknb, stb, rtb_ = wkb2[p], qknb2[p], stb2[p], rtb2[p]
                    pbb = [PSb[x] for x in pb]
                    P.do("dve", [
                        lambda e: e.tensor_tensor(qn[:, 0:512].rearrange("p (h d) -> p h d", d=64),
                                                  PS[pb[0]][:].rearrange("p (h d) -> p h d", d=64),
                                                  ssq[:, 40:48].unsqueeze(2).to_broadcast([128, 8, 64]), op=ALU.mult),
                        lambda e: e.tensor_tensor(qn[:, 512:1024].rearrange("p (h d) -> p h d", d=64),
                                                  PS[pb[1]][:].rearrange("p (h d) -> p h d", d=64),
                                                  ssq[:, 48:56].unsqueeze(2).to_broadcast([128, 8, 64]), op=ALU.mult),
                        lambda e: e.tensor_tensor(qn[:, 1024:1280].rearrange("p (h d) -> p h d", d=64),
                                                  PS[pb[2]][:, 0:256].rearrange("p (h d) -> p h d", d=64),
                                                  ssq[:, 56:60].unsqueeze(2).to_broadcast([128, 4, 64]), op=ALU.mult)],
                        reads=pbb + [stb], writes=[wkb])
                    P.do("dve", lambda e: e.tensor_tensor(qn[:], qn[:], GN[:], op=ALU.mult), reads=[wkb, GNb], writes=[wkb])
                    if c < NCHL:
                        qv = qn[:].rearrange("p (h rc ab f) -> p h rc ab f", rc=2, ab=2, f=16)
                        ov = qkn[:].rearrange("p (h rc ab f) -> p h rc ab f", rc=2, ab=2, f=16)
                        Aq = qv[:, :, :, 0, :]
                        Bq = qv[:, :, :, 1, :]
                        cosv = rope_t[:, c, 0:32].rearrange("p (rc f) -> p rc f", f=16).unsqueeze(1).to_broadcast([128, 20, 2, 16])
                        sinv = rope_t[:, c, 32:64].rearrange("p (rc f) -> p rc f", f=16).unsqueeze(1).to_broadcast([128, 20, 2, 16])
                        r4 = [t[:].rearrange("p (h rc f) -> p h rc f", rc=2, f=16) for t in rt]
                        P.do("dve", [
                            lambda e: e.tensor_tensor(r4[0], Aq, cosv, op=ALU.mult),
                            lambda e: e.tensor_tensor(r4[1], Bq, sinv, op=ALU.mult)],
                            reads=[wkb, const_b], writes=[rtb_])
                        P.do("dve", lambda e: e.tensor_tensor(ov[:, :, :, 0, :], r4[0], r4[1], op=ALU.subtract),
                             reads=[rtb_], writes=[qknb])
                        P.do("dve", [
                            lambda e: e.tensor_tensor(r4[0], Aq, sinv, op=ALU.mult),
                            lambda e: e.tensor_tensor(r4[1], Bq, cosv, op=ALU.mult)],
                            reads=[wkb, const_b], writes=[rtb_])
                        P.do("dve", lambda e: e.tensor_tensor(ov[:, :, :, 1, :], r4[0], r4[1], op=ALU.add),
                             reads=[rtb_, qknb], writes=[qknb])
                    else:
                        P.do("dve", lambda e: e.tensor_copy(qkn[:], qn[:]), reads=[wkb], writes=[qknb])
                    kv = qkn[:, 1024:1280].rearrange("p (g d) -> p g d", d=64)
                    P.do("dve", [lambda e: e.tensor_copy(ktok[:, :, 0, 0:64], kv),
                                 lambda e: e.tensor_copy(ktok[:, :, 1, 64:128], kv)], reads=[qknb], writes=[ktokb])
                    pv = PS[4][:].bitcast(BF16)
                    fns = [lambda e, i_=i_: e.transpose(pv[:, i_ * 128:(i_ + 1) * 128], qkn[:, i_ * 128:(i_ + 1) * 128], ident_b[:]) for i_ in range(8)]
                    P.do("pe", fns, reads=[qknb, const_b], writes=[PSb[4]])
                    P.do("act", lambda e: e.activation(out=hq[:, :, cs_], in_=pv.rearrange("p (k r) -> p k r", r=128), func=AF.Copy),
                         reads=[PSb[4]], writes=[hqb[c]])
                    pk = PS[5][:].bitcast(BF16)
                    fns = [lambda e, i_=i_: e.transpose(pk[:, i_ * 128:(i_ + 1) * 128], ktok[:, i_ // 2, i_ % 2, :], ident_b[:]) for i_ in range(8)]
                    P.do("pe", fns, reads=[ktokb, const_b], writes=[PSb[5]])
                    P.do("act", lambda e: e.activation(out=kT[:, :, :, cs_], in_=pk.rearrange("p (g r t) -> p g r t", r=2, t=128), func=AF.Copy),
                         reads=[PSb[5]], writes=[kTb[c]])

                projB(0, chunks[0])
                if len(chunks) > 1:
                    projB(1, chunks[1])
                restB(0, chunks[0])
                for i, c in enumerate(chunks):
                    if i + 1 < len(chunks):
                        restB(i + 1, chunks[i + 1])
                    restB2(i, c)
                    if i + 2 < len(chunks):
                        projB(i + 2, chunks[i + 2])
                if dbg and l == 0 and b == 0:
                    P.dma("pool", [lambda e: e.dma_start(out=DBGQ, in_=hq[:]), lambda e: e.dma_start(out=DBGK, in_=kT[:]),
                                   lambda e: e.dma_start(out=DBGV, in_=vaug[:])], dbg_ds, reads=hqb + kTb + vb)
            P.flush()
            with ExitStack() as st:
                ep = make_epi(st, l, b, chunks)
                sk0 = sb("sk0", [128, 8], F32, st)
                sk = sb("sk", [128, 8], F32, st)
                skb = Buf(small=True)
                skds = P.dsem()
                P.dma("sp", lambda e: e.dma_start(out=sk0[:], in_=sink_b[j].partition_broadcast(128)), skds, writes=[skb])
                P.do("act", lambda e: e.activation(out=sk0[:], in_=sk0[:], func=AF.Exp), reads=[skb], writes=[skb])
                P.do("dve", lambda e: e.tensor_copy(sk[:].rearrange("p (g r i) -> p g r i", r=2, i=2),
                                                    sk0[:].rearrange("p (g i r) -> p g r i", r=2, i=2)), reads=[skb], writes=[skb])
                PT = [sb(f"PT{i}", [128, 512], BF16, st) for i in range(3)]
                PTb = [Buf() for _ in range(3)]
                otok = [sb(f"otok{i}", [128, D], BF16, st) for i in range(2)]
                otokb = [Buf() for _ in range(2)]
                oT = sb("oT", [128, 8, 128], BF16, st)
                oTb = Buf()
                den = sb("den", [128, 8], F32, st)
                denb = Buf(small=True)
                S_banks = [0, 1]
                O_banks = [2, 3]
                sctr = 0
                octr = 0
                pctr = 0
                items = []
                for qi, qb in enumerate(chunks):
                    for ty in range(2):
                        for g in range(2):
                            if qb >= NCHL:
                                klist = [(NCHL, None), (NCHL + 1, None)]
                            elif ty == 0:
                                klist = [(kc, None) for kc in chunks]
                            else:
                                klist = []
                                if qb - 1 >= 0:
                                    klist.append((qb - 1, 0))
                                klist.append((qb, None))
                                if qb + 1 < NCHL:
                                    klist.append((qb + 1, 1))
                                if has_ctx:
                                    klist += [(NCHL, None), (NCHL + 1, None)]
                            ob = O_banks[octr % 2]
                            octr += 1
                            for ki, (kc, mk) in enumerate(klist):
                                items.append(dict(qi=qi, qb=qb, ty=ty, g=g, kc=kc, mk=mk, first=(ki == 0),
                                                  last=(ki == len(klist) - 1), ob=ob))
                for i, it in enumerate(items):
                    it["sbk"] = S_banks[i % 2]
                    it["pt"] = i % 3

                def emit_S(it):
                    qb, kc, sbk = it["qb"], it["kc"], it["sbk"]
                    gp = it["ty"] * 2 + it["g"]
                    pair0 = it["ty"] * 4 + it["g"] * 2
                    qs = slice(qb * 128, (qb + 1) * 128)
                    ks = slice(kc * 128, (kc + 1) * 128)
                    fns = [lambda e, r=r: e.matmul(
                        PS[sbk][:, r * 256:(r + 1) * 256].rearrange("p (i q) -> p i q", q=128),
                        lhsT=kT[:, gp, r, ks], rhs=hq[:, pair0:pair0 + 2, qs], start=True, stop=True) for r in range(2)]
                    P.do("pe", fns, reads=[kTb[kc], hqb[qb]], writes=[PSb[sbk]])

                def tail_gen(qi, qb):
                    ot, otb = otok[qi % 2], otokb[qi % 2]
                    if dbg and l == 0 and b == 0:
                        P.dma("pool", lambda e: e.dma_start(out=DBGO[:, qb, :], in_=ot[:]), dbg_ds, reads=[otb])
                    transpose8(ot, otb, 128, 4, oT[:], oTb)
                    yield
                    for nb_ in range(2):
                        fns = [lambda e, k=k, nb_=nb_: e.matmul(PS[5 + nb_][:], lhsT=oT[:, k, :], rhs=Wout[:, k, nb_ * 512:(nb_ + 1) * 512],
                                                               start=(k == 0), stop=(k == 7)) for k in range(8)]
                        P.do("pe", fns, reads=[oTb, Woutb], writes=[PSb[5 + nb_]])
                    yield from epilogue_gen(ep, l, b, qb, [5, 6], 4, 7)

                sched = Sched()

                def emit_rest(it):
                    qi, qb, ty, g, kc, mk, ob, sbk = it["qi"], it["qb"], it["ty"], it["g"], it["kc"], it["mk"], it["ob"], it["sbk"]
                    first, last = it["first"], it["last"]
                    gp = ty * 2 + g
                    pt, ptb = PT[it["pt"]], PTb[it["pt"]]
                    ot, otb = otok[qi % 2], otokb[qi % 2]
                    P.do("act", lambda e: e.activation(out=pt[:], in_=PS[sbk][:], func=AF.Exp), reads=[PSb[sbk]], writes=[ptb])
                    if mk is not None:
                        P.do("dve", lambda e: e.tensor_tensor(
                            pt[:].rearrange("p (c q) -> p c q", q=128), pt[:].rearrange("p (c q) -> p c q", q=128),
                            mask_t[:, mk:mk + 1, :].to_broadcast([128, 4, 128]), op=ALU.mult),
                            reads=[const_b, ptb], writes=[ptb])
                    fns = [lambda e, cb=cb: e.matmul(
                        PS[ob][:, cb * 65:(cb + 1) * 65], lhsT=pt[:, cb * 128:(cb + 1) * 128], rhs=vaug[:, kc, gp, :],
                        start=(first and cb == 0), stop=last, skip_group_check=True) for cb in range(4)]
                    if first:
                        P.do("pe", fns, reads=[ptb, vb[kc], ones_b], writes=[PSb[ob]])
                    else:
                        P.use("pe", reads=[ptb, vb[kc]])
                        for fn in fns[:-1]:
                            P.op("pe", fn)
                        ev = P.op("pe", fns[-1], mark=True)
                        P.done(ev, reads=[ptb, vb[kc]], writes=[])
                        PSb[ob].w = ev
                    if not last:
                        return
                    ov = PS[ob][:, 0:260].rearrange("p (c x) -> p c x", x=65)
                    dslice = den[:, g * 4:g * 4 + 4]
                    if ty == 1:
                        P.do("dve", lambda e: e.tensor_tensor(
                            dslice.unsqueeze(2), ov[:, :, 64:65], sk[:, g * 4:(g + 1) * 4].unsqueeze(2), op=ALU.add),
                            reads=[PSb[ob], skb], writes=[denb])
                    else:
                        P.do("dve", lambda e: e.tensor_copy(dslice.unsqueeze(2), ov[:, :, 64:65]), reads=[PSb[ob]], writes=[denb])
                    P.do("dve", lambda e: e.reciprocal(dslice, dslice), writes=[denb])
                    hb = ty * 8 + g * 4
                    outv = ot[:, hb * 64:(hb + 4) * 64].rearrange("p (i r d) -> p r i d", r=2, d=64)
                    inv = ov[:, :, 0:64].rearrange("p (r i) d -> p r i d", i=2)
                    rdv = dslice.rearrange("p (r i) -> p r i", i=2).unsqueeze(3).to_broadcast([128, 2, 2, 64])
                    P.do("dve", lambda e: e.tensor_tensor(outv, inv, rdv, op=ALU.mult), reads=[PSb[ob], denb], writes=[otb])
                    if ty == 1 and g == 1:
                        sched.add(tail_gen(qi, qb))

                emit_S(items[0])
                for i, it in enumerate(items):
                    if i + 1 < len(items):
                        emit_S(items[i + 1])
                    emit_rest(it)
                    if i % 3 == 2:
                        sched.step()
                sched.drain()
            P.flush()

    def mixer_conv(l, b, has_ctx):
        j = l // 2
        chunks = list(range(NCH if has_ctx else NCHL))
        ntok = NT if has_ctx else NL
        with ExitStack() as st0:
            zT = sb("zT", [128, 8, NT], BF16, st0)
            zTb = Buf()
            Wout = sb("Wout", [128, 8, D], BF16, st0)
            Woutb = Buf()
            with ExitStack() as st:
                gsh = load_mixer_mods(st, l, b, has_ctx, 1)
                hT = sb("hT", [128, 8, NT], BF16, st)
                hTb = [Buf() for _ in range(NCH)]
                with ExitStack() as stn:
                    norm1_pass(stn, l, b, chunks, hT, hTb, 0)
                P.barrier()
                load_mixer_mods(st, l, b, has_ctx, 2, gsh)
                wods = P.dsem()
                P.dma("pool", lambda e: e.dma_start(out=Wout[:], in_=conv_w_out[j].rearrange("(k p) n -> p k n", p=128)), wods, writes=[Woutb])
                kb4 = sb("kb4", [4, D], F32, st)
                kk = sb("kk", [128, 8, 4], F32, st)
                kkb = Buf()
                tds = P.dsem()
                P.dma("sp", lambda e: e.dma_start(out=kb4[:], in_=conv_kb[j]), tds, writes=[kkb])
                fns = [lambda e, m=m: e.transpose(PS[1][:, m * 4:(m + 1) * 4], kb4[0:4, m * 128:(m + 1) * 128], ident_f[0:4, 0:4]) for m in range(8)]
                P.do("pe", fns, reads=[kkb, const_b], writes=[PSb[1]])
                P.do("dve", lambda e: e.tensor_copy(kk[:].rearrange("p m j -> p (m j)"), PS[1][:, 0:32]), reads=[PSb[1]], writes=[kkb])
                wc = [sb(f"wc{i}", [128, 8, 3, 128], BF16, st) for i in range(2)]
                wcb = [Buf() for _ in range(2)]
                wcd = [P.dsem() for _ in range(2)]
                UW = NT + 4
                u = sb("u", [128, UW], F32, st)
                ub = Buf()
                bgb_t = sb("bgb", [128, NT], F32, st)
                bgbb = Buf()
                yb_t = sb("yb", [128, NT], F32, st)
                ybb = Buf()
                vsb = [sb(f"vsb{i}", [128, 512], F32, st) for i in range(2)]
                vsbb = [Buf() for _ in range(2)]
                P.do("dve", lambda e: e.memset(u[:], 0.0), writes=[ub])
                src = conv_w_in[j].rearrange("(k p) n -> p k n", p=128)
                tbs = [(t0, 512) for t0 in range(0, NL, 512)]
                if has_ctx:
                    tbs.append((NL, 256))
                uoff = lambda t0: 1 + t0 if t0 < NL else 3 + t0
                vc = 0
                for m in range(8):
                    s = m % 2
                    fns = [lambda e, w=w, m=m, s=s: e.dma_start(out=wc[s][:, :, w, :], in_=src[:, :, w * D + m * 128:w * D + (m + 1) * 128]) for w in range(3)]
                    P.dma("pool", fns, wcd[s], writes=[wcb[s]])
                    for (t0, tw) in tbs:
                        for w in range(3):
                            fns = [lambda e, k=k, w=w, s=s, t0=t0, tw=tw: e.matmul(PS[2 + w][:, 0:tw], lhsT=wc[s][:, k, w, :], rhs=hT[:, k, t0:t0 + tw],
                                                                                  start=(k == 0), stop=(k == 7)) for k in range(8)]
                            P.do("pe", fns, reads=[wcb[s]] + [hTb[cc] for cc in range(t0 // 128, (t0 + tw) // 128)], writes=[PSb[2 + w]])
                        vs_, vsb_ = vsb[vc % 2], vsbb[vc % 2]
                        vc += 1
                        P.do("act", [lambda e, t0=t0, tw=tw: e.activation(out=bgb_t[:, t0:t0 + tw], in_=PS[2][:, 0:tw], func=AF.Copy)],
                             reads=[PSb[2]], writes=[bgbb])
                        P.do("act", [lambda e, vs_=vs_, tw=tw: e.activation(out=vs_[:, 0:tw], in_=PS[4][:, 0:tw], func=AF.Copy)],
                             reads=[PSb[4]], writes=[vsb_])
                        P.do("dve", lambda e, vs_=vs_, t0=t0, tw=tw: e.tensor_tensor(u[:, uoff(t0):uoff(t0) + tw], PS[3][:, 0:tw], vs_[:, 0:tw], op=ALU.mult),
                             reads=[PSb[3], vsb_], writes=[ub])
                    segs = [(0, NL)] + ([(NL, NCTX)] if has_ctx else [])
                    for (t0, tw) in segs:
                        o = uoff(t0)
                        P.do("act", lambda e, m=m, t0=t0, tw=tw, o=o: e.activation(out=yb_t[:, t0:t0 + tw], in_=u[:, o:o + tw], func=AF.Identity,
                                                                               scale=kk[:, m, 1:2], bias=kk[:, m, 3:4]),
                             reads=[ub, kkb], writes=[ybb])
                        P.do("dve", lambda e, m=m, t0=t0, tw=tw, o=o: e.scalar_tensor_tensor(
                            out=yb_t[:, t0:t0 + tw], in0=u[:, o - 1:o - 1 + tw], scalar=kk[:, m, 0:1],
                            in1=yb_t[:, t0:t0 + tw], op0=ALU.mult, op1=ALU.add), reads=[ub, ybb, kkb], writes=[ybb])
                        P.do("dve", lambda e, m=m, t0=t0, tw=tw, o=o: e.scalar_tensor_tensor(
                            out=yb_t[:, t0:t0 + tw], in0=u[:, o + 1:o + 1 + tw], scalar=kk[:, m, 2:3],
                            in1=yb_t[:, t0:t0 + tw], op0=ALU.mult, op1=ALU.add), reads=[ub, ybb, kkb], writes=[ybb])
                        P.do("dve", lambda e, m=m, t0=t0, tw=tw: e.tensor_tensor(
                            zT[:, m, t0:t0 + tw], bgb_t[:, t0:t0 + tw], yb_t[:, t0:t0 + tw], op=ALU.mult),
                            reads=[ybb, bgbb], writes=[zTb])
            P.flush()
            with ExitStack() as st:
                ep = make_epi(st, l, b, chunks)
                yctr = 0
                sched = Sched()
                for c in chunks:
                    cs_ = slice(c * 128, (c + 1) * 128)
                    yb0 = 0 + 2 * (yctr % 2)
                    yctr += 1
                    for nb_ in range(2):
                        fns = [lambda e, k=k, nb_=nb_, cs_=cs_, yb0=yb0: e.matmul(PS[yb0 + nb_][:], lhsT=zT[:, k, cs_], rhs=Wout[:, k, nb_ * 512:(nb_ + 1) * 512],
                                                                                start=(k == 0), stop=(k == 7)) for k in range(8)]
                        P.do("pe", fns, reads=[zTb, Woutb], writes=[PSb[yb0 + nb_]])
                    sched.add(epilogue_gen(ep, l, b, c, [yb0, yb0 + 1], 4, 7))
                    sched.step()
                sched.drain()
            P.flush()

    def moe(l, has_ctx):
        pieces = [(0, 128), (128, 128)] + ([(256, 32)] if has_ctx else [])
        with ExitStack() as st0:
            VALS = sb("VALS", [128, 3, 48], F32, st0)
            IDX = sb("IDX", [128, 3, 48], I32, st0)
            rb = Buf(small=True)
            NS = 2
            Wg = [sb(f"Wg{i}", [128, 8, D], BF16, st0) for i in range(NS)]
            Wu = [sb(f"Wu{i}", [128, 8, D], BF16, st0) for i in range(NS)]
            Wd = [sb(f"Wd{i}", [128, 8, D], BF16, st0) for i in range(NS)]
            Wb = [[Buf() for _ in range(3)] for _ in range(NS)]
            Wds = [[P.dsem(f"wds{i}_{k}") for k in range(3)] for i in range(NS)] if not hasattr(P, "_wds") else P._wds
            P._wds = Wds

            def load_w(e_):
                s = e_ % NS
                for wi, (dst, srcw) in enumerate(((Wg[s], w_gate), (Wu[s], w_up), (Wd[s], w_down))):
                    P.dma("pool", lambda e, dst=dst, srcw=srcw, e_=e_: e.dma_start(out=dst[:], in_=srcw[l, e_].rearrange("(k p) f -> p k f", p=128)),
                          Wds[s][wi], writes=[Wb[s][wi]])

            load_w(0)
            with ExitStack() as st:
                wk = sb("wk", [48, NL], F32, st)
                wkc = sb("wkc", [48, NCTX], F32, st)
                vals = sb("vals", [48, CAP_L + CAP_C], F32, st)
                idxu = sb("idxu", [48, CAP_L + CAP_C], U32, st)
                idxf = sb("idxf", [48, CAP_L + CAP_C], F32, st)
                tb_ = Buf(small=True)
                fns = [lambda e: e.tensor_copy(wk[:], affT[:, 0:NL])]
                for it in range(CAP_L // 8):
                    sl = slice(it * 8, (it + 1) * 8)
                    fns.append(lambda e, sl=sl: e.max(out=vals[:, sl], in_=wk[:]))
                    fns.append(lambda e, sl=sl: e.max_index(out=idxu[:, sl], in_max=vals[:, sl], in_values=wk[:]))
                    fns.append(lambda e, sl=sl: e.match_replace(out=wk[:], in_to_replace=vals[:, sl], in_values=wk[:], imm_value=-1.0))
                if has_ctx:
                    fns.append(lambda e: e.tensor_copy(wkc[:], affT[:, NL:NT]))
                    for it in range(CAP_C // 8):
                        sl = slice(CAP_L + it * 8, CAP_L + (it + 1) * 8)
                        fns.append(lambda e, sl=sl: e.max(out=vals[:, sl], in_=wkc[:]))
                        fns.append(lambda e, sl=sl: e.max_index(out=idxu[:, sl], in_max=vals[:, sl], in_values=wkc[:]))
                        fns.append(lambda e, sl=sl: e.match_replace(out=wkc[:], in_to_replace=vals[:, sl], in_values=wkc[:], imm_value=-1.0))
                fns.append(lambda e: e.tensor_copy(idxf[:], idxu[:]))
                if has_ctx:
                    fns.append(lambda e: e.tensor_scalar(idxf[:, CAP_L:CAP_L + CAP_C], idxf[:, CAP_L:CAP_L + CAP_C], float(NL), None, op0=ALU.add))
                P.use("dve", reads=[affTb])
                for fn in fns:
                    P.do("dve", fn, writes=[tb_])
                for pi, (so, rows) in enumerate(pieces):
                    P.do("pe", [lambda e, so=so, rows=rows: e.transpose(PS[0][0:rows, 0:48], vals[:, so:so + rows], ident_f[0:48, 0:48]),
                                lambda e, so=so, rows=rows: e.transpose(PS[0][0:rows, 64:112], idxf[:, so:so + rows], ident_f[0:48, 0:48])],
                         reads=[tb_, const_b], writes=[PSb[0]])
                    P.do("dve", [lambda e, pi=pi, rows=rows: e.tensor_copy(VALS[0:rows, pi, :], PS[0][0:rows, 0:48]),
                                 lambda e, pi=pi, rows=rows: e.tensor_copy(IDX[0:rows, pi, :], PS[0][0:rows, 64:112])],
                         reads=[PSb[0]], writes=[rb])
            P.flush()
            with ExitStack() as st:
                load_mod(0, l, 0, 5)
                load_mod(1, l, 1, 5)
                if has_ctx:
                    load_mod(3, l, 2, 5)
                xe = [[[sb(f"xe{b}_{pi}", [128, D], BF16, st) for pi in range(len(pieces))] for b in range(NB)] for _ in range(2)]
                xeb = [[[Buf() for _ in pieces] for b in range(NB)] for _ in range(2)]
                xeds = [[P.dsem() for b in range(NB)] for _ in range(2)]
                xeT = sb("xeT", [128, 8, 2 * 288], BF16, st)
                xeTb = [[Buf() for _ in pieces] for b in range(NB)]
                hidT = sb("hidT", [128, 8, 2 * 288], BF16, st)
                hidTb = Buf()
                sg = [sb(f"sg{i}", [128, 288], F32, st) for i in range(2)]
                sgb = [Buf() for _ in range(2)]
                ye = [sb(f"ye{i}", [128, D], F32, st) for i in range(3)]
                yeb = [Buf() for _ in range(3)]
                sc_ds = [P.dsem() for _ in range(NB)]
                ncols = 256 + (32 if has_ctx else 0)
                yectr = 0
                tctr = 0

                def issue_gathers(e_):
                    xs_ = e_ % 2
                    for b in range(NB):
                        col = b * 32 + e_
                        fns = [lambda e, b=b, pi=pi, rows=rows, col=col: e.indirect_dma_start(
                            out=xe[xs_][b][pi][0:rows, :], out_offset=None, in_=H[b],
                            in_offset=bass.IndirectOffsetOnAxis(ap=IDX[0:rows, pi, col:col + 1], axis=0))
                            for pi, (so, rows) in enumerate(pieces)]
                        P.dma("pool", fns, xeds[xs_][b], reads=[rb] + Hc[b], writes=xeb[xs_][b])

                def do_transposes(e_, b):
                    nonlocal tctr
                    xs_ = e_ % 2
                    for pi, (so, rows) in enumerate(pieces):
                        tbk = tctr % 2
                        tctr += 1
                        c0 = b * 288 + so
                        transpose8(xe[xs_][b][pi], xeb[xs_][b][pi], rows, tbk, xeT[:, :, c0:c0 + rows], xeTb[b][pi])

                issue_gathers(0)
                for e_ in range(NE):
                    s = e_ % NS
                    if e_ + 1 < NE:
                        load_w(e_ + 1)
                        issue_gathers(e_ + 1)
                    do_transposes(e_, 0)
                    for m in range(8):
                        ms = slice(m * 128, (m + 1) * 128)
                        for b in range(NB):
                            c0 = b * 288
                            if m == 0 and b == 1:
                                do_transposes(e_, 1)
                            for wi, W in enumerate((Wg[s], Wu[s])):
                                bank = 2 + b * 2 + wi
                                fns = [lambda e, k=k, W=W, bank=bank, c0=c0, ms=ms: e.matmul(PS[bank][:, 0:ncols], lhsT=W[:, k, ms], rhs=xeT[:, k, c0:c0 + ncols],
                                                                                           start=(k == 0), stop=(k == 7)) for k in range(8)]
                                P.do("pe", fns, reads=[Wb[s][wi]] + xeTb[b], writes=[PSb[bank]])
                            gb_, ub_ = 2 + b * 2, 3 + b * 2
                            P.do("act", lambda e, b=b, gb_=gb_: e.activation(out=sg[b][:, 0:ncols], in_=PS[gb_][:, 0:ncols], func=AF.Silu),
                                 reads=[PSb[gb_]], writes=[sgb[b]])
                            P.do("dve", lambda e, b=b, ub_=ub_, m=m, c0=c0: e.tensor_tensor(hidT[:, m, c0:c0 + ncols], sg[b][:, 0:ncols], PS[ub_][:, 0:ncols], op=ALU.mult),
                                 reads=[sgb[b], PSb[ub_]], writes=[hidTb])
                    for b in range(NB):
                        col = b * 32 + e_
                        scat = []
                        yused = []
                        for pi, (so, rows) in enumerate(pieces):
                            c0 = b * 288 + so
                            yi = yectr % 3
                            yectr += 1
                            Gt, Gtb = (modt[3], modb[3]) if pi == 2 else (modt[b], modb[b])
                            for nb_ in range(2):
                                bank = 6 + nb_
                                fns = [lambda e, k=k, bank=bank, c0=c0, rows=rows, nb_=nb_, s=s: e.matmul(PS[bank][0:rows, :], lhsT=hidT[:, k, c0:c0 + rows],
                                                                                                  rhs=Wd[s][:, k, nb_ * 512:(nb_ + 1) * 512],
                                                                                                  start=(k == 0), stop=(k == 7)) for k in range(8)]
                                P.do("pe", fns, reads=[hidTb, Wb[s][2]], writes=[PSb[bank]])
                                P.do("dve", lambda e, yi=yi, rows=rows, nb_=nb_, bank=bank, pi=pi, col=col, Gt=Gt: e.scalar_tensor_tensor(
                                    out=ye[yi][0:rows, nb_ * 512:(nb_ + 1) * 512], in0=PS[bank][0:rows, :], scalar=VALS[0:rows, pi, col:col + 1],
                                    in1=Gt[0:rows, nb_ * 512:(nb_ + 1) * 512], op0=ALU.mult, op1=ALU.mult),
                                    reads=[PSb[bank], rb, Gtb], writes=[yeb[yi]])
                            yused.append(yeb[yi])
                            scat.append(lambda e, b=b, yi=yi, rows=rows, pi=pi, col=col: e.indirect_dma_start(
                                out=X[b], out_offset=bass.IndirectOffsetOnAxis(ap=IDX[0:rows, pi, col:col + 1], axis=0),
                                in_=ye[yi][0:rows, :], in_offset=None, compute_op=ALU.add))
                        P.dma("pool", scat, sc_ds[b], reads=yused + [rb], writes=Xc[b])
            P.flush()

    def final():
        ds = P.dsem("final")
        fns = []
        for b in range(NB):
            for r0 in range(0, NL, 256):
                fns.append(lambda e, b=b, r0=r0: e.dma_start(out=out[b, r0:r0 + 256, :], in_=X[b][r0:r0 + 256, :]))
        ev = P.dma("sp", fns, ds, reads=[c for b in range(NB) for c in Xc[b]])
        P.engs["sp"].q.append(("w", ev[0], ev[1]))
        P.flush()

    phase0()
    done_ = False
    for l in range(n_layers):
        has_ctx = l < DEPTH - 1
        with ExitStack() as stl:
            if l % 2 == 0:
                Win_, Winb_ = load_win(stl, l)
            for b in range(NB):
                if l % 2 == 0:
                    mixer_attn(l, b, has_ctx, Win_, Winb_)
                else:
                    mixer_conv(l, b, has_ctx)
        if stop == (l, "mixer"):
            break
        moe(l, has_ctx)
        if stop == (l, "moe"):
            break
    final()
    es.close()
    return nc


def _host_consts():
    rows = NL // 64
    row = np.repeat(np.arange(rows, dtype=np.float32), 64)
    col = np.tile(np.arange(64, dtype=np.float32), rows)
    inv_freq = (10000.0 ** (-np.arange(0, 32, 2, dtype=np.float32) / 32.0)).astype(np.float32)
    ang_r = row[:, None] * inv_freq[None, :]
    ang_c = col[:, None] * inv_freq[None, :]
    rope = np.concatenate([np.cos(ang_r), np.cos(ang_c), np.sin(ang_r), np.sin(ang_c)], axis=1).astype(np.float32)
    ident = np.eye(128, dtype=np.float32)
    kk = np.arange(128)[:, None]
    qq = np.arange(128)[None, :]
    masks = np.stack([(kk >= qq), (kk <= qq)]).astype(np.float32)
    return rope, ident, masks


_NC_CACHE = {}
_LAST = None


def kernel(x, c, ctx, c_ctx, ada_w, ada_b, norm1_g, norm2_g, attn_w_in, attn_w_out,
           qnorm_a, knorm_a, qnorm_b, knorm_b, sink_b, conv_w_in, conv_k, conv_b, conv_w_out,
           router_w, moe_w_gate, moe_w_up, moe_w_down, _n_layers=DEPTH, _stop=None, _cores=8):
    f = lambda a: np.ascontiguousarray(np.asarray(a, dtype=np.float32))
    rope, ident, masks = _host_consts()
    qk_gain = np.stack([f(qnorm_a), f(qnorm_b), f(knorm_a), f(knorm_b)], axis=1)
    conv_kb = np.concatenate([f(conv_k), f(conv_b)[:, None, :]], axis=1)
    shared = {
        "ada_w": f(ada_w), "ada_b": f(ada_b), "norm1_g": f(norm1_g), "norm2_g": f(norm2_g),
        "attn_w_in": f(attn_w_in), "attn_w_out": f(attn_w_out), "qk_gain": np.ascontiguousarray(qk_gain),
        "sink_b": f(sink_b), "conv_w_in": f(conv_w_in), "conv_kb": np.ascontiguousarray(conv_kb),
        "conv_w_out": f(conv_w_out), "router_w": f(router_w), "moe_w_gate": f(moe_w_gate),
        "moe_w_up": f(moe_w_up), "moe_w_down": f(moe_w_down), "rope": rope, "ident": ident, "masks": masks,
    }
    x = f(x)
    ctx = f(ctx)
    c = f(c)
    c_ctx = f(c_ctx)
    key = (_n_layers, _stop)
    if key not in _NC_CACHE:
        _NC_CACHE[key] = build_program(_n_layers, _stop)
    nc = _NC_CACHE[key]
    in_maps = []
    for i in range(_cores):
        m = dict(shared)
        m["x"] = x[NB * i:NB * (i + 1)]
        m["ctx"] = ctx[NB * i:NB * (i + 1)]
        m["cvec"] = np.ascontiguousarray(np.concatenate([c[NB * i:NB * (i + 1)], c_ctx[None, :]], axis=0))
        in_maps.append(m)
    res = run_bass_kernel_spmd(nc, in_maps, core_ids=list(range(_cores)))
    global _LAST
    _LAST = res.results
    return np.concatenate([np.asarray(r["out"]) for r in res.results], axis=0).astype(np.float32)
```

```python
import numpy as np
from contextlib import ExitStack
import concourse.bass as bass
import concourse.mybir as mybir
from concourse.bass_utils import run_bass_kernel_spmd

F32 = mybir.dt.float32
BF16 = mybir.dt.bfloat16
I32 = mybir.dt.int32
U32 = mybir.dt.uint32
ALU = mybir.AluOpType
AF = mybir.ActivationFunctionType
AX = mybir.AxisListType

D = 1024
NL = 2048
NCTX = 256
NT = NL + NCTX
NCH = NT // 128
NCHL = NL // 128
DEPTH = 4
NE = 16
CAP_L = 256
CAP_C = 32
EPS = 1e-6
NB = 2

DEBUG_STOP = None
SBDBG = False


class Eng:
    def __init__(self, name, sems):
        self.name = name
        self.sems = sems
        self.cur = 0
        self.count = 0
        self.q = []
        self.waited = {}

    def mark(self):
        if self.count >= 30000:
            self.cur += 1
            self.count = 0
        self.count += 1
        return (self.sems[self.cur], self.count)


class DmaSem:
    def __init__(self, sem):
        self.sem = sem
        self.count = 0


class Buf:
    __slots__ = ("name", "w", "r", "small")

    def __init__(self, name="", small=False):
        self.name = name
        self.w = None
        self.r = {}
        self.small = small


class Planner:
    def __init__(self, nc, es):
        self.nc = nc
        mk = lambda n: es.enter_context(nc.semaphore(n))
        self.engs = {
            "pe": Eng("pe", [mk(f"pe{i}") for i in range(6)]),
            "act": Eng("act", [mk(f"act{i}") for i in range(3)]),
            "dve": Eng("dve", [mk(f"dve{i}") for i in range(3)]),
            "pool": Eng("pool", [mk(f"pool{i}") for i in range(2)]),
            "sp": Eng("sp", [mk("sp0")]),
        }
        self.owner = {}
        for e in self.engs.values():
            for s in e.sems:
                self.owner[s.num] = e.name
        self.es = es
        self.n_dsem = 0
        self.free_ds = []
        self.phase_ds = []

    def dsem(self, name=None):
        if name is None and self.free_ds:
            d = self.free_ds.pop()
            self.phase_ds.append(d)
            return d
        self.n_dsem += 1
        d = DmaSem(self.es.enter_context(self.nc.semaphore(name or f"d{self.n_dsem}")))
        if name is None:
            self.phase_ds.append(d)
        return d

    def wait(self, eng, ev, force=False):
        if ev is None:
            return
        sem, val = ev
        e = self.engs[eng]
        if self.owner.get(sem.num) == eng and not (force and eng in ("act", "dve", "pool")):
            return
        if e.waited.get(sem.num, 0) >= val:
            return
        e.waited[sem.num] = val
        e.q.append(("w", sem, val))

    def op(self, eng, fn, mark=False):
        e = self.engs[eng]
        if mark:
            ev = e.mark()
            e.q.append(("o", fn, ev[0], 1))
            return ev
        e.q.append(("o", fn, None, 0))
        return None

    def use(self, eng, reads=(), writes=()):
        for b in reads:
            self.wait(eng, b.w, True)
        for b in writes:
            self.wait(eng, b.w, True)
            for num, (sem, val) in list(b.r.items()):
                self.wait(eng, (sem, val), b.small)

    @staticmethod
    def done(ev, reads=(), writes=()):
        for b in reads:
            old = b.r.get(ev[0].num)
            if old is None or old[1] < ev[1]:
                b.r[ev[0].num] = ev
        for b in writes:
            b.w = ev
            b.r = {}

    def do(self, eng, fns, reads=(), writes=()):
        if not isinstance(fns, (list, tuple)):
            fns = [fns]
        self.use(eng, reads, writes)
        for fn in fns[:-1]:
            self.op(eng, fn)
        ev = self.op(eng, fns[-1], mark=True)
        self.done(ev, reads, writes)
        return ev

    def dma(self, eng, fns, ds, reads=(), writes=()):
        if not isinstance(fns, (list, tuple)):
            fns = [fns]
        self.use(eng, reads, writes)
        e = self.engs[eng]
        for fn in fns:
            ds.count += 16
            e.q.append(("o", fn, ds.sem, 16))
        ev = (ds.sem, ds.count)
        self.done(ev, reads, writes)
        return ev

    def barrier(self):
        for en in ("sp", "pool", "act", "dve", "pe"):
            for cn in ("pe", "act", "dve"):
                c = self.engs[cn]
                if cn != en and c.count > 0:
                    self.wait(en, (c.sems[c.cur], c.count))

    def flush(self):
        nc = self.nc
        self.barrier()
        for d in self.phase_ds:
            if d.count > 0:
                self.wait("sp", (d.sem, d.count))
                self.wait("pool", (d.sem, d.count))
        self.free_ds.extend(self.phase_ds)
        self.phase_ds = []
        qs = {k: e.q for k, e in self.engs.items()}
        for e in self.engs.values():
            e.q = []

        def replay(q, eng):
            for ent in q:
                if ent[0] == "w":
                    eng.wait_ge(ent[1], ent[2])
                else:
                    ins = ent[1](eng)
                    if ent[2] is not None:
                        ins.then_inc(ent[2], ent[3])

        with nc.Block() as blk:
            if qs["pe"]:
                @blk.tensor
                def _(e):
                    replay(qs["pe"], e)
            if qs["act"]:
                @blk.scalar
                def _(e):
                    replay(qs["act"], e)
            if qs["dve"]:
                @blk.vector
                def _(e):
                    replay(qs["dve"], e)
            if qs["pool"]:
                @blk.gpsimd
                def _(e):
                    replay(qs["pool"], e)
            if qs["sp"]:
                @blk.sync
                def _(e):
                    replay(qs["sp"], e)


def build_program(n_layers=DEPTH, stop=None):
    nc = bass.Bass("TRN2", target_bir_lowering=False)
    es = ExitStack()
    P = Planner(nc, es)

    def din(name, shape, dt=F32):
        return nc.dram_tensor(name, shape, dt, kind="ExternalInput").ap()

    x_in = din("x", [NB, NL, D])
    ctx_in = din("ctx", [NB, NCTX, D])
    cvec = din("cvec", [3, D])
    ada_w = din("ada_w", [DEPTH, D, 6 * D])
    ada_b = din("ada_b", [DEPTH, 6 * D])
    norm1_g = din("norm1_g", [DEPTH, D])
    norm2_g = din("norm2_g", [DEPTH, D])
    attn_w_in = din("attn_w_in", [2, D, 1536])
    attn_w_out = din("attn_w_out", [2, D, D])
    qk_gain = din("qk_gain", [2, 4, 64])
    sink_b = din("sink_b", [2, 8])
    conv_w_in = din("conv_w_in", [2, D, 3 * D])
    conv_kb = din("conv_kb", [2, 4, D])
    conv_w_out = din("conv_w_out", [2, D, D])
    router_w = din("router_w", [DEPTH, D, NE])
    w_gate = din("moe_w_gate", [DEPTH, NE, D, D])
    w_up = din("moe_w_up", [DEPTH, NE, D, D])
    w_down = din("moe_w_down", [DEPTH, NE, D, D])
    rope_in = din("rope", [NL, 64])
    ident_in = din("ident", [128, 128])
    masks_in = din("masks", [2, 128, 128])
    out = nc.dram_tensor("out", [NB, NL, D], F32, kind="ExternalOutput").ap()

    dbg = bool(stop) or n_layers < DEPTH
    kindI = "ExternalOutput" if dbg else "Internal"
    X = [nc.dram_tensor(f"Xs{b}", [NT, D], F32, kind=kindI).ap() for b in range(NB)]
    H = [nc.dram_tensor(f"Hs{b}", [NT, D], BF16, kind=kindI).ap() for b in range(NB)]
    MOD = nc.dram_tensor("MODs", [DEPTH, 3, 6 * D], F32, kind=kindI).ap()
    DBG = nc.dram_tensor("DBG", [128, 4096], F32, kind="ExternalOutput").ap() if dbg else None
    DBGQ = nc.dram_tensor("DBGQ", [128, 8, NT], F32, kind="ExternalOutput").ap() if dbg else None
    DBGK = nc.dram_tensor("DBGK", [128, 4, 2, NT], F32, kind="ExternalOutput").ap() if dbg else None
    DBGV = nc.dram_tensor("DBGV", [128, NCH, 4, 65], F32, kind="ExternalOutput").ap() if dbg else None
    DBGO = nc.dram_tensor("DBGO", [128, NCH, D], F32, kind="ExternalOutput").ap() if dbg else None

    Xc = [[Buf(f"X{b}_{c}") for c in range(NCH)] for b in range(NB)]
    Hc = [[Buf(f"H{b}_{c}") for c in range(NCH)] for b in range(NB)]
    MODb = Buf("MOD")

    _uid = [0]

    def sb(name, shape, dt, st=es):
        _uid[0] += 1
        if SBDBG:
            print("SB", name, shape, dt, "remaining", nc.sbuf_bytes_remaining)
        return st.enter_context(nc.sbuf_tensor(f"{name}_{_uid[0]}", shape, dt))

    PS = [es.enter_context(nc.psum_tensor(f"ps{i}", [128, 512], F32)) for i in range(8)]
    PSb = [Buf(f"ps{i}") for i in range(8)]

    ident_f = sb("ident_f", [128, 128], F32)
    ident_b = sb("ident_b", [128, 128], BF16)
    mask_t = sb("mask_t", [128, 2, 128], BF16)
    mask_f = sb("mask_f", [128, 2, 128], F32)
    rope_t = sb("rope_t", [128, NCHL, 64], F32)
    xt = [sb(f"xt{i}", [128, D], F32) for i in range(2)]
    xtb = [Buf(f"xt{i}") for i in range(2)]
    xt_ds = [P.dsem(f"xtd{i}") for i in range(2)]
    modt = [sb(f"modt{i}", [128, D], F32) for i in range(6)]
    modb = [Buf(f"modt{i}") for i in range(6)]
    mod_ds = [P.dsem(f"modd{i}") for i in range(6)]
    affT = sb("affT", [48, NT], F32)
    affTb = Buf("affT")
    eps_t = sb("eps_t", [128, 1], F32)
    const_b = Buf("consts")
    setup_ds = P.dsem("setup")
    setup2_ds = P.dsem("setup2")

    def phase0():
        with ExitStack() as st:
            cs = sb("cs", [3, D], F32, st)
            scs = sb("scs", [3, D], F32, st)
            scT = sb("scT", [128, 8, 3], F32, st)
            adab = sb("adab", [3, 6 * D], F32, st)
            modrow = sb("modrow", [3, 6 * D], F32, st)
            wsl = [sb(f"adaw{i}", [128, 8, 512], F32, st) for i in range(2)]
            wslb = [Buf() for _ in range(2)]
            wds = [P.dsem() for i in range(2)]
            csb, adabb, modrowb, scTb = Buf(), Buf(), Buf(), Buf()
            ds2 = P.dsem()
            store_ds = P.dsem("p0store")

            fns = []
            for b in range(NB):
                for r0 in range(0, NL, 256):
                    fns.append(lambda e, b=b, r0=r0: e.dma_start(out=X[b][r0:r0 + 256, :], in_=x_in[b, r0:r0 + 256, :]))
                fns.append(lambda e, b=b: e.dma_start(out=X[b][NL:NT, :], in_=ctx_in[b, :, :]))
            P.dma("sp", fns, setup_ds, writes=[c for b in range(NB) for c in Xc[b]])
            fns = [
                lambda e: e.dma_start(out=ident_f[:], in_=ident_in),
                lambda e: e.dma_start(out=mask_f[:], in_=masks_in.rearrange("m k q -> k m q")),
                lambda e: e.dma_start(out=rope_t[:], in_=rope_in.rearrange("(c p) f -> p c f", p=128)),
                lambda e: e.dma_start(out=cs[:], in_=cvec),
            ]
            P.dma("sp", fns, setup2_ds, writes=[const_b, csb])
            P.do("dve", [lambda e: e.tensor_copy(ident_b[:], ident_f[:]),
                         lambda e: e.tensor_copy(mask_t[:], mask_f[:]),
                         lambda e: e.memset(eps_t[:], EPS),
                         lambda e: e.memset(affT[:], 0.0)], reads=[], writes=[const_b, affTb])
            scsb = Buf()
            P.do("act", lambda e: e.activation(out=scs[:], in_=cs[:], func=AF.Silu), reads=[csb], writes=[scsb])
            fns = []
            for k in range(8):
                fns.append(lambda e, k=k: e.transpose(PS[0][:, k * 3:(k + 1) * 3], scs[0:3, k * 128:(k + 1) * 128], ident_f[0:3, 0:3]))
            P.do("pe", fns, reads=[scsb, const_b], writes=[PSb[0]])
            P.do("dve", lambda e: e.tensor_copy(scT[:].rearrange("p k j -> p (k j)"), PS[0][:, 0:24]), reads=[PSb[0]], writes=[scTb])

            nblk = 12
            it = 0
            for l in range(n_layers):
                P.dma("sp", lambda e, l=l: e.dma_start(out=adab[:], in_=ada_b[l].partition_broadcast(3)), ds2, writes=[adabb])
                for nb in range(nblk):
                    s = it % 2
                    it += 1
                    P.dma("sp", lambda e, l=l, nb=nb, s=s: e.dma_start(
                        out=wsl[s][:], in_=ada_w[l].rearrange("(k p) n -> p k n", p=128)[:, :, nb * 512:(nb + 1) * 512]),
                        wds[s], writes=[wslb[s]])
                    pb = 1 + (it % 2)
                    fns = []
                    for k in range(8):
                        fns.append(lambda e, k=k, s=s, pb=pb: e.matmul(PS[pb][0:3, :], lhsT=scT[:, k, :], rhs=wsl[s][:, k, :],
                                                                      start=(k == 0), stop=(k == 7)))
                    P.do("pe", fns, reads=[wslb[s], scTb], writes=[PSb[pb]])
                    P.do("dve", lambda e, nb=nb, pb=pb: e.tensor_tensor(modrow[:, nb * 512:(nb + 1) * 512], PS[pb][0:3, :],
                                                                      adab[:, nb * 512:(nb + 1) * 512], op=ALU.add),
                         reads=[PSb[pb], adabb], writes=[modrowb])
                P.dma("sp", lambda e, l=l: e.dma_start(out=MOD[l], in_=modrow[:]), store_ds, reads=[modrowb], writes=[MODb])
        P.flush()

    dbg_ds = P.dsem("dbg") if dbg else None

    def dump(src_ap, bufs, col0, ncols, rows=128):
        if not dbg:
            return
        P.dma("pool", lambda e: e.dma_start(out=DBG[0:rows, col0:col0 + ncols], in_=src_ap), dbg_ds, reads=bufs)

    def load_mod(slot, l, j, idx):
        return P.dma("sp", lambda e: e.dma_start(out=modt[slot][:], in_=MOD[l, j, idx * D:(idx + 1) * D].partition_broadcast(128)),
                     mod_ds[slot], reads=[MODb], writes=[modb[slot]])

    def load_scale(slot, l, j, idx, gsrc, gtile, gtb, gds):
        load_mod(slot, l, j, idx)
        P.dma("sp", lambda e: e.dma_start(out=gtile[:], in_=gsrc[l].partition_broadcast(128)), gds, writes=[gtb])
        P.do("dve", lambda e: e.scalar_tensor_tensor(out=modt[slot][:], in0=modt[slot][:], scalar=1.0, in1=gtile[:],
                                                     op0=ALU.add, op1=ALU.mult), reads=[gtb], writes=[modb[slot]])

    class NormCtx:
        def __init__(self, st, tag):
            self.junk = sb(f"junk{tag}", [128, D], BF16, st)
            self.ss = sb(f"ss{tag}", [128, 8], F32, st)
            self.tmp = sb(f"ntmp{tag}", [128, D], F32, st)
            self.sb_ = [Buf(small=True), Buf(small=True)]
            self.tb = Buf()
            self.i = 0
            P.do("dve", lambda e: e.memset(self.ss[:], 0.0), writes=self.sb_)

    def rms_mod(nctx, src, srcb, A, Ab, Bt, Bb, dst, dstb):
        p = nctx.i % 2
        nctx.i += 1
        ss = nctx.ss[:, 4 * p:4 * p + 4]
        ssb = nctx.sb_[p]
        P.do("act", lambda e: e.activation(out=nctx.junk[:], in_=src[:], func=AF.Square, accum_out=ss[:, 0:1]),
             reads=[srcb, const_b], writes=[ssb])
        P.do("act", lambda e: e.activation(out=ss[:, 1:2], in_=ss[:, 0:1], func=AF.Ln, bias=eps_t[:, 0:1], scale=1.0 / D),
             reads=[const_b], writes=[ssb])
        P.do("act", lambda e: e.activation(out=ss[:, 2:3], in_=ss[:, 1:2], func=AF.Exp, scale=-0.5), writes=[ssb])
        P.do("dve", lambda e: e.scalar_tensor_tensor(out=nctx.tmp[:], in0=src[:], scalar=ss[:, 2:3], in1=A[:],
                                                     op0=ALU.mult, op1=ALU.mult),
             reads=[srcb, Ab, ssb], writes=[nctx.tb])
        P.do("dve", lambda e: e.tensor_tensor(dst[:], nctx.tmp[:], Bt[:], op=ALU.add),
             reads=[Bb, nctx.tb], writes=[dstb])
        P.do("dve", lambda e: e.memset(ss[:, 0:1], 0.0), writes=[ssb])

    def transpose8(src, srcb, rows, bank, dstap, dstb, evac="act"):
        pv = PS[bank][:].bitcast(BF16)
        fns = []
        for k in range(8):
            fns.append(lambda e, k=k: e.transpose(pv[:, k * rows:(k + 1) * rows], src[0:rows, k * 128:(k + 1) * 128],
                                                  ident_b[0:rows, 0:rows]))
        P.do("pe", fns, reads=[srcb, const_b], writes=[PSb[bank]])
        inap = pv[:, 0:8 * rows].rearrange("p (k r) -> p k r", r=rows)
        if evac == "act":
            P.do("act", lambda e: e.activation(out=dstap, in_=inap, func=AF.Copy), reads=[PSb[bank]], writes=[dstb])
        else:
            P.do("dve", lambda e: e.tensor_copy(dstap, inap), reads=[PSb[bank]], writes=[dstb])

    class Epi:
        pass

    def make_epi(st, l, b, chunks):
        ep = Epi()
        ep.xn = [sb(f"xn{i}", [128, D], F32, st) for i in range(2)]
        ep.xnb = [Buf() for _ in range(2)]
        ep.xn_ds = [P.dsem() for _ in range(2)]
        ep.h2 = [sb(f"h2_{i}", [128, D], BF16, st) for i in range(2)]
        ep.h2b = [Buf() for _ in range(2)]
        ep.h2_ds = [P.dsem() for _ in range(2)]
        ep.h2T = sb("h2T", [128, 8, 128], BF16, st)
        ep.h2Tb = Buf()
        ep.nctx = NormCtx(st, "e")
        ep.wr = sb("wr", [128, 8, NE], BF16, st)
        ep.wrb = Buf()
        ep.sm = sb("sm", [128, 8], F32, st)
        ep.smb = Buf(small=True)
        ep.ex = sb("ex", [128, NE], F32, st)
        ep.aff = sb("aff48", [128, 48], F32, st)
        ep.affb = Buf(small=True)
        ep.ds = P.dsem()
        ep.cnt = 0
        P.dma("pool", lambda e: e.dma_start(out=ep.wr[:], in_=router_w[l].rearrange("(k p) n -> p k n", p=128)),
              ep.ds, writes=[ep.wrb])
        P.do("dve", lambda e: e.memset(ep.aff[:], 0.0), writes=[ep.affb])
        return ep

    def epilogue_gen(ep, l, b, c, ybanks, T_bank, L_bank):
        isctx = c >= NCHL
        G = modt[5] if isctx else modt[2]
        Gb = modb[5] if isctx else modb[2]
        A2 = modt[3] if isctx else modt[0]
        A2b = modb[3] if isctx else modb[0]
        B2 = modt[4] if isctx else modt[1]
        B2b = modb[4] if isctx else modb[1]
        s = ep.cnt % 2
        ep.cnt += 1
        xs = xt[s]
        P.dma("sp", lambda e: e.dma_start(out=xs[:], in_=X[b][c * 128:(c + 1) * 128, :]), xt_ds[s],
              reads=[Xc[b][c]], writes=[xtb[s]])
        xn, xnb = ep.xn[s], ep.xnb[s]
        P.do("dve", [lambda e: e.tensor_tensor(xn[:, 0:512], PS[ybanks[0]][:], G[:, 0:512], op=ALU.mult),
                     lambda e: e.tensor_tensor(xn[:, 512:1024], PS[ybanks[1]][:], G[:, 512:1024], op=ALU.mult)],
             reads=[PSb[ybanks[0]], PSb[ybanks[1]], Gb], writes=[xnb])
        P.do("dve", lambda e: e.tensor_tensor(xn[:], xn[:], xs[:], op=ALU.add), reads=[xtb[s], xnb], writes=[xnb])
        P.dma("sp", lambda e: e.dma_start(out=X[b][c * 128:(c + 1) * 128, :], in_=xn[:]), ep.xn_ds[s],
              reads=[xnb], writes=[Xc[b][c]])
        h2, h2b = ep.h2[s], ep.h2b[s]
        rms_mod(ep.nctx, xn, xnb, A2, A2b, B2, B2b, h2, h2b)
        P.dma("sp", lambda e: e.dma_start(out=H[b][c * 128:(c + 1) * 128, :], in_=h2[:]), ep.h2_ds[s],
              reads=[h2b], writes=[Hc[b][c]])
        yield
        transpose8(h2, h2b, 128, T_bank, ep.h2T[:], ep.h2Tb)
        yield
        fns = []
        for k in range(8):
            fns.append(lambda e, k=k: e.matmul(PS[L_bank][:, 0:NE], lhsT=ep.h2T[:, k, :], rhs=ep.wr[:, k, :],
                                               start=(k == 0), stop=(k == 7)))
        P.do("pe", fns, reads=[ep.h2Tb, ep.wrb], writes=[PSb[L_bank]])
        sm = ep.sm
        col0 = 32 * b
        P.do("dve", [lambda e: e.memset(sm[:, 2:3], 0.0),
                     lambda e: e.reduce_max(out=sm[:, 0:1], in_=PS[L_bank][:, 0:NE], axis=AX.X)],
             reads=[PSb[L_bank]], writes=[ep.smb])
        P.do("dve", lambda e: e.tensor_scalar(sm[:, 1:2], sm[:, 0:1], -1.0, None, op0=ALU.mult), writes=[ep.smb])
        P.do("act", lambda e: e.activation(out=ep.ex[:], in_=PS[L_bank][:, 0:NE], func=AF.Exp, bias=sm[:, 1:2],
                                           accum_out=sm[:, 2:3]), reads=[PSb[L_bank], ep.smb], writes=[ep.smb])
        P.do("dve", lambda e: e.reciprocal(sm[:, 3:4], sm[:, 2:3]), writes=[ep.smb])
        P.do("dve", lambda e: e.tensor_scalar(ep.aff[:, col0:col0 + NE], ep.ex[:], sm[:, 3:4], None, op0=ALU.mult),
             reads=[ep.smb], writes=[ep.affb])
        ncol = col0 + NE
        yield
        P.do("pe", lambda e: e.transpose(PS[L_bank][0:ncol, 128:256], ep.aff[:, 0:ncol], ident_f[:]),
             reads=[ep.affb, const_b], writes=[PSb[L_bank]])
        P.do("dve", lambda e: e.tensor_copy(affT[col0:col0 + NE, c * 128:(c + 1) * 128], PS[L_bank][col0:col0 + NE, 128:256]),
             reads=[PSb[L_bank]], writes=[affTb])

    def epilogue(ep, l, b, c, ybanks, T_bank, L_bank):
        for _ in epilogue_gen(ep, l, b, c, ybanks, T_bank, L_bank):
            pass

    class Sched:
        def __init__(self):
            self.gens = []

        def add(self, g):
            self.gens.append(g)

        def step(self):
            alive = []
            for g in self.gens:
                try:
                    next(g)
                    alive.append(g)
                except StopIteration:
                    pass
            self.gens = alive

        def drain(self):
            while self.gens:
                self.step()

    def norm1_pass(st, l, b, chunks, hT, hTb, T_bank):
        nctx = NormCtx(st, "n")
        htok = [sb(f"htok{i}", [128, D], BF16, st) for i in range(2)]
        htokb = [Buf() for _ in range(2)]
        xts = list(xt) + [sb(f"xtn{i}", [128, D], F32, st) for i in range(2)]
        xtbs = list(xtb) + [Buf(), Buf()]
        xds = list(xt_ds) + [P.dsem(), P.dsem()]
        NSL = 4

        def load(i):
            c = chunks[i]
            s = i % NSL
            P.dma("sp", lambda e: e.dma_start(out=xts[s][:], in_=X[b][c * 128:(c + 1) * 128, :]), xds[s],
                  reads=[Xc[b][c]], writes=[xtbs[s]])

        for i in range(min(NSL - 1, len(chunks))):
            load(i)
        for i, c in enumerate(chunks):
            if i + NSL - 1 < len(chunks):
                load(i + NSL - 1)
            s = i % NSL
            h = i % 2
            isctx = c >= NCHL
            A, Ab = (modt[3], modb[3]) if isctx else (modt[0], modb[0])
            Bt, Bb = (modt[4], modb[4]) if isctx else (modt[1], modb[1])
            rms_mod(nctx, xts[s], xtbs[s], A, Ab, Bt, Bb, htok[h], htokb[h])
            if l == 0 and b == 0 and c == 0:
                dump(nctx.ss[:, 0:4], nctx.sb_, 0, 4)
                dump(htok[h][:], [htokb[h]], 1024, 1024)
                dump(A[:], [Ab], 2048, 1024)
                dump(Bt[:], [Bb], 3072, 1024)
            transpose8(htok[h], htokb[h], 128, T_bank, hT[:, :, c * 128:(c + 1) * 128], hTb[c])

    def load_mixer_mods(st, l, b, has_ctx, which, shared=None):
        if shared is None:
            shared = (sb(f"gtile{which}", [128, D], F32, st), Buf())
        gt, gtb = shared
        gds = P.dsem()
        base = 0 if which == 1 else 3
        gsrc = norm1_g if which == 1 else norm2_g
        load_scale(0, l, b, base + 1, gsrc, gt, gtb, gds)
        load_mod(1, l, b, base + 0)
        if which == 1:
            load_mod(2, l, b, 2)
        if has_ctx:
            load_scale(3, l, 2, base + 1, gsrc, gt, gtb, gds)
            load_mod(4, l, 2, base + 0)
            if which == 1:
                load_mod(5, l, 2, 2)
        return shared

    def load_win(st, l):
        j = l // 2
        Win = sb("Win", [128, 8, 1536], BF16, st)
        Winb = Buf()
        wds = P.dsem(f"winds{l}")
        src = attn_w_in[j].rearrange("(k p) n -> p k n", p=128)
        colmap = [(0, 512, 0), (768, 1280, 512), (512, 640, 1024), (1280, 1408, 1152), (640, 768, 1280), (1408, 1536, 1408)]
        fns = [lambda e, a=a, bb=bb, o=o: e.dma_start(out=Win[:, :, o:o + (bb - a)], in_=src[:, :, a:bb]) for a, bb, o in colmap]
        P.dma("pool", fns, wds, writes=[Winb])
        return Win, Winb

    def mixer_attn(l, b, has_ctx, Win, Winb):
        j = l // 2
        chunks = list(range(NCH if has_ctx else NCHL))
        with ExitStack() as st0:
            hq = sb("hq", [128, 8, NT], BF16, st0)
            hqb = [Buf() for _ in range(NCH)]
            kT = sb("kT", [128, 4, 2, NT], BF16, st0)
            kTb = [Buf() for _ in range(NCH)]
            vaug = sb("vaug", [128, NCH, 4, 65], BF16, st0)
            vb = [Buf() for _ in range(NCH)]
            ones_b = Buf()
            Wout = sb("Wout", [128, 8, D], BF16, st0)
            Woutb = Buf()
            with ExitStack() as st:
                gsh = load_mixer_mods(st, l, b, has_ctx, 1)
                g4 = sb("g4", [128, 4, 64], F32, st)
                GN = sb("GN", [128, 1280], F32, st)
                GNb = Buf()
                gds_ = P.dsem()
                P.dma("sp", lambda e: e.dma_start(out=g4[:].rearrange("p a d -> p (a d)"),
                                                  in_=qk_gain[j].rearrange("a d -> (a d)").partition_broadcast(128)),
                      gds_, writes=[GNb])
                hoffs = [(0, 0, 8), (1, 512, 8), (2, 1024, 2), (3, 1152, 2)]
                fns = []
                for gi, off, nh in hoffs:
                    fns.append(lambda e, gi=gi, off=off, nh=nh: e.tensor_copy(
                        GN[:, off:off + nh * 64].rearrange("p (h d) -> p h d", d=64),
                        g4[:, gi:gi + 1, :].to_broadcast([128, nh, 64])))
                P.do("dve", fns, reads=[GNb], writes=[GNb])
                P.do("dve", lambda e: e.tensor_scalar(GN[:, 0:1024], GN[:, 0:1024], 0.125, None, op0=ALU.mult), reads=[GNb], writes=[GNb])
                ktok = sb("ktok", [128, 4, 2, 128], BF16, st)
                ktokb = Buf()
                P.do("dve", [lambda e: e.memset(vaug[:, :, :, 64:65], 1.0), lambda e: e.memset(ktok[:], 0.0)],
                     writes=[ones_b, ktokb])
                with ExitStack() as stn:
                    norm1_pass(stn, l, b, chunks, hq, hqb, 0)
                P.barrier()
                load_mixer_mods(st, l, b, has_ctx, 2, gsh)
                wods = P.dsem()
                P.dma("pool", lambda e: e.dma_start(out=Wout[:], in_=attn_w_out[j].rearrange("(k p) n -> p k n", p=128)), wods, writes=[Woutb])

                sq2 = [sb(f"sq{i}", [128, 1280], F32, st) for i in range(2)]
                qn_1 = sb("qn", [128, 1280], F32, st)
                qn2 = [qn_1, qn_1]
                ssq2 = [sb(f"ssq{i}", [128, 64], F32, st) for i in range(2)]
                rt_1 = [sb(f"rt{i}", [128, 640], F32, st) for i in range(2)]
                rt2 = [rt_1, rt_1]
                qkn2 = [sb(f"qkn{i}", [128, 1280], BF16, st) for i in range(2)]
                wkb_1 = Buf()
                wkb2 = [wkb_1, wkb_1]
                sqb2 = [Buf(), Buf()]
                qknb2 = [Buf() for _ in range(2)]
                stb2 = [Buf(small=True) for _ in range(2)]
                rtb_1 = Buf()
                rtb2 = [rtb_1, rtb_1]
                pbanks2 = [(1, 2, 3), (0, 6, 7)]

                def projB(i, c):
                    cs_ = slice(c * 128, (c + 1) * 128)
                    pb = pbanks2[i % 2]
                    for nb_ in range(3):
                        fns = [lambda e, k=k, nb_=nb_: e.matmul(
                            PS[pb[nb_]][:], lhsT=hq[:, k, cs_], rhs=Win[:, k, nb_ * 512:(nb_ + 1) * 512],
                            start=(k == 0), stop=(k == 7)) for k in range(8)]
                        P.do("pe", fns, reads=[hqb[c], Winb], writes=[PSb[pb[nb_]]])

                def restB(i, c):
                    cs_ = slice(c * 128, (c + 1) * 128)
                    p = i % 2
                    pb = pbanks2[p]
                    sq, qn, ssq, rt, qkn = sq2[p], qn2[p], ssq2[p], rt2[p], qkn2[p]
                    wkb, qknb, stb, rtb_ = wkb2[p], qknb2[p], stb2[p], rtb2[p]
                    pbb = [PSb[x] for x in pb]
                    P.do("act", [lambda e: e.activation(out=sq[:, 0:512], in_=PS[pb[0]][:], func=AF.Square),
                                 lambda e: e.activation(out=sq[:, 512:1024], in_=PS[pb[1]][:], func=AF.Square),
                                 lambda e: e.activation(out=sq[:, 1024:1280], in_=PS[pb[2]][:, 0:256], func=AF.Square)],
                         reads=pbb, writes=[sqb2[p]])
                    P.do("act", lambda e: e.activation(out=vaug[:, c, :, 0:64], in_=PS[pb[2]][:, 256:512].rearrange("p (g d) -> p g d", d=64),
                                                       func=AF.Copy), reads=[pbb[2], ones_b], writes=[vb[c]])
                    P.do("dve", lambda e: e.tensor_reduce(out=ssq[:, 0:20], in_=sq[:].rearrange("p (h d) -> p h d", d=64),
                                                          axis=AX.X, op=ALU.add), reads=[sqb2[p]], writes=[stb])
                    P.do("act", lambda e: e.activation(out=ssq[:, 20:40], in_=ssq[:, 0:20], func=AF.Ln, bias=eps_t[:, 0:1], scale=1.0 / 64),
                         reads=[const_b], writes=[stb])
                    P.do("act", lambda e: e.activation(out=ssq[:, 40:60], in_=ssq[:, 20:40], func=AF.Exp, scale=-0.5), writes=[stb])

                def restB2(i, c):
                    cs_ = slice(c * 128, (c + 1) * 128)
                    p = i % 2
                    pb = pbanks2[p]
                    sq, qn, ssq, rt, qkn = sq2[p], qn2[p], ssq2[p], rt2[p], qkn2[p]
                    wkb, qknb, stb, rtb_ = wkb2[p], qknb2[p], stb2[p], rtb2[p]
                    pbb = [PSb[x] for x in pb]
                    P.do("dve", [
                        lambda e: e.tensor_tensor(qn[:, 0:512].rearrange("p (h d) -> p h d", d=64),
                                                  PS[pb[0]][:].rearrange("p (h d) -> p h d", d=64),
                                                  ssq[:, 40:48].unsqueeze(2).to_broadcast([128, 8, 64]), op=ALU.mult),
                        lambda e: e.tensor_tensor(qn[:, 512:1024].rearrange("p (h d) -> p h d", d=64),
                                                  PS[pb[1]][:].rearrange("p (h d) -> p h d", d=64),
                                                  ssq[:, 48:56].unsqueeze(2).to_broadcast([128, 8, 64]), op=ALU.mult),
                        lambda e: e.tensor_tensor(qn[:, 1024:1280].rearrange("p (h d) -> p h d", d=64),
                                                  PS[pb[2]][:, 0:256].rearrange("p (h d) -> p h d", d=64),
                                                  ssq[:, 56:60].unsqueeze(2).to_broadcast([128, 4, 64]), op=ALU.mult)],
                        reads=pbb + [stb], writes=[wkb])
                    if i + 2 < len(chunks):
                        projB(i + 2, chunks[i + 2])
                    P.do("dve", lambda e: e.tensor_tensor(qn[:], qn[:], GN[:], op=ALU.mult), reads=[wkb, GNb], writes=[wkb])
                    if c < NCHL:
                        qv = qn[:].rearrange("p (h rc ab f) -> p h rc ab f", rc=2, ab=2, f=16)
                        ov = qkn[:].rearrange("p (h rc ab f) -> p h rc ab f", rc=2, ab=2, f=16)
                        Aq = qv[:, :, :, 0, :]
                        Bq = qv[:, :, :, 1, :]
                        cosv = rope_t[:, c, 0:32].rearrange("p (rc f) -> p rc f", f=16).unsqueeze(1).to_broadcast([128, 20, 2, 16])
                        sinv = rope_t[:, c, 32:64].rearrange("p (rc f) -> p rc f", f=16).unsqueeze(1).to_broadcast([128, 20, 2, 16])
                        r4 = [t[:].rearrange("p (h rc f) -> p h rc f", rc=2, f=16) for t in rt]
                        P.do("dve", [
                            lambda e: e.tensor_tensor(r4[0], Aq, cosv, op=ALU.mult),
                            lambda e: e.tensor_tensor(r4[1], Bq, sinv, op=ALU.mult)],
                            reads=[wkb, const_b], writes=[rtb_])
                        P.do("dve", lambda e: e.tensor_tensor(ov[:, :, :, 0, :], r4[0], r4[1], op=ALU.subtract),
                             reads=[rtb_], writes=[qknb])
                        P.do("dve", [
                            lambda e: e.tensor_tensor(r4[0], Aq, sinv, op=ALU.mult),
                            lambda e: e.tensor_tensor(r4[1], Bq, cosv, op=ALU.mult)],
                            reads=[wkb, const_b], writes=[rtb_])
                        P.do("dve", lambda e: e.tensor_tensor(ov[:, :, :, 1, :], r4[0], r4[1], op=ALU.add),
                             reads=[rtb_, qknb], writes=[qknb])
                    else:
                        P.do("dve", lambda e: e.tensor_copy(qkn[:], qn[:]), reads=[wkb], writes=[qknb])
                    kv = qkn[:, 1024:1280].rearrange("p (g d) -> p g d", d=64)
                    P.do("dve", [lambda e: e.tensor_copy(ktok[:, :, 0, 0:64], kv),
                                 lambda e: e.tensor_copy(ktok[:, :, 1, 64:128], kv)], reads=[qknb], writes=[ktokb])
                    pv = PS[4][:].bitcast(BF16)
                    fns = [lambda e, i_=i_: e.transpose(pv[:, i_ * 128:(i_ + 1) * 128], qkn[:, i_ * 128:(i_ + 1) * 128], ident_b[:]) for i_ in range(8)]
                    P.do("pe", fns, reads=[qknb, const_b], writes=[PSb[4]])
                    P.do("act", lambda e: e.activation(out=hq[:, :, cs_], in_=pv.rearrange("p (k r) -> p k r", r=128), func=AF.Copy),
                         reads=[PSb[4]], writes=[hqb[c]])
                    pk = PS[5][:].bitcast(BF16)
                    fns = [lambda e, i_=i_: e.transpose(pk[:, i_ * 128:(i_ + 1) * 128], ktok[:, i_ // 2, i_ % 2, :], ident_b[:]) for i_ in range(8)]
                    P.do("pe", fns, reads=[ktokb, const_b], writes=[PSb[5]])
                    P.do("act", lambda e: e.activation(out=kT[:, :, :, cs_], in_=pk.rearrange("p (g r t) -> p g r t", r=2, t=128), func=AF.Copy),
                         reads=[PSb[5]], writes=[kTb[c]])

                projB(0, chunks[0])
                if len(chunks) > 1:
                    projB(1, chunks[1])
                restB(0, chunks[0])
                for i, c in enumerate(chunks):
                    if i + 1 < len(chunks):
                        restB(i + 1, chunks[i + 1])
                    restB2(i, c)
                if dbg and l == 0 and b == 0:
                    P.dma("pool", [lambda e: e.dma_start(out=DBGQ, in_=hq[:]), lambda e: e.dma_start(out=DBGK, in_=kT[:]),
                                   lambda e: e.dma_start(out=DBGV, in_=vaug[:])], dbg_ds, reads=hqb + kTb + vb)
            P.flush()
            with ExitStack() as st:
                ep = make_epi(st, l, b, chunks)
                sk0 = sb("sk0", [128, 8], F32, st)
                sk = sb("sk", [128, 8], F32, st)
                skb = Buf(small=True)
                skds = P.dsem()
                P.dma("sp", lambda e: e.dma_start(out=sk0[:], in_=sink_b[j].partition_broadcast(128)), skds, writes=[skb])
                P.do("act", lambda e: e.activation(out=sk0[:], in_=sk0[:], func=AF.Exp), reads=[skb], writes=[skb])
                P.do("dve", lambda e: e.tensor_copy(sk[:].rearrange("p (g r i) -> p g r i", r=2, i=2),
                                                    sk0[:].rearrange("p (g i r) -> p g r i", r=2, i=2)), reads=[skb], writes=[skb])
                PT = [sb(f"PT{i}", [128, 512], BF16, st) for i in range(3)]
                PTb = [Buf() for _ in range(3)]
                otok = [sb(f"otok{i}", [128, D], BF16, st) for i in range(2)]
                otokb = [Buf() for _ in range(2)]
                oT = sb("oT", [128, 8, 128], BF16, st)
                oTb = Buf()
                den = sb("den", [128, 8], F32, st)
                denb = Buf(small=True)
                S_banks = [0, 1]
                O_banks = [2, 3]
                sctr = 0
                octr = 0
                pctr = 0
                items = []
                for qi, qb in enumerate(chunks):
                    for ty in range(2):
                        for g in range(2):
                            if qb >= NCHL:
                                klist = [(NCHL, None), (NCHL + 1, None)]
                            elif ty == 0:
                                klist = [(kc, None) for kc in chunks]
                            else:
                                klist = []
                                if qb - 1 >= 0:
                                    klist.append((qb - 1, 0))
                                klist.append((qb, None))
                                if qb + 1 < NCHL:
                                    klist.append((qb + 1, 1))
                                if has_ctx:
                                    klist += [(NCHL, None), (NCHL + 1, None)]
                            ob = O_banks[octr % 2]
                            octr += 1
                            for ki, (kc, mk) in enumerate(klist):
                                items.append(dict(qi=qi, qb=qb, ty=ty, g=g, kc=kc, mk=mk, first=(ki == 0),
                                                  last=(ki == len(klist) - 1), ob=ob))
                for i, it in enumerate(items):
                    it["sbk"] = S_banks[i % 2]
                    it["pt"] = i % 3

                def emit_S(it):
                    qb, kc, sbk = it["qb"], it["kc"], it["sbk"]
                    gp = it["ty"] * 2 + it["g"]
                    pair0 = it["ty"] * 4 + it["g"] * 2
                    qs = slice(qb * 128, (qb + 1) * 128)
                    ks = slice(kc * 128, (kc + 1) * 128)
                    fns = [lambda e, r=r: e.matmul(
                        PS[sbk][:, r * 256:(r + 1) * 256].rearrange("p (i q) -> p i q", q=128),
                        lhsT=kT[:, gp, r, ks], rhs=hq[:, pair0:pair0 + 2, qs], start=True, stop=True) for r in range(2)]
                    P.do("pe", fns, reads=[kTb[kc], hqb[qb]], writes=[PSb[sbk]])

                def tail_gen(qi, qb):
                    ot, otb = otok[qi % 2], otokb[qi % 2]
                    if dbg and l == 0 and b == 0:
                        P.dma("pool", lambda e: e.dma_start(out=DBGO[:, qb, :], in_=ot[:]), dbg_ds, reads=[otb])
                    transpose8(ot, otb, 128, 4, oT[:], oTb)
                    yield
                    for nb_ in range(2):
                        fns = [lambda e, k=k, nb_=nb_: e.matmul(PS[5 + nb_][:], lhsT=oT[:, k, :], rhs=Wout[:, k, nb_ * 512:(nb_ + 1) * 512],
                                                               start=(k == 0), stop=(k == 7)) for k in range(8)]
                        P.do("pe", fns, reads=[oTb, Woutb], writes=[PSb[5 + nb_]])
                    yield from epilogue_gen(ep, l, b, qb, [5, 6], 4, 7)

                sched = Sched()

                def emit_rest(it):
                    qi, qb, ty, g, kc, mk, ob, sbk = it["qi"], it["qb"], it["ty"], it["g"], it["kc"], it["mk"], it["ob"], it["sbk"]
                    first, last = it["first"], it["last"]
                    gp = ty * 2 + g
                    pt, ptb = PT[it["pt"]], PTb[it["pt"]]
                    ot, otb = otok[qi % 2], otokb[qi % 2]
                    P.do("act", lambda e: e.activation(out=pt[:], in_=PS[sbk][:], func=AF.Exp), reads=[PSb[sbk]], writes=[ptb])
                    if mk is not None:
                        P.do("dve", lambda e: e.tensor_tensor(
                            pt[:].rearrange("p (c q) -> p c q", q=128), pt[:].rearrange("p (c q) -> p c q", q=128),
                            mask_t[:, mk:mk + 1, :].to_broadcast([128, 4, 128]), op=ALU.mult),
                            reads=[const_b, ptb], writes=[ptb])
                    fns = [lambda e, cb=cb: e.matmul(
                        PS[ob][:, cb * 65:(cb + 1) * 65], lhsT=pt[:, cb * 128:(cb + 1) * 128], rhs=vaug[:, kc, gp, :],
                        start=(first and cb == 0), stop=last, skip_group_check=True) for cb in range(4)]
                    if first:
                        P.do("pe", fns, reads=[ptb, vb[kc], ones_b], writes=[PSb[ob]])
                    else:
                        P.use("pe", reads=[ptb, vb[kc]])
                        for fn in fns[:-1]:
                            P.op("pe", fn)
                        ev = P.op("pe", fns[-1], mark=True)
                        P.done(ev, reads=[ptb, vb[kc]], writes=[])
                        PSb[ob].w = ev
                    if not last:
                        return
                    ov = PS[ob][:, 0:260].rearrange("p (c x) -> p c x", x=65)
                    dslice = den[:, g * 4:g * 4 + 4]
                    if ty == 1:
                        P.do("dve", lambda e: e.tensor_tensor(
                            dslice.unsqueeze(2), ov[:, :, 64:65], sk[:, g * 4:(g + 1) * 4].unsqueeze(2), op=ALU.add),
                            reads=[PSb[ob], skb], writes=[denb])
                    else:
                        P.do("dve", lambda e: e.tensor_copy(dslice.unsqueeze(2), ov[:, :, 64:65]), reads=[PSb[ob]], writes=[denb])
                    P.do("dve", lambda e: e.reciprocal(dslice, dslice), writes=[denb])
                    hb = ty * 8 + g * 4
                    outv = ot[:, hb * 64:(hb + 4) * 64].rearrange("p (i r d) -> p r i d", r=2, d=64)
                    inv = ov[:, :, 0:64].rearrange("p (r i) d -> p r i d", i=2)
                    rdv = dslice.rearrange("p (r i) -> p r i", i=2).unsqueeze(3).to_broadcast([128, 2, 2, 64])
                    P.do("dve", lambda e: e.tensor_tensor(outv, inv, rdv, op=ALU.mult), reads=[PSb[ob], denb], writes=[otb])
                    if ty == 1 and g == 1:
                        sched.add(tail_gen(qi, qb))

                emit_S(items[0])
                for i, it in enumerate(items):
                    if i + 1 < len(items):
                        emit_S(items[i + 1])
                    emit_rest(it)
                    if i % 3 == 2:
                        sched.step()
                sched.drain()
            P.flush()

    def mixer_conv(l, b, has_ctx):
        j = l // 2
        chunks = list(range(NCH if has_ctx else NCHL))
        ntok = NT if has_ctx else NL
        with ExitStack() as st0:
            zT = sb("zT", [128, 8, NT], BF16, st0)
            zTb = Buf()
            Wout = sb("Wout", [128, 8, D], BF16, st0)
            Woutb = Buf()
            with ExitStack() as st:
                gsh = load_mixer_mods(st, l, b, has_ctx, 1)
                hT = sb("hT", [128, 8, NT], BF16, st)
                hTb = [Buf() for _ in range(NCH)]
                with ExitStack() as stn:
                    norm1_pass(stn, l, b, chunks, hT, hTb, 0)
                P.barrier()
                load_mixer_mods(st, l, b, has_ctx, 2, gsh)
                wods = P.dsem()
                P.dma("pool", lambda e: e.dma_start(out=Wout[:], in_=conv_w_out[j].rearrange("(k p) n -> p k n", p=128)), wods, writes=[Woutb])
                kb4 = sb("kb4", [4, D], F32, st)
                kk = sb("kk", [128, 8, 4], F32, st)
                kkb = Buf()
                tds = P.dsem()
                P.dma("sp", lambda e: e.dma_start(out=kb4[:], in_=conv_kb[j]), tds, writes=[kkb])
                fns = [lambda e, m=m: e.transpose(PS[1][:, m * 4:(m + 1) * 4], kb4[0:4, m * 128:(m + 1) * 128], ident_f[0:4, 0:4]) for m in range(8)]
                P.do("pe", fns, reads=[kkb, const_b], writes=[PSb[1]])
                P.do("dve", lambda e: e.tensor_copy(kk[:].rearrange("p m j -> p (m j)"), PS[1][:, 0:32]), reads=[PSb[1]], writes=[kkb])
                wc = [sb(f"wc{i}", [128, 8, 3, 128], BF16, st) for i in range(2)]
                wcb = [Buf() for _ in range(2)]
                wcd = [P.dsem() for _ in range(2)]
                UW = NT + 4
                u = sb("u", [128, UW], F32, st)
                ub = Buf()
                bgb_t = sb("bgb", [128, NT], F32, st)
                bgbb = Buf()
                yb_t = sb("yb", [128, NT], F32, st)
                ybb = Buf()
                vsb = [sb(f"vsb{i}", [128, 512], F32, st) for i in range(2)]
                vsbb = [Buf() for _ in range(2)]
                P.do("dve", lambda e: e.memset(u[:], 0.0), writes=[ub])
                src = conv_w_in[j].rearrange("(k p) n -> p k n", p=128)
                tbs = [(t0, 512) for t0 in range(0, NL, 512)]
                if has_ctx:
                    tbs.append((NL, 256))
                uoff = lambda t0: 1 + t0 if t0 < NL else 3 + t0
                vc = 0
                for m in range(8):
                    s = m % 2
                    fns = [lambda e, w=w, m=m, s=s: e.dma_start(out=wc[s][:, :, w, :], in_=src[:, :, w * D + m * 128:w * D + (m + 1) * 128]) for w in range(3)]
                    P.dma("pool", fns, wcd[s], writes=[wcb[s]])
                    for (t0, tw) in tbs:
                        for w in range(3):
                            fns = [lambda e, k=k, w=w, s=s, t0=t0, tw=tw: e.matmul(PS[2 + w][:, 0:tw], lhsT=wc[s][:, k, w, :], rhs=hT[:, k, t0:t0 + tw],
                                                                                  start=(k == 0), stop=(k == 7)) for k in range(8)]
                            P.do("pe", fns, reads=[wcb[s]] + [hTb[cc] for cc in range(t0 // 128, (t0 + tw) // 128)], writes=[PSb[2 + w]])
                        vs_, vsb_ = vsb[vc % 2], vsbb[vc % 2]
                        vc += 1
                        P.do("act", [lambda e, t0=t0, tw=tw: e.activation(out=bgb_t[:, t0:t0 + tw], in_=PS[2][:, 0:tw], func=AF.Copy)],
                             reads=[PSb[2]], writes=[bgbb])
                        P.do("act", [lambda e, vs_=vs_, tw=tw: e.activation(out=vs_[:, 0:tw], in_=PS[4][:, 0:tw], func=AF.Copy)],
                             reads=[PSb[4]], writes=[vsb_])
                        P.do("dve", lambda e, vs_=vs_, t0=t0, tw=tw: e.tensor_tensor(u[:, uoff(t0):uoff(t0) + tw], PS[3][:, 0:tw], vs_[:, 0:tw], op=ALU.mult),
                             reads=[PSb[3], vsb_], writes=[ub])
                    segs = [(0, NL)] + ([(NL, NCTX)] if has_ctx else [])
                    for (t0, tw) in segs:
                        o = uoff(t0)
                        P.do("act", lambda e, m=m, t0=t0, tw=tw, o=o: e.activation(out=yb_t[:, t0:t0 + tw], in_=u[:, o:o + tw], func=AF.Identity,
                                                                               scale=kk[:, m, 1:2], bias=kk[:, m, 3:4]),
                             reads=[ub, kkb], writes=[ybb])
                        P.do("dve", lambda e, m=m, t0=t0, tw=tw, o=o: e.scalar_tensor_tensor(
                            out=yb_t[:, t0:t0 + tw], in0=u[:, o - 1:o - 1 + tw], scalar=kk[:, m, 0:1],
                            in1=yb_t[:, t0:t0 + tw], op0=ALU.mult, op1=ALU.add), reads=[ub, ybb, kkb], writes=[ybb])
                        P.do("dve", lambda e, m=m, t0=t0, tw=tw, o=o: e.scalar_tensor_tensor(
                            out=yb_t[:, t0:t0 + tw], in0=u[:, o + 1:o + 1 + tw], scalar=kk[:, m, 2:3],
                            in1=yb_t[:, t0:t0 + tw], op0=ALU.mult, op1=ALU.add), reads=[ub, ybb, kkb], writes=[ybb])
                        P.do("dve", lambda e, m=m, t0=t0, tw=tw: e.tensor_tensor(
                            zT[:, m, t0:t0 + tw], bgb_t[:, t0:t0 + tw], yb_t[:, t0:t0 + tw], op=ALU.mult),
                            reads=[ybb, bgbb], writes=[zTb])
            P.flush()
            with ExitStack() as st:
                ep = make_epi(st, l, b, chunks)
                yctr = 0
                sched = Sched()
                for c in chunks:
                    cs_ = slice(c * 128, (c + 1) * 128)
                    yb0 = 0 + 2 * (yctr % 2)
                    yctr += 1
                    for nb_ in range(2):
                        fns = [lambda e, k=k, nb_=nb_, cs_=cs_, yb0=yb0: e.matmul(PS[yb0 + nb_][:], lhsT=zT[:, k, cs_], rhs=Wout[:, k, nb_ * 512:(nb_ + 1) * 512],
                                                                                start=(k == 0), stop=(k == 7)) for k in range(8)]
                        P.do("pe", fns, reads=[zTb, Woutb], writes=[PSb[yb0 + nb_]])
                    sched.add(epilogue_gen(ep, l, b, c, [yb0, yb0 + 1], 4, 7))
                    sched.step()
                sched.drain()
            P.flush()

    def moe(l, has_ctx):
        pieces = [(0, 128), (128, 128)] + ([(256, 32)] if has_ctx else [])
        with ExitStack() as st0:
            VALS = sb("VALS", [128, 3, 48], F32, st0)
            IDX = sb("IDX", [128, 3, 48], I32, st0)
            rb = Buf(small=True)
            NS = 2
            Wg = [sb(f"Wg{i}", [128, 8, D], BF16, st0) for i in range(NS)]
            Wu = [sb(f"Wu{i}", [128, 8, D], BF16, st0) for i in range(NS)]
            Wd = [sb(f"Wd{i}", [128, 8, D], BF16, st0) for i in range(NS)]
            Wb = [[Buf() for _ in range(3)] for _ in range(NS)]
            Wds = [[P.dsem(f"wds{i}_{k}") for k in range(3)] for i in range(NS)] if not hasattr(P, "_wds") else P._wds
            P._wds = Wds

            def load_w(e_):
                s = e_ % NS
                for wi, (dst, srcw) in enumerate(((Wg[s], w_gate), (Wu[s], w_up), (Wd[s], w_down))):
                    P.dma("pool", lambda e, dst=dst, srcw=srcw, e_=e_: e.dma_start(out=dst[:], in_=srcw[l, e_].rearrange("(k p) f -> p k f", p=128)),
                          Wds[s][wi], writes=[Wb[s][wi]])

            load_w(0)
            with ExitStack() as st:
                wk = sb("wk", [48, NL], F32, st)
                wkc = sb("wkc", [48, NCTX], F32, st)
                vals = sb("vals", [48, CAP_L + CAP_C], F32, st)
                idxu = sb("idxu", [48, CAP_L + CAP_C], U32, st)
                idxf = sb("idxf", [48, CAP_L + CAP_C], F32, st)
                tb_ = Buf(small=True)
                fns = [lambda e: e.tensor_copy(wk[:], affT[:, 0:NL])]
                for it in range(CAP_L // 8):
                    sl = slice(it * 8, (it + 1) * 8)
                    fns.append(lambda e, sl=sl: e.max(out=vals[:, sl], in_=wk[:]))
                    fns.append(lambda e, sl=sl: e.max_index(out=idxu[:, sl], in_max=vals[:, sl], in_values=wk[:]))
                    fns.append(lambda e, sl=sl: e.match_replace(out=wk[:], in_to_replace=vals[:, sl], in_values=wk[:], imm_value=-1.0))
                if has_ctx:
                    fns.append(lambda e: e.tensor_copy(wkc[:], affT[:, NL:NT]))
                    for it in range(CAP_C // 8):
                        sl = slice(CAP_L + it * 8, CAP_L + (it + 1) * 8)
                        fns.append(lambda e, sl=sl: e.max(out=vals[:, sl], in_=wkc[:]))
                        fns.append(lambda e, sl=sl: e.max_index(out=idxu[:, sl], in_max=vals[:, sl], in_values=wkc[:]))
                        fns.append(lambda e, sl=sl: e.match_replace(out=wkc[:], in_to_replace=vals[:, sl], in_values=wkc[:], imm_value=-1.0))
                fns.append(lambda e: e.tensor_copy(idxf[:], idxu[:]))
                if has_ctx:
                    fns.append(lambda e: e.tensor_scalar(idxf[:, CAP_L:CAP_L + CAP_C], idxf[:, CAP_L:CAP_L + CAP_C], float(NL), None, op0=ALU.add))
                P.use("dve", reads=[affTb])
                for fn in fns:
                    P.do("dve", fn, writes=[tb_])
                for pi, (so, rows) in enumerate(pieces):
                    P.do("pe", [lambda e, so=so, rows=rows: e.transpose(PS[0][0:rows, 0:48], vals[:, so:so + rows], ident_f[0:48, 0:48]),
                                lambda e, so=so, rows=rows: e.transpose(PS[0][0:rows, 64:112], idxf[:, so:so + rows], ident_f[0:48, 0:48])],
                         reads=[tb_, const_b], writes=[PSb[0]])
                    P.do("dve", [lambda e, pi=pi, rows=rows: e.tensor_copy(VALS[0:rows, pi, :], PS[0][0:rows, 0:48]),
                                 lambda e, pi=pi, rows=rows: e.tensor_copy(IDX[0:rows, pi, :], PS[0][0:rows, 64:112])],
                         reads=[PSb[0]], writes=[rb])
            P.flush()
            with ExitStack() as st:
                load_mod(0, l, 0, 5)
                load_mod(1, l, 1, 5)
                if has_ctx:
                    load_mod(3, l, 2, 5)
                xe = [[[sb(f"xe{b}_{pi}", [128, D], BF16, st) for pi in range(len(pieces))] for b in range(NB)] for _ in range(2)]
                xeb = [[[Buf() for _ in pieces] for b in range(NB)] for _ in range(2)]
                xeds = [[P.dsem() for b in range(NB)] for _ in range(2)]
                xeT = sb("xeT", [128, 8, 2 * 288], BF16, st)
                xeTb = [[Buf() for _ in pieces] for b in range(NB)]
                hidT = sb("hidT", [128, 8, 2 * 288], BF16, st)
                hidTb = Buf()
                sg = [sb(f"sg{i}", [128, 288], F32, st) for i in range(2)]
                sgb = [Buf() for _ in range(2)]
                ye = [sb(f"ye{i}", [128, D], F32, st) for i in range(3)]
                yeb = [Buf() for _ in range(3)]
                sc_ds = [P.dsem() for _ in range(NB)]
                ncols = 256 + (32 if has_ctx else 0)
                yectr = 0
                tctr = 0

                def issue_gathers(e_):
                    xs_ = e_ % 2
                    for b in range(NB):
                        col = b * 32 + e_
                        fns = [lambda e, b=b, pi=pi, rows=rows, col=col: e.indirect_dma_start(
                            out=xe[xs_][b][pi][0:rows, :], out_offset=None, in_=H[b],
                            in_offset=bass.IndirectOffsetOnAxis(ap=IDX[0:rows, pi, col:col + 1], axis=0))
                            for pi, (so, rows) in enumerate(pieces)]
                        P.dma("pool", fns, xeds[xs_][b], reads=[rb] + Hc[b], writes=xeb[xs_][b])

                def do_transposes(e_, b):
                    nonlocal tctr
                    xs_ = e_ % 2
                    for pi, (so, rows) in enumerate(pieces):
                        tbk = tctr % 2
                        tctr += 1
                        c0 = b * 288 + so
                        transpose8(xe[xs_][b][pi], xeb[xs_][b][pi], rows, tbk, xeT[:, :, c0:c0 + rows], xeTb[b][pi])

                issue_gathers(0)
                for e_ in range(NE):
                    s = e_ % NS
                    if e_ + 1 < NE:
                        load_w(e_ + 1)
                        issue_gathers(e_ + 1)
                    do_transposes(e_, 0)
                    for m in range(8):
                        ms = slice(m * 128, (m + 1) * 128)
                        for b in range(NB):
                            c0 = b * 288
                            if m == 0 and b == 1:
                                do_transposes(e_, 1)
                            for wi, W in enumerate((Wg[s], Wu[s])):
                                bank = 2 + b * 2 + wi
                                fns = [lambda e, k=k, W=W, bank=bank, c0=c0, ms=ms: e.matmul(PS[bank][:, 0:ncols], lhsT=W[:, k, ms], rhs=xeT[:, k, c0:c0 + ncols],
                                                                                           start=(k == 0), stop=(k == 7)) for k in range(8)]
                                P.do("pe", fns, reads=[Wb[s][wi]] + xeTb[b], writes=[PSb[bank]])
                            gb_, ub_ = 2 + b * 2, 3 + b * 2
                            P.do("act", lambda e, b=b, gb_=gb_: e.activation(out=sg[b][:, 0:ncols], in_=PS[gb_][:, 0:ncols], func=AF.Silu),
                                 reads=[PSb[gb_]], writes=[sgb[b]])
                            P.do("dve", lambda e, b=b, ub_=ub_, m=m, c0=c0: e.tensor_tensor(hidT[:, m, c0:c0 + ncols], sg[b][:, 0:ncols], PS[ub_][:, 0:ncols], op=ALU.mult),
                                 reads=[sgb[b], PSb[ub_]], writes=[hidTb])
                    for b in range(NB):
                        col = b * 32 + e_
                        scat = []
                        yused = []
                        for pi, (so, rows) in enumerate(pieces):
                            c0 = b * 288 + so
                            yi = yectr % 3
                            yectr += 1
                            Gt, Gtb = (modt[3], modb[3]) if pi == 2 else (modt[b], modb[b])
                            for nb_ in range(2):
                                bank = 6 + nb_
                                fns = [lambda e, k=k, bank=bank, c0=c0, rows=rows, nb_=nb_, s=s: e.matmul(PS[bank][0:rows, :], lhsT=hidT[:, k, c0:c0 + rows],
                                                                                                  rhs=Wd[s][:, k, nb_ * 512:(nb_ + 1) * 512],
                                                                                                  start=(k == 0), stop=(k == 7)) for k in range(8)]
                                P.do("pe", fns, reads=[hidTb, Wb[s][2]], writes=[PSb[bank]])
                                P.do("dve", lambda e, yi=yi, rows=rows, nb_=nb_, bank=bank, pi=pi, col=col, Gt=Gt: e.scalar_tensor_tensor(
                                    out=ye[yi][0:rows, nb_ * 512:(nb_ + 1) * 512], in0=PS[bank][0:rows, :], scalar=VALS[0:rows, pi, col:col + 1],
                                    in1=Gt[0:rows, nb_ * 512:(nb_ + 1) * 512], op0=ALU.mult, op1=ALU.mult),
                                    reads=[PSb[bank], rb, Gtb], writes=[yeb[yi]])
                            yused.append(yeb[yi])
                            scat.append(lambda e, b=b, yi=yi, rows=rows, pi=pi, col=col: e.indirect_dma_start(
                                out=X[b], out_offset=bass.IndirectOffsetOnAxis(ap=IDX[0:rows, pi, col:col + 1], axis=0),
                                in_=ye[yi][0:rows, :], in_offset=None, compute_op=ALU.add))
                        P.dma("pool", scat, sc_ds[b], reads=yused + [rb], writes=Xc[b])
            P.flush()

    def final():
        ds = P.dsem("final")
        fns = []
        for b in range(NB):
            for r0 in range(0, NL, 256):
                fns.append(lambda e, b=b, r0=r0: e.dma_start(out=out[b, r0:r0 + 256, :], in_=X[b][r0:r0 + 256, :]))
        ev = P.dma("sp", fns, ds, reads=[c for b in range(NB) for c in Xc[b]])
        P.engs["sp"].q.append(("w", ev[0], ev[1]))
        P.flush()

    phase0()
    done_ = False
    for l in range(n_layers):
        has_ctx = l < DEPTH - 1
        with ExitStack() as stl:
            if l % 2 == 0:
                Win_, Winb_ = load_win(stl, l)
            for b in range(NB):
                if l % 2 == 0:
                    mixer_attn(l, b, has_ctx, Win_, Winb_)
                else:
                    mixer_conv(l, b, has_ctx)
        if stop == (l, "mixer"):
            break
        moe(l, has_ctx)
        if stop == (l, "moe"):
            break
    final()
    es.close()
    return nc


def _host_consts():
    rows = NL // 64
    row = np.repeat(np.arange(rows, dtype=np.float32), 64)
    col = np.tile(np.arange(64, dtype=np.float32), rows)
    inv_freq = (10000.0 ** (-np.arange(0, 32, 2, dtype=np.float32) / 32.0)).astype(np.float32)
    ang_r = row[:, None] * inv_freq[None, :]
    ang_c = col[:, None] * inv_freq[None, :]
    rope = np.concatenate([np.cos(ang_r), np.cos(ang_c), np.sin(ang_r), np.sin(ang_c)], axis=1).astype(np.float32)
    ident = np.eye(128, dtype=np.float32)
    kk = np.arange(128)[:, None]
    qq = np.arange(128)[None, :]
    masks = np.stack([(kk >= qq), (kk <= qq)]).astype(np.float32)
    return rope, ident, masks


_NC_CACHE = {}
_LAST = None


def kernel(x, c, ctx, c_ctx, ada_w, ada_b, norm1_g, norm2_g, attn_w_in, attn_w_out,
           qnorm_a, knorm_a, qnorm_b, knorm_b, sink_b, conv_w_in, conv_k, conv_b, conv_w_out,
           router_w, moe_w_gate, moe_w_up, moe_w_down, _n_layers=DEPTH, _stop=None, _cores=8, _trace=False):
    f = lambda a: np.ascontiguousarray(np.asarray(a, dtype=np.float32))
    rope, ident, masks = _host_consts()
    qk_gain = np.stack([f(qnorm_a), f(qnorm_b), f(knorm_a), f(knorm_b)], axis=1)
    conv_kb = np.concatenate([f(conv_k), f(conv_b)[:, None, :]], axis=1)
    shared = {
        "ada_w": f(ada_w), "ada_b": f(ada_b), "norm1_g": f(norm1_g), "norm2_g": f(norm2_g),
        "attn_w_in": f(attn_w_in), "attn_w_out": f(attn_w_out), "qk_gain": np.ascontiguousarray(qk_gain),
        "sink_b": f(sink_b), "conv_w_in": f(conv_w_in), "conv_kb": np.ascontiguousarray(conv_kb),
        "conv_w_out": f(conv_w_out), "router_w": f(router_w), "moe_w_gate": f(moe_w_gate),
        "moe_w_up": f(moe_w_up), "moe_w_down": f(moe_w_down), "rope": rope, "ident": ident, "masks": masks,
    }
    x = f(x)
    ctx = f(ctx)
    c = f(c)
    c_ctx = f(c_ctx)
    key = (_n_layers, _stop)
    if key not in _NC_CACHE:
        _NC_CACHE[key] = build_program(_n_layers, _stop)
    nc = _NC_CACHE[key]
    in_maps = []
    for i in range(_cores):
        m = dict(shared)
        m["x"] = x[NB * i:NB * (i + 1)]
        m["ctx"] = ctx[NB * i:NB * (i + 1)]
        m["cvec"] = np.ascontiguousarray(np.concatenate([c[NB * i:NB * (i + 1)], c_ctx[None, :]], axis=0))
        in_maps.append(m)
    res = run_bass_kernel_spmd(nc, in_maps, core_ids=list(range(_cores)), **({"trace": True} if _trace else {}))
    global _LAST
    _LAST = res.results
    if _trace:
        print("EXEC_TIME_NS", res.exec_time_ns)
    return np.concatenate([np.asarray(r["out"]) for r in res.results], axis=0).astype(np.float32)
```

```python
import numpy as np
from contextlib import ExitStack
import concourse.bass as bass
import concourse.mybir as mybir
from concourse.bass_utils import run_bass_kernel_spmd

F32 = mybir.dt.float32
BF16 = mybir.dt.bfloat16
I32 = mybir.dt.int32
U32 = mybir.dt.uint32
ALU = mybir.AluOpType
AF = mybir.ActivationFunctionType
AX = mybir.AxisListType

D = 1024
NL = 2048
NCTX = 256
NT = NL + NCTX
NCH = NT // 128
NCHL = NL // 128
DEPTH = 4
NE = 16
CAP_L = 256
CAP_C = 32
EPS = 1e-6
NB = 2

DEBUG_STOP = None
SBDBG = False


class Eng:
    def __init__(self, name, sems):
        self.name = name
        self.sems = sems
        self.cur = 0
        self.count = 0
        self.q = []
        self.waited = {}

    def mark(self):
        if self.count >= 30000:
            self.cur += 1
            self.count = 0
        self.count += 1
        return (self.sems[self.cur], self.count)


class DmaSem:
    def __init__(self, sem):
        self.sem = sem
        self.count = 0


class Buf:
    __slots__ = ("name", "w", "r", "small")

    def __init__(self, name="", small=False):
        self.name = name
        self.w = None
        self.r = {}
        self.small = small


class Planner:
    def __init__(self, nc, es):
        self.nc = nc
        mk = lambda n: es.enter_context(nc.semaphore(n))
        self.engs = {
            "pe": Eng("pe", [mk(f"pe{i}") for i in range(6)]),
            "act": Eng("act", [mk(f"act{i}") for i in range(3)]),
            "dve": Eng("dve", [mk(f"dve{i}") for i in range(3)]),
            "pool": Eng("pool", [mk(f"pool{i}") for i in range(2)]),
            "sp": Eng("sp", [mk("sp0")]),
        }
        self.owner = {}
        for e in self.engs.values():
            for s in e.sems:
                self.owner[s.num] = e.name
        self.es = es
        self.n_dsem = 0
        self.free_ds = []
        self.phase_ds = []

    def dsem(self, name=None):
        if name is None and self.free_ds:
            d = self.free_ds.pop()
            self.phase_ds.append(d)
            return d
        self.n_dsem += 1
        d = DmaSem(self.es.enter_context(self.nc.semaphore(name or f"d{self.n_dsem}")))
        if name is None:
            self.phase_ds.append(d)
        return d

    def wait(self, eng, ev, force=False):
        if ev is None:
            return
        sem, val = ev
        e = self.engs[eng]
        if self.owner.get(sem.num) == eng and not (force and eng in ("act", "dve", "pool")):
            return
        if e.waited.get(sem.num, 0) >= val:
            return
        e.waited[sem.num] = val
        e.q.append(("w", sem, val))

    def op(self, eng, fn, mark=False):
        e = self.engs[eng]
        if mark:
            ev = e.mark()
            e.q.append(("o", fn, ev[0], 1))
            return ev
        e.q.append(("o", fn, None, 0))
        return None

    def use(self, eng, reads=(), writes=()):
        for b in reads:
            self.wait(eng, b.w, True)
        for b in writes:
            self.wait(eng, b.w, True)
            for num, (sem, val) in list(b.r.items()):
                self.wait(eng, (sem, val), b.small)

    @staticmethod
    def done(ev, reads=(), writes=()):
        for b in reads:
            old = b.r.get(ev[0].num)
            if old is None or old[1] < ev[1]:
                b.r[ev[0].num] = ev
        for b in writes:
            b.w = ev
            b.r = {}

    def do(self, eng, fns, reads=(), writes=()):
        if not isinstance(fns, (list, tuple)):
            fns = [fns]
        self.use(eng, reads, writes)
        for fn in fns[:-1]:
            self.op(eng, fn)
        ev = self.op(eng, fns[-1], mark=True)
        self.done(ev, reads, writes)
        return ev

    def dma(self, eng, fns, ds, reads=(), writes=()):
        if not isinstance(fns, (list, tuple)):
            fns = [fns]
        self.use(eng, reads, writes)
        e = self.engs[eng]
        for fn in fns:
            ds.count += 16
            e.q.append(("o", fn, ds.sem, 16))
        ev = (ds.sem, ds.count)
        self.done(ev, reads, writes)
        return ev

    def barrier(self):
        for en in ("sp", "pool", "act", "dve", "pe"):
            for cn in ("pe", "act", "dve"):
                c = self.engs[cn]
                if cn != en and c.count > 0:
                    self.wait(en, (c.sems[c.cur], c.count))

    def flush(self):
        nc = self.nc
        self.barrier()
        for d in self.phase_ds:
            if d.count > 0:
                self.wait("sp", (d.sem, d.count))
                self.wait("pool", (d.sem, d.count))
        self.free_ds.extend(self.phase_ds)
        self.phase_ds = []
        qs = {k: e.q for k, e in self.engs.items()}
        for e in self.engs.values():
            e.q = []

        def replay(q, eng):
            for ent in q:
                if ent[0] == "w":
                    eng.wait_ge(ent[1], ent[2])
                else:
                    ins = ent[1](eng)
                    if ent[2] is not None:
                        ins.then_inc(ent[2], ent[3])

        with nc.Block() as blk:
            if qs["pe"]:
                @blk.tensor
                def _(e):
                    replay(qs["pe"], e)
            if qs["act"]:
                @blk.scalar
                def _(e):
                    replay(qs["act"], e)
            if qs["dve"]:
                @blk.vector
                def _(e):
                    replay(qs["dve"], e)
            if qs["pool"]:
                @blk.gpsimd
                def _(e):
                    replay(qs["pool"], e)
            if qs["sp"]:
                @blk.sync
                def _(e):
                    replay(qs["sp"], e)


def build_program(n_layers=DEPTH, stop=None):
    nc = bass.Bass("TRN2", target_bir_lowering=False)
    es = ExitStack()
    P = Planner(nc, es)

    def din(name, shape, dt=F32):
        return nc.dram_tensor(name, shape, dt, kind="ExternalInput").ap()

    x_in = din("x", [NB, NL, D])
    ctx_in = din("ctx", [NB, NCTX, D])
    cvec = din("cvec", [3, D])
    ada_w = din("ada_w", [DEPTH, D, 6 * D])
    ada_b = din("ada_b", [DEPTH, 6 * D])
    norm1_g = din("norm1_g", [DEPTH, D])
    norm2_g = din("norm2_g", [DEPTH, D])
    attn_w_in = din("attn_w_in", [2, D, 1536])
    attn_w_out = din("attn_w_out", [2, D, D])
    qk_gain = din("qk_gain", [2, 4, 64])
    sink_b = din("sink_b", [2, 8])
    conv_w_in = din("conv_w_in", [2, D, 3 * D])
    conv_kb = din("conv_kb", [2, 4, D])
    conv_w_out = din("conv_w_out", [2, D, D])
    router_w = din("router_w", [DEPTH, D, NE])
    w_gate = din("moe_w_gate", [DEPTH, NE, D, D])
    w_up = din("moe_w_up", [DEPTH, NE, D, D])
    w_down = din("moe_w_down", [DEPTH, NE, D, D])
    rope_in = din("rope", [NL, 64])
    ident_in = din("ident", [128, 128])
    masks_in = din("masks", [2, 128, 128])
    OUT = [nc.dram_tensor(f"out{b}", [NL, D], F32, kind="ExternalOutput").ap() for b in range(NB)]
    direct_out = (n_layers == DEPTH and stop is None)

    def xsrc(l, b, c):
        if l == 0:
            return x_in[b, c * 128:(c + 1) * 128, :] if c < NCHL else ctx_in[b, (c - NCHL) * 128:(c - NCHL + 1) * 128, :]
        return X[b][c * 128:(c + 1) * 128, :]

    def xdst(l, b):
        return OUT[b] if (direct_out and l == DEPTH - 1) else X[b]

    dbg = bool(stop) or n_layers < DEPTH
    kindI = "ExternalOutput" if dbg else "Internal"
    X = [nc.dram_tensor(f"Xs{b}", [NT, D], F32, kind=kindI).ap() for b in range(NB)]
    H = [nc.dram_tensor(f"Hs{b}", [NT, D], BF16, kind=kindI).ap() for b in range(NB)]
    MOD = nc.dram_tensor("MODs", [DEPTH, 3, 6 * D], F32, kind=kindI).ap()
    DBG = nc.dram_tensor("DBG", [128, 4096], F32, kind="ExternalOutput").ap() if dbg else None
    DBGQ = nc.dram_tensor("DBGQ", [128, 8, NT], F32, kind="ExternalOutput").ap() if dbg else None
    DBGK = nc.dram_tensor("DBGK", [128, 4, 2, NT], F32, kind="ExternalOutput").ap() if dbg else None
    DBGV = nc.dram_tensor("DBGV", [128, NCH, 4, 65], F32, kind="ExternalOutput").ap() if dbg else None
    DBGO = nc.dram_tensor("DBGO", [128, NCH, D], F32, kind="ExternalOutput").ap() if dbg else None

    WB = nc.dram_tensor("WBs", [3, NE, D, D], BF16, kind="Internal").ap()
    WBb = [[Buf() for _ in range(3)] for _ in range(NE)]
    PRE = {"jobs": [], "ds": None, "k": 0}

    def precast_begin(l, half):
        srcs = (w_gate, w_up, w_down)
        PRE["jobs"] = [(l, e_, wi, srcs[wi]) for e_ in range(half * 8, half * 8 + 8) for wi in range(3)]
        if PRE["ds"] is None:
            PRE["ds"] = [P.dsem(f"pre{i}") for i in range(4)]
        PRE["k"] = 0

    def precast_step(n=1):
        for _ in range(n):
            if not PRE["jobs"]:
                return
            l, e_, wi, srcw = PRE["jobs"].pop(0)
            ds = PRE["ds"][PRE["k"] % 4]
            PRE["k"] += 1
            P.dma("pool", lambda e, l=l, e_=e_, wi=wi, srcw=srcw: e.dma_start(
                out=WB[wi, e_].rearrange("(k p) f -> p k f", p=128), in_=srcw[l, e_].rearrange("(k p) f -> p k f", p=128)),
                ds, writes=[WBb[e_][wi]])

    def precast_finish():
        precast_step(len(PRE["jobs"]))
        for d in PRE["ds"]:
            if d.count > 0:
                P.wait("sp", (d.sem, d.count))
                P.wait("pool", (d.sem, d.count))

    Xc = [[Buf(f"X{b}_{c}") for c in range(NCH)] for b in range(NB)]
    Hc = [[Buf(f"H{b}_{c}") for c in range(NCH)] for b in range(NB)]
    MODb = Buf("MOD")

    _uid = [0]

    def sb(name, shape, dt, st=es):
        _uid[0] += 1
        if SBDBG:
            print("SB", name, shape, dt, "remaining", nc.sbuf_bytes_remaining)
        return st.enter_context(nc.sbuf_tensor(f"{name}_{_uid[0]}", shape, dt))

    PS = [es.enter_context(nc.psum_tensor(f"ps{i}", [128, 512], F32)) for i in range(8)]
    PSb = [Buf(f"ps{i}") for i in range(8)]

    ident_f = sb("ident_f", [128, 128], F32)
    ident_b = sb("ident_b", [128, 128], BF16)
    mask_t = sb("mask_t", [128, 2, 128], BF16)
    mask_f = sb("mask_f", [128, 2, 128], F32)
    rope_t = sb("rope_t", [128, NCHL, 64], F32)
    xt = [sb(f"xt{i}", [128, D], F32) for i in range(2)]
    xtb = [Buf(f"xt{i}") for i in range(2)]
    xt_ds = [P.dsem(f"xtd{i}") for i in range(2)]
    modt = [sb(f"modt{i}", [128, D], F32) for i in range(6)]
    modb = [Buf(f"modt{i}") for i in range(6)]
    mod_ds = [P.dsem(f"modd{i}") for i in range(6)]
    affT = sb("affT", [48, NT], F32)
    affTb = Buf("affT")
    eps_t = sb("eps_t", [128, 1], F32)
    const_b = Buf("consts")
    setup_ds = P.dsem("setup")
    setup2_ds = P.dsem("setup2")

    def phase0():
        with ExitStack() as st:
            cs = sb("cs", [3, D], F32, st)
            scs = sb("scs", [3, D], F32, st)
            scT = sb("scT", [128, 8, 3], F32, st)
            adab = sb("adab", [3, 6 * D], F32, st)
            modrow = sb("modrow", [3, 6 * D], F32, st)
            wsl = [sb(f"adaw{i}", [128, 8, 512], F32, st) for i in range(2)]
            wslb = [Buf() for _ in range(2)]
            wds = [P.dsem() for i in range(2)]
            csb, adabb, modrowb, scTb = Buf(), Buf(), Buf(), Buf()
            ds2 = P.dsem()
            store_ds = P.dsem("p0store")

            fns = [
                lambda e: e.dma_start(out=ident_f[:], in_=ident_in),
                lambda e: e.dma_start(out=mask_f[:], in_=masks_in.rearrange("m k q -> k m q")),
                lambda e: e.dma_start(out=rope_t[:], in_=rope_in.rearrange("(c p) f -> p c f", p=128)),
                lambda e: e.dma_start(out=cs[:], in_=cvec),
            ]
            P.dma("sp", fns, setup2_ds, writes=[const_b, csb])
            P.do("dve", [lambda e: e.tensor_copy(ident_b[:], ident_f[:]),
                         lambda e: e.tensor_copy(mask_t[:], mask_f[:]),
                         lambda e: e.memset(eps_t[:], EPS),
                         lambda e: e.memset(affT[:], 0.0)], reads=[], writes=[const_b, affTb])
            scsb = Buf()
            P.do("act", lambda e: e.activation(out=scs[:], in_=cs[:], func=AF.Silu), reads=[csb], writes=[scsb])
            fns = []
            for k in range(8):
                fns.append(lambda e, k=k: e.transpose(PS[0][:, k * 3:(k + 1) * 3], scs[0:3, k * 128:(k + 1) * 128], ident_f[0:3, 0:3]))
            P.do("pe", fns, reads=[scsb, const_b], writes=[PSb[0]])
            P.do("dve", lambda e: e.tensor_copy(scT[:].rearrange("p k j -> p (k j)"), PS[0][:, 0:24]), reads=[PSb[0]], writes=[scTb])

            nblk = 12
            it = 0
            for l in range(n_layers):
                P.dma("sp", lambda e, l=l: e.dma_start(out=adab[:], in_=ada_b[l].partition_broadcast(3)), ds2, writes=[adabb])
                for nb in range(nblk):
                    s = it % 2
                    it += 1
                    P.dma("sp", lambda e, l=l, nb=nb, s=s: e.dma_start(
                        out=wsl[s][:], in_=ada_w[l].rearrange("(k p) n -> p k n", p=128)[:, :, nb * 512:(nb + 1) * 512]),
                        wds[s], writes=[wslb[s]])
                    pb = 1 + (it % 2)
                    fns = []
                    for k in range(8):
                        fns.append(lambda e, k=k, s=s, pb=pb: e.matmul(PS[pb][0:3, :], lhsT=scT[:, k, :], rhs=wsl[s][:, k, :],
                                                                      start=(k == 0), stop=(k == 7)))
                    P.do("pe", fns, reads=[wslb[s], scTb], writes=[PSb[pb]])
                    P.do("dve", lambda e, nb=nb, pb=pb: e.tensor_tensor(modrow[:, nb * 512:(nb + 1) * 512], PS[pb][0:3, :],
                                                                      adab[:, nb * 512:(nb + 1) * 512], op=ALU.add),
                         reads=[PSb[pb], adabb], writes=[modrowb])
                P.dma("sp", lambda e, l=l: e.dma_start(out=MOD[l], in_=modrow[:]), store_ds, reads=[modrowb], writes=[MODb])
        P.flush()

    dbg_ds = P.dsem("dbg") if dbg else None

    def dump(src_ap, bufs, col0, ncols, rows=128):
        if not dbg:
            return
        P.dma("pool", lambda e: e.dma_start(out=DBG[0:rows, col0:col0 + ncols], in_=src_ap), dbg_ds, reads=bufs)

    def load_mod(slot, l, j, idx):
        return P.dma("sp", lambda e: e.dma_start(out=modt[slot][:], in_=MOD[l, j, idx * D:(idx + 1) * D].partition_broadcast(128)),
                     mod_ds[slot], reads=[MODb], writes=[modb[slot]])

    def load_scale(slot, l, j, idx, gsrc, gtile, gtb, gds):
        load_mod(slot, l, j, idx)
        P.dma("sp", lambda e: e.dma_start(out=gtile[:], in_=gsrc[l].partition_broadcast(128)), gds, writes=[gtb])
        P.do("dve", lambda e: e.scalar_tensor_tensor(out=modt[slot][:], in0=modt[slot][:], scalar=1.0, in1=gtile[:],
                                                     op0=ALU.add, op1=ALU.mult), reads=[gtb], writes=[modb[slot]])

    class NormCtx:
        def __init__(self, st, tag):
            self.junk = sb(f"junk{tag}", [128, D], BF16, st)
            self.ss = sb(f"ss{tag}", [128, 8], F32, st)
            self.tmp = sb(f"ntmp{tag}", [128, D], F32, st)
            self.sb_ = [Buf(small=True), Buf(small=True)]
            self.tb = Buf()
            self.i = 0
            P.do("dve", lambda e: e.memset(self.ss[:], 0.0), writes=self.sb_)

    def rms_mod(nctx, src, srcb, A, Ab, Bt, Bb, dst, dstb):
        p = nctx.i % 2
        nctx.i += 1
        ss = nctx.ss[:, 4 * p:4 * p + 4]
        ssb = nctx.sb_[p]
        P.do("act", lambda e: e.activation(out=nctx.junk[:], in_=src[:], func=AF.Square, accum_out=ss[:, 0:1]),
             reads=[srcb, const_b], writes=[ssb])
        P.do("act", lambda e: e.activation(out=ss[:, 1:2], in_=ss[:, 0:1], func=AF.Ln, bias=eps_t[:, 0:1], scale=1.0 / D),
             reads=[const_b], writes=[ssb])
        P.do("act", lambda e: e.activation(out=ss[:, 2:3], in_=ss[:, 1:2], func=AF.Exp, scale=-0.5), writes=[ssb])
        P.do("dve", lambda e: e.scalar_tensor_tensor(out=nctx.tmp[:], in0=src[:], scalar=ss[:, 2:3], in1=A[:],
                                                     op0=ALU.mult, op1=ALU.mult),
             reads=[srcb, Ab, ssb], writes=[nctx.tb])
        P.do("dve", lambda e: e.tensor_tensor(dst[:], nctx.tmp[:], Bt[:], op=ALU.add),
             reads=[Bb, nctx.tb], writes=[dstb])
        P.do("dve", lambda e: e.memset(ss[:, 0:1], 0.0), writes=[ssb])

    def transpose8(src, srcb, rows, bank, dstap, dstb, evac="act"):
        pv = PS[bank][:].bitcast(BF16)
        fns = []
        for k in range(8):
            fns.append(lambda e, k=k: e.transpose(pv[:, k * rows:(k + 1) * rows], src[0:rows, k * 128:(k + 1) * 128],
                                                  ident_b[0:rows, 0:rows]))
        P.do("pe", fns, reads=[srcb, const_b], writes=[PSb[bank]])
        inap = pv[:, 0:8 * rows].rearrange("p (k r) -> p k r", r=rows)
        if evac == "act":
            P.do("act", lambda e: e.activation(out=dstap, in_=inap, func=AF.Copy), reads=[PSb[bank]], writes=[dstb])
        else:
            P.do("dve", lambda e: e.tensor_copy(dstap, inap), reads=[PSb[bank]], writes=[dstb])

    class Epi:
        pass

    def make_epi(st, l, b, chunks):
        ep = Epi()
        ep.xn = [sb(f"xn{i}", [128, D], F32, st) for i in range(2)]
        ep.xnb = [Buf() for _ in range(2)]
        ep.xn_ds = [P.dsem() for _ in range(2)]
        ep.h2 = [sb(f"h2_{i}", [128, D], BF16, st) for i in range(2)]
        ep.h2b = [Buf() for _ in range(2)]
        ep.h2_ds = [P.dsem() for _ in range(2)]
        ep.h2T = sb("h2T", [128, 8, 128], BF16, st)
        ep.h2Tb = Buf()
        ep.nctx = NormCtx(st, "e")
        ep.wr = sb("wr", [128, 8, NE], BF16, st)
        ep.wrb = Buf()
        ep.sm = sb("sm", [128, 8], F32, st)
        ep.smb = Buf(small=True)
        ep.ex = sb("ex", [128, NE], F32, st)
        ep.aff = sb("aff48", [128, 48], F32, st)
        ep.affb = Buf(small=True)
        ep.ds = P.dsem()
        ep.cnt = 0
        P.dma("pool", lambda e: e.dma_start(out=ep.wr[:], in_=router_w[l].rearrange("(k p) n -> p k n", p=128)),
              ep.ds, writes=[ep.wrb])
        P.do("dve", lambda e: e.memset(ep.aff[:], 0.0), writes=[ep.affb])
        return ep

    def epilogue_gen(ep, l, b, c, ybanks, T_bank, L_bank):
        isctx = c >= NCHL
        G = modt[5] if isctx else modt[2]
        Gb = modb[5] if isctx else modb[2]
        A2 = modt[3] if isctx else modt[0]
        A2b = modb[3] if isctx else modb[0]
        B2 = modt[4] if isctx else modt[1]
        B2b = modb[4] if isctx else modb[1]
        s = ep.cnt % 2
        ep.cnt += 1
        xs = xt[s]
        P.dma("sp", lambda e: e.dma_start(out=xs[:], in_=xsrc(l, b, c)), xt_ds[s],
              reads=[Xc[b][c]], writes=[xtb[s]])
        xn, xnb = ep.xn[s], ep.xnb[s]
        P.do("dve", [lambda e: e.tensor_tensor(xn[:, 0:512], PS[ybanks[0]][:], G[:, 0:512], op=ALU.mult),
                     lambda e: e.tensor_tensor(xn[:, 512:1024], PS[ybanks[1]][:], G[:, 512:1024], op=ALU.mult)],
             reads=[PSb[ybanks[0]], PSb[ybanks[1]], Gb], writes=[xnb])
        P.do("dve", lambda e: e.tensor_tensor(xn[:], xn[:], xs[:], op=ALU.add), reads=[xtb[s], xnb], writes=[xnb])
        P.dma("sp", lambda e: e.dma_start(out=xdst(l, b)[c * 128:(c + 1) * 128, :], in_=xn[:]), ep.xn_ds[s],
              reads=[xnb], writes=[Xc[b][c]])
        h2, h2b = ep.h2[s], ep.h2b[s]
        rms_mod(ep.nctx, xn, xnb, A2, A2b, B2, B2b, h2, h2b)
        P.dma("sp", lambda e: e.dma_start(out=H[b][c * 128:(c + 1) * 128, :], in_=h2[:]), ep.h2_ds[s],
              reads=[h2b], writes=[Hc[b][c]])
        yield
        transpose8(h2, h2b, 128, T_bank, ep.h2T[:], ep.h2Tb)
        yield
        fns = []
        for k in range(8):
            fns.append(lambda e, k=k: e.matmul(PS[L_bank][:, 0:NE], lhsT=ep.h2T[:, k, :], rhs=ep.wr[:, k, :],
                                               start=(k == 0), stop=(k == 7)))
        P.do("pe", fns, reads=[ep.h2Tb, ep.wrb], writes=[PSb[L_bank]])
        sm = ep.sm
        col0 = 32 * b
        P.do("dve", [lambda e: e.memset(sm[:, 2:3], 0.0),
                     lambda e: e.reduce_max(out=sm[:, 0:1], in_=PS[L_bank][:, 0:NE], axis=AX.X)],
             reads=[PSb[L_bank]], writes=[ep.smb])
        P.do("dve", lambda e: e.tensor_scalar(sm[:, 1:2], sm[:, 0:1], -1.0, None, op0=ALU.mult), writes=[ep.smb])
        P.do("act", lambda e: e.activation(out=ep.ex[:], in_=PS[L_bank][:, 0:NE], func=AF.Exp, bias=sm[:, 1:2],
                                           accum_out=sm[:, 2:3]), reads=[PSb[L_bank], ep.smb], writes=[ep.smb])
        P.do("dve", lambda e: e.reciprocal(sm[:, 3:4], sm[:, 2:3]), writes=[ep.smb])
        P.do("dve", lambda e: e.tensor_scalar(ep.aff[:, col0:col0 + NE], ep.ex[:], sm[:, 3:4], None, op0=ALU.mult),
             reads=[ep.smb], writes=[ep.affb])
        ncol = col0 + NE
        yield
        P.do("pe", lambda e: e.transpose(PS[L_bank][0:ncol, 128:256], ep.aff[:, 0:ncol], ident_f[:]),
             reads=[ep.affb, const_b], writes=[PSb[L_bank]])
        P.do("dve", lambda e: e.tensor_copy(affT[col0:col0 + NE, c * 128:(c + 1) * 128], PS[L_bank][col0:col0 + NE, 128:256]),
             reads=[PSb[L_bank]], writes=[affTb])

    def epilogue(ep, l, b, c, ybanks, T_bank, L_bank):
        for _ in epilogue_gen(ep, l, b, c, ybanks, T_bank, L_bank):
            pass

    class Sched:
        def __init__(self):
            self.gens = []

        def add(self, g):
            self.gens.append(g)

        def step(self):
            alive = []
            for g in self.gens:
                try:
                    next(g)
                    alive.append(g)
                except StopIteration:
                    pass
            self.gens = alive

        def drain(self):
            while self.gens:
                self.step()

    def norm1_pass(st, l, b, chunks, hT, hTb, T_bank):
        nctx = NormCtx(st, "n")
        htok = [sb(f"htok{i}", [128, D], BF16, st) for i in range(2)]
        htokb = [Buf() for _ in range(2)]
        xts = list(xt) + [sb(f"xtn{i}", [128, D], F32, st) for i in range(2)]
        xtbs = list(xtb) + [Buf(), Buf()]
        xds = list(xt_ds) + [P.dsem(), P.dsem()]
        NSL = 4

        def load(i):
            c = chunks[i]
            s = i % NSL
            P.dma("sp", lambda e: e.dma_start(out=xts[s][:], in_=xsrc(l, b, c)), xds[s],
                  reads=[Xc[b][c]], writes=[xtbs[s]])

        for i in range(min(NSL - 1, len(chunks))):
            load(i)
        for i, c in enumerate(chunks):
            if i + NSL - 1 < len(chunks):
                load(i + NSL - 1)
            s = i % NSL
            h = i % 2
            isctx = c >= NCHL
            A, Ab = (modt[3], modb[3]) if isctx else (modt[0], modb[0])
            Bt, Bb = (modt[4], modb[4]) if isctx else (modt[1], modb[1])
            rms_mod(nctx, xts[s], xtbs[s], A, Ab, Bt, Bb, htok[h], htokb[h])
            if l == 0 and b == 0 and c == 0:
                dump(nctx.ss[:, 0:4], nctx.sb_, 0, 4)
                dump(htok[h][:], [htokb[h]], 1024, 1024)
                dump(A[:], [Ab], 2048, 1024)
                dump(Bt[:], [Bb], 3072, 1024)
            transpose8(htok[h], htokb[h], 128, T_bank, hT[:, :, c * 128:(c + 1) * 128], hTb[c])

    def load_mixer_mods(st, l, b, has_ctx, which, shared=None):
        if shared is None:
            shared = (sb(f"gtile{which}", [128, D], F32, st), Buf())
        gt, gtb = shared
        gds = P.dsem()
        base = 0 if which == 1 else 3
        gsrc = norm1_g if which == 1 else norm2_g
        load_scale(0, l, b, base + 1, gsrc, gt, gtb, gds)
        load_mod(1, l, b, base + 0)
        if which == 1:
            load_mod(2, l, b, 2)
        if has_ctx:
            load_scale(3, l, 2, base + 1, gsrc, gt, gtb, gds)
            load_mod(4, l, 2, base + 0)
            if which == 1:
                load_mod(5, l, 2, 2)
        return shared

    def load_win(st, l):
        j = l // 2
        Win = sb("Win", [128, 8, 1536], BF16, st)
        Winb = Buf()
        wds = P.dsem(f"winds{l}")
        src = attn_w_in[j].rearrange("(k p) n -> p k n", p=128)
        colmap = [(0, 512, 0), (768, 1280, 512), (512, 640, 1024), (1280, 1408, 1152), (640, 768, 1280), (1408, 1536, 1408)]
        fns = [lambda e, a=a, bb=bb, o=o: e.dma_start(out=Win[:, :, o:o + (bb - a)], in_=src[:, :, a:bb]) for a, bb, o in colmap]
        P.dma("pool", fns, wds, writes=[Winb])
        return Win, Winb

    def mixer_attn(l, b, has_ctx, Win, Winb):
        j = l // 2
        chunks = list(range(NCH if has_ctx else NCHL))
        precast_begin(l, b)
        with ExitStack() as st0:
            hq = sb("hq", [128, 8, NT], BF16, st0)
            hqb = [Buf() for _ in range(NCH)]
            kT = sb("kT", [128, 4, 2, NT], BF16, st0)
            kTb = [Buf() for _ in range(NCH)]
            vaug = sb("vaug", [128, NCH, 4, 65], BF16, st0)
            vb = [Buf() for _ in range(NCH)]
            ones_b = Buf()
            Wout = sb("Wout", [128, 8, D], BF16, st0)
            Woutb = Buf()
            with ExitStack() as st:
                gsh = load_mixer_mods(st, l, b, has_ctx, 1)
                g4 = sb("g4", [128, 4, 64], F32, st)
                GN = sb("GN", [128, 1280], F32, st)
                GNb = Buf()
                gds_ = P.dsem()
                P.dma("sp", lambda e: e.dma_start(out=g4[:].rearrange("p a d -> p (a d)"),
                                                  in_=qk_gain[j].rearrange("a d -> (a d)").partition_broadcast(128)),
                      gds_, writes=[GNb])
                hoffs = [(0, 0, 8), (1, 512, 8), (2, 1024, 2), (3, 1152, 2)]
                fns = []
                for gi, off, nh in hoffs:
                    fns.append(lambda e, gi=gi, off=off, nh=nh: e.tensor_copy(
                        GN[:, off:off + nh * 64].rearrange("p (h d) -> p h d", d=64),
                        g4[:, gi:gi + 1, :].to_broadcast([128, nh, 64])))
                P.do("dve", fns, reads=[GNb], writes=[GNb])
                P.do("dve", lambda e: e.tensor_scalar(GN[:, 0:1024], GN[:, 0:1024], 0.125, None, op0=ALU.mult), reads=[GNb], writes=[GNb])
                ktok = sb("ktok", [128, 4, 2, 128], BF16, st)
                ktokb = Buf()
                P.do("dve", [lambda e: e.memset(vaug[:, :, :, 64:65], 1.0), lambda e: e.memset(ktok[:], 0.0)],
                     writes=[ones_b, ktokb])
                with ExitStack() as stn:
                    norm1_pass(stn, l, b, chunks, hq, hqb, 0)
                P.barrier()
                load_mixer_mods(st, l, b, has_ctx, 2, gsh)
                wods = P.dsem()
                P.dma("pool", lambda e: e.dma_start(out=Wout[:], in_=attn_w_out[j].rearrange("(k p) n -> p k n", p=128)), wods, writes=[Woutb])

                sq2 = [sb(f"sq{i}", [128, 1280], F32, st) for i in range(2)]
                qn_1 = sb("qn", [128, 1280], F32, st)
                qn2 = [qn_1, qn_1]
                ssq2 = [sb(f"ssq{i}", [128, 64], F32, st) for i in range(2)]
                rt_1 = [sb(f"rt{i}", [128, 640], F32, st) for i in range(2)]
                rt2 = [rt_1, rt_1]
                qkn2 = [sb(f"qkn{i}", [128, 1280], BF16, st) for i in range(2)]
                wkb_1 = Buf()
                wkb2 = [wkb_1, wkb_1]
                sqb2 = [Buf(), Buf()]
                qknb2 = [Buf() for _ in range(2)]
                stb2 = [Buf(small=True) for _ in range(2)]
                rtb_1 = Buf()
                rtb2 = [rtb_1, rtb_1]
                pbanks2 = [(1, 2, 3), (0, 6, 7)]

                def projB(i, c):
                    cs_ = slice(c * 128, (c + 1) * 128)
                    pb = pbanks2[i % 2]
                    for nb_ in range(3):
                        fns = [lambda e, k=k, nb_=nb_: e.matmul(
                            PS[pb[nb_]][:], lhsT=hq[:, k, cs_], rhs=Win[:, k, nb_ * 512:(nb_ + 1) * 512],
                            start=(k == 0), stop=(k == 7)) for k in range(8)]
                        P.do("pe", fns, reads=[hqb[c], Winb], writes=[PSb[pb[nb_]]])

                def restB(i, c):
                    cs_ = slice(c * 128, (c + 1) * 128)
                    p = i % 2
                    pb = pbanks2[p]
                    sq, qn, ssq, rt, qkn = sq2[p], qn2[p], ssq2[p], rt2[p], qkn2[p]
                    wkb, qknb, stb, rtb_ = wkb2[p], qknb2[p], stb2[p], rtb2[p]
                    pbb = [PSb[x] for x in pb]
                    P.do("act", [lambda e: e.activation(out=sq[:, 0:512], in_=PS[pb[0]][:], func=AF.Square),
                                 lambda e: e.activation(out=sq[:, 512:1024], in_=PS[pb[1]][:], func=AF.Square),
                                 lambda e: e.activation(out=sq[:, 1024:1280], in_=PS[pb[2]][:, 0:256], func=AF.Square)],
                         reads=pbb, writes=[sqb2[p]])
                    P.do("act", lambda e: e.activation(out=vaug[:, c, :, 0:64], in_=PS[pb[2]][:, 256:512].rearrange("p (g d) -> p g d", d=64),
                                                       func=AF.Copy), reads=[pbb[2], ones_b], writes=[vb[c]])
                    P.do("dve", lambda e: e.tensor_reduce(out=ssq[:, 0:20], in_=sq[:].rearrange("p (h d) -> p h d", d=64),
                                                          axis=AX.X, op=ALU.add), reads=[sqb2[p]], writes=[stb])
                    P.do("act", lambda e: e.activation(out=ssq[:, 20:40], in_=ssq[:, 0:20], func=AF.Ln, bias=eps_t[:, 0:1], scale=1.0 / 64),
                         reads=[const_b], writes=[stb])
                    P.do("act", lambda e: e.activation(out=ssq[:, 40:60], in_=ssq[:, 20:40], func=AF.Exp, scale=-0.5), writes=[stb])

                def restB2(i, c):
                    cs_ = slice(c * 128, (c + 1) * 128)
                    p = i % 2
                    pb = pbanks2[p]
                    sq, qn, ssq, rt, qkn = sq2[p], qn2[p], ssq2[p], rt2[p], qkn2[p]
                    wkb, qknb, stb, rtb_ = wkb2[p], qknb2[p], stb2[p], rtb2[p]
                    pbb = [PSb[x] for x in pb]
                    P.do("dve", [
                        lambda e: e.tensor_tensor(qn[:, 0:512].rearrange("p (h d) -> p h d", d=64),
                                                  PS[pb[0]][:].rearrange("p (h d) -> p h d", d=64),
                                                  ssq[:, 40:48].unsqueeze(2).to_broadcast([128, 8, 64]), op=ALU.mult),
                        lambda e: e.tensor_tensor(qn[:, 512:1024].rearrange("p (h d) -> p h d", d=64),
                                                  PS[pb[1]][:].rearrange("p (h d) -> p h d", d=64),
                                                  ssq[:, 48:56].unsqueeze(2).to_broadcast([128, 8, 64]), op=ALU.mult),
                        lambda e: e.tensor_tensor(qn[:, 1024:1280].rearrange("p (h d) -> p h d", d=64),
                                                  PS[pb[2]][:, 0:256].rearrange("p (h d) -> p h d", d=64),
                                                  ssq[:, 56:60].unsqueeze(2).to_broadcast([128, 4, 64]), op=ALU.mult)],
                        reads=pbb + [stb], writes=[wkb])
                    if i + 2 < len(chunks):
                        projB(i + 2, chunks[i + 2])
                    P.do("dve", lambda e: e.tensor_tensor(qn[:], qn[:], GN[:], op=ALU.mult), reads=[wkb, GNb], writes=[wkb])
                    if c < NCHL:
                        qv = qn[:].rearrange("p (h rc ab f) -> p h rc ab f", rc=2, ab=2, f=16)
                        ov = qkn[:].rearrange("p (h rc ab f) -> p h rc ab f", rc=2, ab=2, f=16)
                        Aq = qv[:, :, :, 0, :]
                        Bq = qv[:, :, :, 1, :]
                        cosv = rope_t[:, c, 0:32].rearrange("p (rc f) -> p rc f", f=16).unsqueeze(1).to_broadcast([128, 20, 2, 16])
                        sinv = rope_t[:, c, 32:64].rearrange("p (rc f) -> p rc f", f=16).unsqueeze(1).to_broadcast([128, 20, 2, 16])
                        r4 = [t[:].rearrange("p (h rc f) -> p h rc f", rc=2, f=16) for t in rt]
                        P.do("dve", [
                            lambda e: e.tensor_tensor(r4[0], Aq, cosv, op=ALU.mult),
                            lambda e: e.tensor_tensor(r4[1], Bq, sinv, op=ALU.mult)],
                            reads=[wkb, const_b], writes=[rtb_])
                        P.do("dve", lambda e: e.tensor_tensor(ov[:, :, :, 0, :], r4[0], r4[1], op=ALU.subtract),
                             reads=[rtb_], writes=[qknb])
                        P.do("dve", [
                            lambda e: e.tensor_tensor(r4[0], Aq, sinv, op=ALU.mult),
                            lambda e: e.tensor_tensor(r4[1], Bq, cosv, op=ALU.mult)],
                            reads=[wkb, const_b], writes=[rtb_])
                        P.do("dve", lambda e: e.tensor_tensor(ov[:, :, :, 1, :], r4[0], r4[1], op=ALU.add),
                             reads=[rtb_, qknb], writes=[qknb])
                    else:
                        P.do("dve", lambda e: e.tensor_copy(qkn[:], qn[:]), reads=[wkb], writes=[qknb])
                    kv = qkn[:, 1024:1280].rearrange("p (g d) -> p g d", d=64)
                    P.do("dve", [lambda e: e.tensor_copy(ktok[:, :, 0, 0:64], kv),
                                 lambda e: e.tensor_copy(ktok[:, :, 1, 64:128], kv)], reads=[qknb], writes=[ktokb])
                    pv = PS[4][:].bitcast(BF16)
                    fns = [lambda e, i_=i_: e.transpose(pv[:, i_ * 128:(i_ + 1) * 128], qkn[:, i_ * 128:(i_ + 1) * 128], ident_b[:]) for i_ in range(8)]
                    P.do("pe", fns, reads=[qknb, const_b], writes=[PSb[4]])
                    P.do("act", lambda e: e.activation(out=hq[:, :, cs_], in_=pv.rearrange("p (k r) -> p k r", r=128), func=AF.Copy),
                         reads=[PSb[4]], writes=[hqb[c]])
                    pk = PS[5][:].bitcast(BF16)
                    fns = [lambda e, i_=i_: e.transpose(pk[:, i_ * 128:(i_ + 1) * 128], ktok[:, i_ // 2, i_ % 2, :], ident_b[:]) for i_ in range(8)]
                    P.do("pe", fns, reads=[ktokb, const_b], writes=[PSb[5]])
                    P.do("act", lambda e: e.activation(out=kT[:, :, :, cs_], in_=pk.rearrange("p (g r t) -> p g r t", r=2, t=128), func=AF.Copy),
                         reads=[PSb[5]], writes=[kTb[c]])

                projB(0, chunks[0])
                if len(chunks) > 1:
                    projB(1, chunks[1])
                restB(0, chunks[0])
                for i, c in enumerate(chunks):
                    if i + 1 < len(chunks):
                        restB(i + 1, chunks[i + 1])
                    restB2(i, c)
                if dbg and l == 0 and b == 0:
                    P.dma("pool", [lambda e: e.dma_start(out=DBGQ, in_=hq[:]), lambda e: e.dma_start(out=DBGK, in_=kT[:]),
                                   lambda e: e.dma_start(out=DBGV, in_=vaug[:])], dbg_ds, reads=hqb + kTb + vb)
            P.flush()
            with ExitStack() as st:
                ep = make_epi(st, l, b, chunks)
                sk0 = sb("sk0", [128, 8], F32, st)
                sk = sb("sk", [128, 8], F32, st)
                skb = Buf(small=True)
                skds = P.dsem()
                P.dma("sp", lambda e: e.dma_start(out=sk0[:], in_=sink_b[j].partition_broadcast(128)), skds, writes=[skb])
                P.do("act", lambda e: e.activation(out=sk0[:], in_=sk0[:], func=AF.Exp), reads=[skb], writes=[skb])
                P.do("dve", lambda e: e.tensor_copy(sk[:].rearrange("p (g r i) -> p g r i", r=2, i=2),
                                                    sk0[:].rearrange("p (g i r) -> p g r i", r=2, i=2)), reads=[skb], writes=[skb])
                PT = [sb(f"PT{i}", [128, 512], BF16, st) for i in range(3)]
                PTb = [Buf() for _ in range(3)]
                otok = [sb(f"otok{i}", [128, D], BF16, st) for i in range(2)]
                otokb = [Buf() for _ in range(2)]
                oT = sb("oT", [128, 8, 128], BF16, st)
                oTb = Buf()
                den = sb("den", [128, 8], F32, st)
                denb = Buf(small=True)
                S_banks = [0, 1]
                O_banks = [2, 3]
                sctr = 0
                octr = 0
                pctr = 0
                items = []
                for qi, qb in enumerate(chunks):
                    for ty in range(2):
                        for g in range(2):
                            if qb >= NCHL:
                                klist = [(NCHL, None), (NCHL + 1, None)]
                            elif ty == 0:
                                klist = [(kc, None) for kc in chunks]
                            else:
                                klist = []
                                if qb - 1 >= 0:
                                    klist.append((qb - 1, 0))
                                klist.append((qb, None))
                                if qb + 1 < NCHL:
                                    klist.append((qb + 1, 1))
                                if has_ctx:
                                    klist += [(NCHL, None), (NCHL + 1, None)]
                            ob = O_banks[octr % 2]
                            octr += 1
                            for ki, (kc, mk) in enumerate(klist):
                                items.append(dict(qi=qi, qb=qb, ty=ty, g=g, kc=kc, mk=mk, first=(ki == 0),
                                                  last=(ki == len(klist) - 1), ob=ob))
                for i, it in enumerate(items):
                    it["sbk"] = S_banks[i % 2]
                    it["pt"] = i % 3

                def emit_S(it):
                    qb, kc, sbk = it["qb"], it["kc"], it["sbk"]
                    gp = it["ty"] * 2 + it["g"]
                    pair0 = it["ty"] * 4 + it["g"] * 2
                    qs = slice(qb * 128, (qb + 1) * 128)
                    ks = slice(kc * 128, (kc + 1) * 128)
                    fns = [lambda e, r=r: e.matmul(
                        PS[sbk][:, r * 256:(r + 1) * 256].rearrange("p (i q) -> p i q", q=128),
                        lhsT=kT[:, gp, r, ks], rhs=hq[:, pair0:pair0 + 2, qs], start=True, stop=True) for r in range(2)]
                    P.do("pe", fns, reads=[kTb[kc], hqb[qb]], writes=[PSb[sbk]])

                def tail_gen(qi, qb):
                    ot, otb = otok[qi % 2], otokb[qi % 2]
                    if dbg and l == 0 and b == 0:
                        P.dma("pool", lambda e: e.dma_start(out=DBGO[:, qb, :], in_=ot[:]), dbg_ds, reads=[otb])
                    transpose8(ot, otb, 128, 4, oT[:], oTb)
                    yield
                    for nb_ in range(2):
                        fns = [lambda e, k=k, nb_=nb_: e.matmul(PS[5 + nb_][:], lhsT=oT[:, k, :], rhs=Wout[:, k, nb_ * 512:(nb_ + 1) * 512],
                                                               start=(k == 0), stop=(k == 7)) for k in range(8)]
                        P.do("pe", fns, reads=[oTb, Woutb], writes=[PSb[5 + nb_]])
                    yield from epilogue_gen(ep, l, b, qb, [5, 6], 4, 7)

                sched = Sched()

                def emit_rest(it):
                    qi, qb, ty, g, kc, mk, ob, sbk = it["qi"], it["qb"], it["ty"], it["g"], it["kc"], it["mk"], it["ob"], it["sbk"]
                    first, last = it["first"], it["last"]
                    gp = ty * 2 + g
                    pt, ptb = PT[it["pt"]], PTb[it["pt"]]
                    ot, otb = otok[qi % 2], otokb[qi % 2]
                    P.do("act", lambda e: e.activation(out=pt[:], in_=PS[sbk][:], func=AF.Exp), reads=[PSb[sbk]], writes=[ptb])
                    if mk is not None:
                        P.do("dve", lambda e: e.tensor_tensor(
                            pt[:].rearrange("p (c q) -> p c q", q=128), pt[:].rearrange("p (c q) -> p c q", q=128),
                            mask_t[:, mk:mk + 1, :].to_broadcast([128, 4, 128]), op=ALU.mult),
                            reads=[const_b, ptb], writes=[ptb])
                    fns = [lambda e, cb=cb: e.matmul(
                        PS[ob][:, cb * 65:(cb + 1) * 65], lhsT=pt[:, cb * 128:(cb + 1) * 128], rhs=vaug[:, kc, gp, :],
                        start=(first and cb == 0), stop=last, skip_group_check=True) for cb in range(4)]
                    if first:
                        P.do("pe", fns, reads=[ptb, vb[kc], ones_b], writes=[PSb[ob]])
                    else:
                        P.use("pe", reads=[ptb, vb[kc]])
                        for fn in fns[:-1]:
                            P.op("pe", fn)
                        ev = P.op("pe", fns[-1], mark=True)
                        P.done(ev, reads=[ptb, vb[kc]], writes=[])
                        PSb[ob].w = ev
                    if not last:
                        return
                    ov = PS[ob][:, 0:260].rearrange("p (c x) -> p c x", x=65)
                    dslice = den[:, g * 4:g * 4 + 4]
                    if ty == 1:
                        P.do("dve", lambda e: e.tensor_tensor(
                            dslice.unsqueeze(2), ov[:, :, 64:65], sk[:, g * 4:(g + 1) * 4].unsqueeze(2), op=ALU.add),
                            reads=[PSb[ob], skb], writes=[denb])
                    else:
                        P.do("dve", lambda e: e.tensor_copy(dslice.unsqueeze(2), ov[:, :, 64:65]), reads=[PSb[ob]], writes=[denb])
                    P.do("dve", lambda e: e.reciprocal(dslice, dslice), writes=[denb])
                    hb = ty * 8 + g * 4
                    outv = ot[:, hb * 64:(hb + 4) * 64].rearrange("p (i r d) -> p r i d", r=2, d=64)
                    inv = ov[:, :, 0:64].rearrange("p (r i) d -> p r i d", i=2)
                    rdv = dslice.rearrange("p (r i) -> p r i", i=2).unsqueeze(3).to_broadcast([128, 2, 2, 64])
                    P.do("dve", lambda e: e.tensor_tensor(outv, inv, rdv, op=ALU.mult), reads=[PSb[ob], denb], writes=[otb])
                    if ty == 1 and g == 1:
                        sched.add(tail_gen(qi, qb))

                emit_S(items[0])
                for i, it in enumerate(items):
                    if i + 1 < len(items):
                        emit_S(items[i + 1])
                    emit_rest(it)
                    if i % 3 == 2:
                        sched.step()
                    if i % 30 == 5:
                        precast_step()
                sched.drain()
                precast_finish()
            P.flush()

    def mixer_conv(l, b, has_ctx):
        j = l // 2
        chunks = list(range(NCH if has_ctx else NCHL))
        precast_begin(l, b)
        ntok = NT if has_ctx else NL
        with ExitStack() as st0:
            zT = sb("zT", [128, 8, NT], BF16, st0)
            zTb = Buf()
            Wout = sb("Wout", [128, 8, D], BF16, st0)
            Woutb = Buf()
            with ExitStack() as st:
                gsh = load_mixer_mods(st, l, b, has_ctx, 1)
                hT = sb("hT", [128, 8, NT], BF16, st)
                hTb = [Buf() for _ in range(NCH)]
                with ExitStack() as stn:
                    norm1_pass(stn, l, b, chunks, hT, hTb, 0)
                P.barrier()
                load_mixer_mods(st, l, b, has_ctx, 2, gsh)
                wods = P.dsem()
                P.dma("pool", lambda e: e.dma_start(out=Wout[:], in_=conv_w_out[j].rearrange("(k p) n -> p k n", p=128)), wods, writes=[Woutb])
                kb4 = sb("kb4", [4, D], F32, st)
                kk = sb("kk", [128, 8, 4], F32, st)
                kkb = Buf()
                tds = P.dsem()
                P.dma("sp", lambda e: e.dma_start(out=kb4[:], in_=conv_kb[j]), tds, writes=[kkb])
                fns = [lambda e, m=m: e.transpose(PS[1][:, m * 4:(m + 1) * 4], kb4[0:4, m * 128:(m + 1) * 128], ident_f[0:4, 0:4]) for m in range(8)]
                P.do("pe", fns, reads=[kkb, const_b], writes=[PSb[1]])
                P.do("dve", lambda e: e.tensor_copy(kk[:].rearrange("p m j -> p (m j)"), PS[1][:, 0:32]), reads=[PSb[1]], writes=[kkb])
                wc = [sb(f"wc{i}", [128, 8, 3, 128], BF16, st) for i in range(2)]
                wcb = [Buf() for _ in range(2)]
                wcd = [P.dsem() for _ in range(2)]
                UW = NT + 4
                u = sb("u", [128, UW], F32, st)
                ub = Buf()
                bgb_t = sb("bgb", [128, NT], F32, st)
                bgbb = Buf()
                yb_t = sb("yb", [128, NT], F32, st)
                ybb = Buf()
                vsb = [sb(f"vsb{i}", [128, 512], F32, st) for i in range(2)]
                vsbb = [Buf() for _ in range(2)]
                P.do("dve", lambda e: e.memset(u[:], 0.0), writes=[ub])
                src = conv_w_in[j].rearrange("(k p) n -> p k n", p=128)
                tbs = [(t0, 512) for t0 in range(0, NL, 512)]
                if has_ctx:
                    tbs.append((NL, 256))
                uoff = lambda t0: 1 + t0 if t0 < NL else 3 + t0
                vc = 0
                for m in range(8):
                    s = m % 2
                    fns = [lambda e, w=w, m=m, s=s: e.dma_start(out=wc[s][:, :, w, :], in_=src[:, :, w * D + m * 128:w * D + (m + 1) * 128]) for w in range(3)]
                    P.dma("pool", fns, wcd[s], writes=[wcb[s]])
                    for ti_, (t0, tw) in enumerate(tbs):
                        if ti_ % 2 == 0:
                            precast_step()
                        for w in range(3):
                            fns = [lambda e, k=k, w=w, s=s, t0=t0, tw=tw: e.matmul(PS[2 + w][:, 0:tw], lhsT=wc[s][:, k, w, :], rhs=hT[:, k, t0:t0 + tw],
                                                                                  start=(k == 0), stop=(k == 7)) for k in range(8)]
                            P.do("pe", fns, reads=[wcb[s]] + [hTb[cc] for cc in range(t0 // 128, (t0 + tw) // 128)], writes=[PSb[2 + w]])
                        vs_, vsb_ = vsb[vc % 2], vsbb[vc % 2]
                        vc += 1
                        P.do("act", [lambda e, t0=t0, tw=tw: e.activation(out=bgb_t[:, t0:t0 + tw], in_=PS[2][:, 0:tw], func=AF.Copy)],
                             reads=[PSb[2]], writes=[bgbb])
                        P.do("act", [lambda e, vs_=vs_, tw=tw: e.activation(out=vs_[:, 0:tw], in_=PS[4][:, 0:tw], func=AF.Copy)],
                             reads=[PSb[4]], writes=[vsb_])
                        P.do("dve", lambda e, vs_=vs_, t0=t0, tw=tw: e.tensor_tensor(u[:, uoff(t0):uoff(t0) + tw], PS[3][:, 0:tw], vs_[:, 0:tw], op=ALU.mult),
                             reads=[PSb[3], vsb_], writes=[ub])
                    segs = [(0, NL)] + ([(NL, NCTX)] if has_ctx else [])
                    for (t0, tw) in segs:
                        o = uoff(t0)
                        P.do("act", lambda e, m=m, t0=t0, tw=tw, o=o: e.activation(out=yb_t[:, t0:t0 + tw], in_=u[:, o:o + tw], func=AF.Identity,
                                                                               scale=kk[:, m, 1:2], bias=kk[:, m, 3:4]),
                             reads=[ub, kkb], writes=[ybb])
                        P.do("dve", lambda e, m=m, t0=t0, tw=tw, o=o: e.scalar_tensor_tensor(
                            out=yb_t[:, t0:t0 + tw], in0=u[:, o - 1:o - 1 + tw], scalar=kk[:, m, 0:1],
                            in1=yb_t[:, t0:t0 + tw], op0=ALU.mult, op1=ALU.add), reads=[ub, ybb, kkb], writes=[ybb])
                        P.do("dve", lambda e, m=m, t0=t0, tw=tw, o=o: e.scalar_tensor_tensor(
                            out=yb_t[:, t0:t0 + tw], in0=u[:, o + 1:o + 1 + tw], scalar=kk[:, m, 2:3],
                            in1=yb_t[:, t0:t0 + tw], op0=ALU.mult, op1=ALU.add), reads=[ub, ybb, kkb], writes=[ybb])
                        P.do("dve", lambda e, m=m, t0=t0, tw=tw: e.tensor_tensor(
                            zT[:, m, t0:t0 + tw], bgb_t[:, t0:t0 + tw], yb_t[:, t0:t0 + tw], op=ALU.mult),
                            reads=[ybb, bgbb], writes=[zTb])
            P.flush()
            with ExitStack() as st:
                ep = make_epi(st, l, b, chunks)
                yctr = 0
                sched = Sched()
                for c in chunks:
                    cs_ = slice(c * 128, (c + 1) * 128)
                    yb0 = 0 + 2 * (yctr % 2)
                    yctr += 1
                    for nb_ in range(2):
                        fns = [lambda e, k=k, nb_=nb_, cs_=cs_, yb0=yb0: e.matmul(PS[yb0 + nb_][:], lhsT=zT[:, k, cs_], rhs=Wout[:, k, nb_ * 512:(nb_ + 1) * 512],
                                                                                start=(k == 0), stop=(k == 7)) for k in range(8)]
                        P.do("pe", fns, reads=[zTb, Woutb], writes=[PSb[yb0 + nb_]])
                    sched.add(epilogue_gen(ep, l, b, c, [yb0, yb0 + 1], 4, 7))
                    sched.step()
                sched.drain()
                precast_finish()
            P.flush()

    def moe(l, has_ctx):
        pieces = [(0, 128), (128, 128)] + ([(256, 32)] if has_ctx else [])
        with ExitStack() as st0:
            VALS = sb("VALS", [128, 3, 48], F32, st0)
            IDX = sb("IDX", [128, 3, 48], I32, st0)
            rb = Buf(small=True)
            NS = 2
            Wg = [sb(f"Wg{i}", [128, 8, D], BF16, st0) for i in range(NS)]
            Wu = [sb(f"Wu{i}", [128, 8, D], BF16, st0) for i in range(NS)]
            Wd = [sb(f"Wd{i}", [128, 8, D], BF16, st0) for i in range(NS)]
            Wb = [[Buf() for _ in range(3)] for _ in range(NS)]
            Wds = [[P.dsem(f"wds{i}_{k}") for k in range(3)] for i in range(NS)] if not hasattr(P, "_wds") else P._wds
            P._wds = Wds

            def load_w(e_):
                s = e_ % NS
                for wi, dst in enumerate((Wg[s], Wu[s], Wd[s])):
                    P.dma("sp", lambda e, dst=dst, wi=wi, e_=e_: e.dma_start(out=dst[:], in_=WB[wi, e_].rearrange("(k p) f -> p k f", p=128)),
                          Wds[s][wi], reads=[WBb[e_][wi]], writes=[Wb[s][wi]])

            load_w(0)
            with ExitStack() as st:
                wk = sb("wk", [48, NL], F32, st)
                wkc = sb("wkc", [48, NCTX], F32, st)
                vals = sb("vals", [48, CAP_L + CAP_C], F32, st)
                idxu = sb("idxu", [48, CAP_L + CAP_C], U32, st)
                idxf = sb("idxf", [48, CAP_L + CAP_C], F32, st)
                tb_ = Buf(small=True)
                fns = [lambda e: e.tensor_copy(wk[:], affT[:, 0:NL])]
                for it in range(CAP_L // 8):
                    sl = slice(it * 8, (it + 1) * 8)
                    fns.append(lambda e, sl=sl: e.max(out=vals[:, sl], in_=wk[:]))
                    fns.append(lambda e, sl=sl: e.max_index(out=idxu[:, sl], in_max=vals[:, sl], in_values=wk[:]))
                    fns.append(lambda e, sl=sl: e.match_replace(out=wk[:], in_to_replace=vals[:, sl], in_values=wk[:], imm_value=-1.0))
                if has_ctx:
                    fns.append(lambda e: e.tensor_copy(wkc[:], affT[:, NL:NT]))
                    for it in range(CAP_C // 8):
                        sl = slice(CAP_L + it * 8, CAP_L + (it + 1) * 8)
                        fns.append(lambda e, sl=sl: e.max(out=vals[:, sl], in_=wkc[:]))
                        fns.append(lambda e, sl=sl: e.max_index(out=idxu[:, sl], in_max=vals[:, sl], in_values=wkc[:]))
                        fns.append(lambda e, sl=sl: e.match_replace(out=wkc[:], in_to_replace=vals[:, sl], in_values=wkc[:], imm_value=-1.0))
                fns.append(lambda e: e.tensor_copy(idxf[:], idxu[:]))
                if has_ctx:
                    fns.append(lambda e: e.tensor_scalar(idxf[:, CAP_L:CAP_L + CAP_C], idxf[:, CAP_L:CAP_L + CAP_C], float(NL), None, op0=ALU.add))
                P.use("dve", reads=[affTb])
                for fn in fns:
                    P.do("dve", fn, writes=[tb_])
                for pi, (so, rows) in enumerate(pieces):
                    P.do("pe", [lambda e, so=so, rows=rows: e.transpose(PS[0][0:rows, 0:48], vals[:, so:so + rows], ident_f[0:48, 0:48]),
                                lambda e, so=so, rows=rows: e.transpose(PS[0][0:rows, 64:112], idxf[:, so:so + rows], ident_f[0:48, 0:48])],
                         reads=[tb_, const_b], writes=[PSb[0]])
                    P.do("dve", [lambda e, pi=pi, rows=rows: e.tensor_copy(VALS[0:rows, pi, :], PS[0][0:rows, 0:48]),
                                 lambda e, pi=pi, rows=rows: e.tensor_copy(IDX[0:rows, pi, :], PS[0][0:rows, 64:112])],
                         reads=[PSb[0]], writes=[rb])
            P.flush()
            with ExitStack() as st:
                load_mod(0, l, 0, 5)
                load_mod(1, l, 1, 5)
                if has_ctx:
                    load_mod(3, l, 2, 5)
                xe = [[[sb(f"xe{b}_{pi}", [128, D], BF16, st) for pi in range(len(pieces))] for b in range(NB)] for _ in range(2)]
                xeb = [[[Buf() for _ in pieces] for b in range(NB)] for _ in range(2)]
                xeds = [[P.dsem() for b in range(NB)] for _ in range(2)]
                xeT = sb("xeT", [128, 8, 2 * 288], BF16, st)
                xeTb = [[Buf() for _ in pieces] for b in range(NB)]
                hidT = sb("hidT", [128, 8, 2 * 288], BF16, st)
                hidTb = Buf()
                sg = [sb(f"sg{i}", [128, 288], F32, st) for i in range(2)]
                sgb = [Buf() for _ in range(2)]
                ye = [sb(f"ye{i}", [128, D], F32, st) for i in range(3)]
                yeb = [Buf() for _ in range(3)]
                sc_ds = [P.dsem() for _ in range(NB)]
                ncols = 256 + (32 if has_ctx else 0)
                yectr = 0
                tctr = 0

                def issue_gathers(e_):
                    xs_ = e_ % 2
                    for b in range(NB):
                        col = b * 32 + e_
                        fns = [lambda e, b=b, pi=pi, rows=rows, col=col: e.indirect_dma_start(
                            out=xe[xs_][b][pi][0:rows, :], out_offset=None, in_=H[b],
                            in_offset=bass.IndirectOffsetOnAxis(ap=IDX[0:rows, pi, col:col + 1], axis=0))
                            for pi, (so, rows) in enumerate(pieces)]
                        P.dma("pool", fns, xeds[xs_][b], reads=[rb] + Hc[b], writes=xeb[xs_][b])

                def do_transposes(e_, b):
                    nonlocal tctr
                    xs_ = e_ % 2
                    for pi, (so, rows) in enumerate(pieces):
                        tbk = tctr % 2
                        tctr += 1
                        c0 = b * 288 + so
                        transpose8(xe[xs_][b][pi], xeb[xs_][b][pi], rows, tbk, xeT[:, :, c0:c0 + rows], xeTb[b][pi])

                issue_gathers(0)
                for e_ in range(NE):
                    s = e_ % NS
                    if e_ + 1 < NE:
                        load_w(e_ + 1)
                        issue_gathers(e_ + 1)
                    do_transposes(e_, 0)
                    for m in range(8):
                        ms = slice(m * 128, (m + 1) * 128)
                        for b in range(NB):
                            c0 = b * 288
                            if m == 0 and b == 1:
                                do_transposes(e_, 1)
                            for wi, W in enumerate((Wg[s], Wu[s])):
                                bank = 2 + b * 2 + wi
                                fns = [lambda e, k=k, W=W, bank=bank, c0=c0, ms=ms: e.matmul(PS[bank][:, 0:ncols], lhsT=W[:, k, ms], rhs=xeT[:, k, c0:c0 + ncols],
                                                                                           start=(k == 0), stop=(k == 7)) for k in range(8)]
                                P.do("pe", fns, reads=[Wb[s][wi]] + xeTb[b], writes=[PSb[bank]])
                            gb_, ub_ = 2 + b * 2, 3 + b * 2
                            P.do("act", lambda e, b=b, gb_=gb_: e.activation(out=sg[b][:, 0:ncols], in_=PS[gb_][:, 0:ncols], func=AF.Silu),
                                 reads=[PSb[gb_]], writes=[sgb[b]])
                            P.do("dve", lambda e, b=b, ub_=ub_, m=m, c0=c0: e.tensor_tensor(hidT[:, m, c0:c0 + ncols], sg[b][:, 0:ncols], PS[ub_][:, 0:ncols], op=ALU.mult),
                                 reads=[sgb[b], PSb[ub_]], writes=[hidTb])
                    for b in range(NB):
                        col = b * 32 + e_
                        scat = []
                        yused = []
                        for pi, (so, rows) in enumerate(pieces):
                            c0 = b * 288 + so
                            yi = yectr % 3
                            yectr += 1
                            Gt, Gtb = (modt[3], modb[3]) if pi == 2 else (modt[b], modb[b])
                            for nb_ in range(2):
                                bank = 6 + nb_
                                fns = [lambda e, k=k, bank=bank, c0=c0, rows=rows, nb_=nb_, s=s: e.matmul(PS[bank][0:rows, :], lhsT=hidT[:, k, c0:c0 + rows],
                                                                                                  rhs=Wd[s][:, k, nb_ * 512:(nb_ + 1) * 512],
                                                                                                  start=(k == 0), stop=(k == 7)) for k in range(8)]
                                P.do("pe", fns, reads=[hidTb, Wb[s][2]], writes=[PSb[bank]])
                                P.do("dve", lambda e, yi=yi, rows=rows, nb_=nb_, bank=bank, pi=pi, col=col, Gt=Gt: e.scalar_tensor_tensor(
                                    out=ye[yi][0:rows, nb_ * 512:(nb_ + 1) * 512], in0=PS[bank][0:rows, :], scalar=VALS[0:rows, pi, col:col + 1],
                                    in1=Gt[0:rows, nb_ * 512:(nb_ + 1) * 512], op0=ALU.mult, op1=ALU.mult),
                                    reads=[PSb[bank], rb, Gtb], writes=[yeb[yi]])
                            yused.append(yeb[yi])
                            scat.append(lambda e, b=b, yi=yi, rows=rows, pi=pi, col=col: e.indirect_dma_start(
                                out=xdst(l, b), out_offset=bass.IndirectOffsetOnAxis(ap=IDX[0:rows, pi, col:col + 1], axis=0),
                                in_=ye[yi][0:rows, :], in_offset=None, compute_op=ALU.add))
                        P.dma("pool", scat, sc_ds[b], reads=yused + [rb], writes=Xc[b])
            P.flush()

    def final():
        if not direct_out:
            ds = P.dsem("final")
            fns = []
            for b in range(NB):
                for r0 in range(0, NL, 256):
                    fns.append(lambda e, b=b, r0=r0: e.dma_start(out=OUT[b][r0:r0 + 256, :], in_=X[b][r0:r0 + 256, :]))
            ev = P.dma("sp", fns, ds, reads=[c for b in range(NB) for c in Xc[b]])
            P.engs["sp"].q.append(("w", ev[0], ev[1]))
        else:
            for b in range(NB):
                for c in Xc[b]:
                    P.wait("sp", c.w)
                    P.wait("pool", c.w)
        P.flush()

    phase0()
    done_ = False
    for l in range(n_layers):
        has_ctx = l < DEPTH - 1
        with ExitStack() as stl:
            if l % 2 == 0:
                Win_, Winb_ = load_win(stl, l)
            for b in range(NB):
                if l % 2 == 0:
                    mixer_attn(l, b, has_ctx, Win_, Winb_)
                else:
                    mixer_conv(l, b, has_ctx)
        if stop == (l, "mixer"):
            break
        moe(l, has_ctx)
        if stop == (l, "moe"):
            break
    final()
    es.close()
    return nc


def _host_consts():
    rows = NL // 64
    row = np.repeat(np.arange(rows, dtype=np.float32), 64)
    col = np.tile(np.arange(64, dtype=np.float32), rows)
    inv_freq = (10000.0 ** (-np.arange(0, 32, 2, dtype=np.float32) / 32.0)).astype(np.float32)
    ang_r = row[:, None] * inv_freq[None, :]
    ang_c = col[:, None] * inv_freq[None, :]
    rope = np.concatenate([np.cos(ang_r), np.cos(ang_c), np.sin(ang_r), np.sin(ang_c)], axis=1).astype(np.float32)
    ident = np.eye(128, dtype=np.float32)
    kk = np.arange(128)[:, None]
    qq = np.arange(128)[None, :]
    masks = np.stack([(kk >= qq), (kk <= qq)]).astype(np.float32)
    return rope, ident, masks


_NC_CACHE = {}
_LAST = None


def kernel(x, c, ctx, c_ctx, ada_w, ada_b, norm1_g, norm2_g, attn_w_in, attn_w_out,
           qnorm_a, knorm_a, qnorm_b, knorm_b, sink_b, conv_w_in, conv_k, conv_b, conv_w_out,
           router_w, moe_w_gate, moe_w_up, moe_w_down, _n_layers=DEPTH, _stop=None, _cores=8, _trace=False):
    f = lambda a: np.ascontiguousarray(np.asarray(a, dtype=np.float32))
    rope, ident, masks = _host_consts()
    qk_gain = np.stack([f(qnorm_a), f(qnorm_b), f(knorm_a), f(knorm_b)], axis=1)
    conv_kb = np.concatenate([f(conv_k), f(conv_b)[:, None, :]], axis=1)
    shared = {
        "ada_w": f(ada_w), "ada_b": f(ada_b), "norm1_g": f(norm1_g), "norm2_g": f(norm2_g),
        "attn_w_in": f(attn_w_in), "attn_w_out": f(attn_w_out), "qk_gain": np.ascontiguousarray(qk_gain),
        "sink_b": f(sink_b), "conv_w_in": f(conv_w_in), "conv_kb": np.ascontiguousarray(conv_kb),
        "conv_w_out": f(conv_w_out), "router_w": f(router_w), "moe_w_gate": f(moe_w_gate),
        "moe_w_up": f(moe_w_up), "moe_w_down": f(moe_w_down), "rope": rope, "ident": ident, "masks": masks,
    }
    x = f(x)
    ctx = f(ctx)
    c = f(c)
    c_ctx = f(c_ctx)
    key = (_n_layers, _stop)
    if key not in _NC_CACHE:
        _NC_CACHE[key] = build_program(_n_layers, _stop)
    nc = _NC_CACHE[key]
    in_maps = []
    for i in range(_cores):
        m = dict(shared)
        m["x"] = x[NB * i:NB * (i + 1)]
        m["ctx"] = ctx[NB * i:NB * (i + 1)]
        m["cvec"] = np.ascontiguousarray(np.concatenate([c[NB * i:NB * (i + 1)], c_ctx[None, :]], axis=0))
        in_maps.append(m)
    res = run_bass_kernel_spmd(nc, in_maps, core_ids=list(range(_cores)), **({"trace": True} if _trace else {}))
    global _LAST
    _LAST = res.results
    if _trace:
        print("EXEC_TIME_NS", res.exec_time_ns)
    return np.stack([np.asarray(r[f"out{b}"]) for r in res.results for b in range(NB)], axis=0).astype(np.float32)
```
